# Optimizing a Trainium2 kernel written in Bass

```python
import jax
import jax.numpy as jnp
from jax import lax
import numpy as np

D_MODEL = 2048
BATCH = 2
SEQ = 4096
DEPTH = 1

GRID_W = 64
CTX_LEN = 256
N_MOD = 6
NORM_EPS = 1e-6

RW_WIDTH = D_MODEL // 2
RW_HEAD_DIM = 64
RW_HEADS = RW_WIDTH // RW_HEAD_DIM
RW_DECAY_RANK = 64
RW_ICL_RANK = 64
RW_GATE_RANK = 160
RW_GN_EPS = 64e-5
RW_SIZES = (RW_WIDTH, RW_WIDTH, RW_WIDTH, RW_DECAY_RANK, RW_DECAY_RANK, RW_ICL_RANK, RW_ICL_RANK, RW_GATE_RANK)
RW_COLS = 3 * RW_WIDTH + 2 * RW_DECAY_RANK + 2 * RW_ICL_RANK + RW_GATE_RANK

HG_WIDTH = D_MODEL // 2
HG_KEY_DIM = 128
HG_HEADS = HG_WIDTH // HG_KEY_DIM
HG_VAL_DIM = HG_WIDTH // HG_HEADS
HG_CHUNK = 64
HG_SIZES = (HG_WIDTH, HG_WIDTH, HG_WIDTH, HG_WIDTH, HG_WIDTH)
HG_COLS = 5 * HG_WIDTH

IN_COLS = RW_COLS + HG_COLS + 2 * D_MODEL

PEER_HEADS = 8
PEER_NKEYS = 128
PEER_EXPERTS = PEER_NKEYS * PEER_NKEYS
PEER_KEY_DIM = 256
PEER_TOPK = 16
PEER_BLOCK = 128

kernel_name = "hybrid_rwkv7_hgrn2_peer_dit_block"


def rms_norm(x, gain):
    xf = x.astype(jnp.float32)
    y = xf * lax.rsqrt(jnp.mean(xf * xf, axis=-1, keepdims=True) + NORM_EPS)
    return (y * gain.astype(jnp.float32)).astype(x.dtype)


def modulate(h, shift, scale):
    return h * (1 + scale) + shift


def split_cols(z, sizes):
    return jnp.split(z, [int(s) for s in np.cumsum(sizes)[:-1]], axis=-1)


def to_heads(t, d):
    return t.reshape(t.shape[:-1] + (t.shape[-1] // d, d))


def shift_conv_grid(z, w, rows):
    b, l, ch = z.shape
    zg = z.reshape(b, rows, GRID_W, ch)
    y = lax.conv_general_dilated(zg, w[:, :, None, :].astype(z.dtype), window_strides=(1, 1), padding="SAME",
                                 dimension_numbers=("NHWC", "HWIO", "NHWC"), feature_group_count=ch)
    return y.reshape(b, l, ch)


def shift_conv_seq(z, w):
    zp = jnp.pad(z, ((0, 0), (1, 1), (0, 0)))
    wr = w[1].astype(z.dtype)
    return zp[:, :-2] * wr[0] + zp[:, 1:-1] * wr[1] + zp[:, 2:] * wr[2]


def rwkv_streams(z, w0, w_lora_b, a0, a_lora_b, g_lora_b, k_k, k_a):
    f32 = jnp.float32
    r, k, v, wa_f, wa_b, aa_f, aa_b, ga = split_cols(z, RW_SIZES)
    wa = jnp.stack([wa_f, wa_b])
    aa = jnp.stack([aa_f, aa_b])
    w_pre = (w0[:, None, None, :] + jnp.einsum("dblr,drc->dblc", jnp.tanh(wa), w_lora_b)).astype(f32)
    decay = jnp.exp(-jnp.exp(-jax.nn.softplus(-w_pre) - 0.5))
    a = jax.nn.sigmoid((a0[:, None, None, :] + jnp.einsum("dblr,drc->dblc", aa, a_lora_b)).astype(f32))
    g = jnp.einsum("blr,rc->blc", jax.nn.sigmoid(ga), g_lora_b)
    kk = to_heads((k * k_k).astype(f32), RW_HEAD_DIM)
    kk = kk / jnp.maximum(jnp.sqrt(jnp.sum(kk * kk, axis=-1, keepdims=True)), 1e-12)
    k_dir = k.astype(f32)[None] * (1 + (a - 1) * k_a.astype(f32))
    return {"r": to_heads(r.astype(f32), RW_HEAD_DIM), "v": to_heads(v.astype(f32), RW_HEAD_DIM),
            "k": to_heads(k_dir, RW_HEAD_DIM), "decay": to_heads(decay, RW_HEAD_DIM), "kk": kk,
            "kka": kk[None] * to_heads(a, RW_HEAD_DIM), "g": g}


def rwkv_scan(state0, r, decay, k, v, kk, kka):
    def step(state, inp):
        r_t, w_t, k_t, v_t, kk_t, kka_t = inp
        removal = jnp.einsum("bhvk,bhk->bhv", state, kk_t)
        state = (state * w_t[:, :, None, :] - removal[..., None] * kka_t[:, :, None, :]
                 + v_t[..., None] * k_t[:, :, None, :])
        return state, jnp.einsum("bhvk,bhk->bhv", state, r_t)
    xs = tuple(jnp.swapaxes(t, 0, 1) for t in (r, decay, k, v, kk, kka))
    state, out = lax.scan(step, state0, xs)
    return jnp.swapaxes(out, 0, 1), state


def rwkv_bidir(s, states0):
    flip = lambda t: jnp.flip(t, axis=1)
    o_f, s_f = rwkv_scan(states0[0], s["r"], s["decay"][0], s["k"][0], s["v"], s["kk"], s["kka"][0])
    o_b, s_b = rwkv_scan(states0[1], flip(s["r"]), flip(s["decay"][1]), flip(s["k"][1]), flip(s["v"]),
                         flip(s["kk"]), flip(s["kka"][1]))
    return o_f + flip(o_b), (s_f, s_b)


def hgrn_streams(z, lb):
    f32 = jnp.float32
    q, f_f, f_b, i, og = split_cols(z, HG_SIZES)
    lbb = lb[:, None, None, :]
    forget = lbb + (1 - lbb) * jax.nn.sigmoid(jnp.stack([f_f, f_b]).astype(f32))
    return {"q": to_heads(jax.nn.silu(q.astype(f32)), HG_KEY_DIM), "logf": to_heads(jnp.log(forget), HG_KEY_DIM),
            "k": to_heads(1 - forget, HG_KEY_DIM), "v": to_heads(i.astype(f32), HG_VAL_DIM), "og": og}


def hgrn_chunk_scan(state0, q, logf, k, v):
    b, l, h, _ = q.shape
    dv = v.shape[-1]
    n = l // HG_CHUNK
    blk = lambda t: t.reshape(b, n, HG_CHUNK, h, t.shape[-1])
    q, logf, k, v = blk(q), blk(logf), blk(k), blk(v)
    cum = jnp.cumsum(logf, axis=2)
    ref = cum[:, :, HG_CHUNK // 2][:, :, None]
    last = cum[:, :, -1]
    tri = jnp.tril(jnp.ones((HG_CHUNK, HG_CHUNK), dtype=bool))
    scores = jnp.einsum("bnthk,bnshk->bnhts", q * jnp.exp(cum - ref), k * jnp.exp(ref - cum))
    scores = jnp.where(tri, scores, 0.0)
    o_intra = jnp.einsum("bnhts,bnshv->bnthv", scores, v)
    d_state = jnp.einsum("bnshk,bnshv->bnhkv", k * jnp.exp(last[:, :, None] - cum), v)

    def step(state, inp):
        dec, ds = inp
        return dec[..., None] * state + ds, state

    state, starts = lax.scan(step, state0, (jnp.swapaxes(jnp.exp(last), 0, 1), jnp.swapaxes(d_state, 0, 1)))
    starts = jnp.swapaxes(starts, 0, 1)
    o_inter = jnp.einsum("bnthk,bnhkv->bnthv", q * jnp.exp(cum), starts)
    return (o_intra + o_inter).reshape(b, l, h, dv), state


def hgrn_bidir(s, states0):
    flip = lambda t: jnp.flip(t, axis=1)
    o_f, s_f = hgrn_chunk_scan(states0[0], s["q"], s["logf"][0], s["k"][0], s["v"])
    o_b, s_b = hgrn_chunk_scan(states0[1], flip(s["q"]), flip(s["logf"][1]), flip(s["k"][1]), flip(s["v"]))
    return o_f + flip(o_b), (s_f, s_b)


def token_mixer_core(h, rows, states0, lp):
    z = jnp.einsum("bld,dc->blc", h, lp["w_in"])
    z_rw, z_hg, gate_rw, gate_hg = split_cols(z, (RW_COLS, HG_COLS, D_MODEL, D_MODEL))
    if rows is None:
        z_rw = shift_conv_seq(z_rw, lp["rw_conv"])
    else:
        z_rw = shift_conv_grid(z_rw, lp["rw_conv"], rows)
    s_rw = rwkv_streams(z_rw, lp["rw_w0"], lp["rw_w_lora_b"], lp["rw_a0"], lp["rw_a_lora_b"],
                        lp["rw_g_lora_b"], lp["rw_k_k"], lp["rw_k_a"])
    s_hg = hgrn_streams(z_hg, lp["hg_lb"])
    o_rw, st_rw = rwkv_bidir(s_rw, states0[0])
    o_hg, st_hg = hgrn_bidir(s_hg, states0[1])
    return (o_rw, s_rw, o_hg, s_hg, gate_rw, gate_hg), (st_rw, st_hg)


def token_mixer_readout(feats, lp, dt):
    f32 = jnp.float32
    o_rw, s_rw, o_hg, s_hg, gate_rw, gate_hg = feats
    bl = o_rw.shape[:2]
    mu = jnp.mean(o_rw, axis=-1, keepdims=True)
    var = jnp.mean(jnp.square(o_rw - mu), axis=-1, keepdims=True)
    o_rw_n = ((o_rw - mu) * lax.rsqrt(var + RW_GN_EPS)).reshape(bl + (RW_WIDTH,))
    bonus = jnp.sum(s_rw["r"][None] * s_rw["k"] * lp["rw_r_k"].astype(f32), axis=(0, -1))[..., None] * s_rw["v"]
    y_rw = (o_rw_n * lp["rw_ln_w"].astype(f32) + lp["rw_ln_b"].astype(f32)
            + bonus.reshape(bl + (RW_WIDTH,))) * s_rw["g"].astype(f32)
    y_hg = o_hg * lax.rsqrt(jnp.mean(o_hg * o_hg, axis=-1, keepdims=True) + NORM_EPS) * lp["hg_norm"].astype(f32)
    y_hg = y_hg.reshape(bl + (HG_WIDTH,)) * jax.nn.silu(s_hg["og"].astype(f32))
    branch_rw = jnp.einsum("blc,cd->bld", y_rw.astype(dt), lp["w_up_rw"])
    branch_hg = jnp.einsum("blc,cd->bld", y_hg.astype(dt), lp["w_up_hg"])
    merged = jax.nn.sigmoid(gate_rw) * branch_rw + jax.nn.sigmoid(gate_hg) * branch_hg
    return jnp.einsum("bld,de->ble", merged, lp["w_out"])


def peer_ffn(h, wq, k1, k2, u_tab, v_tab):
    b, l, d = h.shape
    q = jnp.einsum("bld,dq->blq", h, wq).reshape(b, l, PEER_HEADS, 2, PEER_KEY_DIM // 2)
    s1 = jnp.einsum("blhd,nd->blhn", q[..., 0, :], k1).astype(jnp.float32)
    s2 = jnp.einsum("blhd,nd->blhn", q[..., 1, :], k2).astype(jnp.float32)
    v1, i1 = lax.top_k(s1, PEER_TOPK)
    v2, i2 = lax.top_k(s2, PEER_TOPK)
    cand = (v1[..., :, None] + v2[..., None, :]).reshape(b, l, PEER_HEADS, PEER_TOPK * PEER_TOPK)
    best, pos = lax.top_k(cand, PEER_TOPK)
    e1 = jnp.take_along_axis(i1, pos // PEER_TOPK, axis=-1)
    e2 = jnp.take_along_axis(i2, pos % PEER_TOPK, axis=-1)
    experts = e1 * PEER_NKEYS + e2
    gates = jax.nn.softmax(best, axis=-1).astype(h.dtype)
    n_sel = PEER_HEADS * PEER_TOPK
    nb = (b * l) // PEER_BLOCK
    hb = h.reshape(nb, PEER_BLOCK, d)
    eb = experts.reshape(nb, PEER_BLOCK, n_sel)
    gb = gates.reshape(nb, PEER_BLOCK, n_sel)

    def block(args):
        hx, ex, gx = args
        act = jax.nn.gelu(jnp.einsum("td,tkd->tk", hx, jnp.take(u_tab, ex, axis=0)), approximate=False)
        return jnp.einsum("tk,tkd->td", gx * act, jnp.take(v_tab, ex, axis=0))

    return lax.map(block, (hb, eb, gb)).reshape(b, l, d)


def setup_inputs(seed: int = 0) -> dict:
    key = jax.random.key(seed)
    ks = jax.random.split(key, 31)
    f32 = jnp.float32

    def nrm(k, shape, scale):
        return jax.random.normal(k, shape, f32) * scale

    d = D_MODEL
    rw_conv = nrm(ks[10], (DEPTH, 3, 3, RW_COLS), 0.1).at[:, 1, 1, :].add(0.5)
    return {
        "x": nrm(ks[0], (BATCH, SEQ, d), 1.0),
        "c": nrm(ks[1], (BATCH, d), 1.0),
        "ctx": nrm(ks[2], (BATCH, CTX_LEN, d), 1.0),
        "c_ctx": nrm(ks[3], (d,), 1.0),
        "w_ada": nrm(ks[4], (DEPTH, d, N_MOD * d), 0.5 * d ** -0.5),
        "b_ada": nrm(ks[5], (DEPTH, N_MOD * d), 0.02),
        "norm_mix": 1.0 + nrm(ks[6], (DEPTH, d), 0.05),
        "norm_ffn": 1.0 + nrm(ks[7], (DEPTH, d), 0.05),
        "norm_final": 1.0 + nrm(ks[8], (d,), 0.05),
        "w_in": nrm(ks[9], (DEPTH, d, IN_COLS), d ** -0.5),
        "rw_conv": rw_conv,
        "rw_w0": jax.random.uniform(ks[11], (DEPTH, 2, RW_WIDTH), f32, -5.0, 1.0),
        "rw_w_lora_b": nrm(ks[12], (DEPTH, 2, RW_DECAY_RANK, RW_WIDTH), 0.1 * RW_DECAY_RANK ** -0.5),
        "rw_a0": nrm(ks[13], (DEPTH, 2, RW_WIDTH), 0.5),
        "rw_a_lora_b": nrm(ks[14], (DEPTH, 2, RW_ICL_RANK, RW_WIDTH), 0.5 * RW_ICL_RANK ** -0.5),
        "rw_g_lora_b": nrm(ks[15], (DEPTH, RW_GATE_RANK, RW_WIDTH), RW_GATE_RANK ** -0.5),
        "rw_k_k": 0.85 + nrm(ks[16], (DEPTH, RW_WIDTH), 0.1),
        "rw_k_a": 1.0 + nrm(ks[17], (DEPTH, RW_WIDTH), 0.1),
        "rw_r_k": nrm(ks[18], (DEPTH, RW_HEADS, RW_HEAD_DIM), 0.5),
        "rw_ln_w": 1.0 + nrm(ks[19], (DEPTH, RW_WIDTH), 0.05),
        "rw_ln_b": nrm(ks[20], (DEPTH, RW_WIDTH), 0.02),
        "hg_lb": nrm(ks[21], (DEPTH + 1, 2, HG_WIDTH), 0.5),
        "hg_norm": 1.0 + nrm(ks[22], (DEPTH, HG_VAL_DIM), 0.05),
        "w_up_rw": nrm(ks[23], (DEPTH, RW_WIDTH, d), RW_WIDTH ** -0.5),
        "w_up_hg": nrm(ks[24], (DEPTH, HG_WIDTH, d), HG_WIDTH ** -0.5),
        "w_out": nrm(ks[25], (DEPTH, d, d), d ** -0.5),
        "peer_wq": nrm(ks[26], (DEPTH, d, PEER_HEADS * PEER_KEY_DIM), d ** -0.5),
        "peer_k1": nrm(ks[27], (DEPTH, PEER_NKEYS, PEER_KEY_DIM // 2), (PEER_KEY_DIM // 2) ** -0.5),
        "peer_k2": nrm(ks[28], (DEPTH, PEER_NKEYS, PEER_KEY_DIM // 2), (PEER_KEY_DIM // 2) ** -0.5),
        "peer_u": nrm(ks[29], (DEPTH, PEER_EXPERTS, d), d ** -0.5),
        "peer_v": nrm(ks[30], (DEPTH, PEER_EXPERTS, d), 0.5),
    }


def reference(x, c, ctx, c_ctx, w_ada, b_ada, norm_mix, norm_ffn, norm_final, w_in, rw_conv, rw_w0,
              rw_w_lora_b, rw_a0, rw_a_lora_b, rw_g_lora_b, rw_k_k, rw_k_a, rw_r_k, rw_ln_w, rw_ln_b,
              hg_lb, hg_norm, w_up_rw, w_up_hg, w_out, peer_wq, peer_k1, peer_k2, peer_u, peer_v):
    f32 = jnp.float32
    dt = x.dtype
    b = x.shape[0]
    rows = x.shape[1] // GRID_W
    lower_bounds = jnp.cumsum(jax.nn.softmax(hg_lb.astype(f32), axis=0), axis=0)
    zero_rw = jnp.zeros((b, RW_HEADS, RW_HEAD_DIM, RW_HEAD_DIM), f32)
    zero_hg = jnp.zeros((b, HG_HEADS, HG_KEY_DIM, HG_VAL_DIM), f32)
    for l in range(DEPTH):
        lp = {"w_in": w_in[l], "rw_conv": rw_conv[l], "rw_w0": rw_w0[l], "rw_w_lora_b": rw_w_lora_b[l],
              "rw_a0": rw_a0[l], "rw_a_lora_b": rw_a_lora_b[l], "rw_g_lora_b": rw_g_lora_b[l],
              "rw_k_k": rw_k_k[l], "rw_k_a": rw_k_a[l], "rw_r_k": rw_r_k[l], "rw_ln_w": rw_ln_w[l],
              "rw_ln_b": rw_ln_b[l], "hg_lb": lower_bounds[l], "hg_norm": hg_norm[l],
              "w_up_rw": w_up_rw[l], "w_up_hg": w_up_hg[l], "w_out": w_out[l]}
        mod_x = jnp.einsum("bd,de->be", jax.nn.silu(c), w_ada[l]) + b_ada[l]
        mod_c = jnp.einsum("d,de->e", jax.nn.silu(c_ctx), w_ada[l]) + b_ada[l]
        sh_mx, sc_mx, g_mx, sh_fx, sc_fx, g_fx = jnp.split(mod_x[:, None, :], N_MOD, axis=-1)
        sh_mc, sc_mc, g_mc, sh_fc, sc_fc, g_fc = jnp.split(mod_c[None, None, :], N_MOD, axis=-1)

        hc = modulate(rms_norm(ctx, norm_mix[l]), sh_mc, sc_mc)
        feats_c, ctx_states = token_mixer_core(hc, None, ((zero_rw, zero_rw), (zero_hg, zero_hg)), lp)

        hx = modulate(rms_norm(x, norm_mix[l]), sh_mx, sc_mx)
        feats_x, _ = token_mixer_core(hx, rows, ctx_states, lp)
        x = x + g_mx * token_mixer_readout(feats_x, lp, dt)
        hx = modulate(rms_norm(x, norm_ffn[l]), sh_fx, sc_fx)
        x = x + g_fx * peer_ffn(hx, peer_wq[l], peer_k1[l], peer_k2[l], peer_u[l], peer_v[l])

        if l < DEPTH - 1:
            ctx = ctx + g_mc * token_mixer_readout(feats_c, lp, dt)
            hc = modulate(rms_norm(ctx, norm_ffn[l]), sh_fc, sc_fc)
            ctx = ctx + g_fc * peer_ffn(hc, peer_wq[l], peer_k1[l], peer_k2[l], peer_u[l], peer_v[l])
    return rms_norm(x, norm_final)
```

```python
import os
import numpy as np
from contextlib import ExitStack
import concourse.bass as bass
import concourse.mybir as mybir
from concourse.bass_utils import run_bass_kernel_spmd

F32 = mybir.dt.float32
BF16 = mybir.dt.bfloat16
I32 = mybir.dt.int32
U32 = mybir.dt.uint32
AF = mybir.ActivationFunctionType
ALU = mybir.AluOpType
AX = mybir.AxisListType

D = 2048
T = 4352
NCH = 68
OWN = 1024
EPS = 1e-6
STAGE = 99
MARKS = []


class Buf:
    __slots__ = ("w", "r", "excl")

    def __init__(self, excl=False):
        self.w = None
        self.r = {}
        self.excl = excl


def PBuf():
    return Buf(True)


class Sch:
    def __init__(self, nc, es):
        self.nc = nc
        self.eng = {"pe": nc.tensor, "act": nc.scalar, "dve": nc.vector, "pool": nc.gpsimd, "sp": nc.sync}
        self.sem = {}
        self.cnt = {}
        self.NDS = 16
        names = ["pe", "act", "dve", "pool", "cc"] + [f"d_{q}_{i}" for q in ("sp", "act", "pool") for i in range(self.NDS)]
        for p in names:
            self.sem[p] = es.enter_context(nc.semaphore("s_" + p))
            self.cnt[p] = 0
        self.rr = {"sp": 0, "act": 0, "pool": 0}
        self.pe_pos = (0, 0)
        self.seen = {e: {} for e in self.eng}
        self.ninst = 0

    def _deps(self, reads, writes, e=None):
        deps = {}
        for b in reads:
            if b.w is not None and deps.get(b.w[0], 0) < b.w[1]:
                deps[b.w[0]] = b.w[1]
            if b.excl:
                for p, c in b.r.items():
                    if p != e and deps.get(p, 0) < c:
                        deps[p] = c
        for b in writes:
            if b.w is not None and deps.get(b.w[0], 0) < b.w[1]:
                deps[b.w[0]] = b.w[1]
            for p, c in b.r.items():
                if deps.get(p, 0) < c:
                    deps[p] = c
        return deps

    def _wait(self, e, deps):
        eng = self.eng[e]
        seen = self.seen[e]
        for p, c in deps.items():
            if p == "pe" and e == "pe":
                continue
            if seen.get(p, 0) >= c:
                continue
            eng.wait_ge(self.sem[p], c)
            seen[p] = c

    def _mark(self, prod, reads, writes):
        c = self.cnt[prod]
        for b in reads:
            if b.r.get(prod, 0) < c:
                b.r[prod] = c
        for b in writes:
            b.w = (prod, c)
            b.r = {}

    def op(self, e, fn, reads=(), writes=(), pos=(0, 0)):
        if e == "pe":
            if pos != self.pe_pos and self.cnt["pe"] > 0 and self.seen["pe"].get("pe_self", 0) < self.cnt["pe"]:
                self.eng["pe"].wait_ge(self.sem["pe"], self.cnt["pe"])
                self.seen["pe"]["pe_self"] = self.cnt["pe"]
            self.pe_pos = pos
        self._wait(e, self._deps(reads, writes, e))
        inst = fn(self.eng[e])
        self.cnt[e] += 1
        inst.then_inc(self.sem[e], 1)
        self._mark(e, reads, writes)
        self.ninst += 1
        return inst

    def _dsem(self, q):
        i = self.rr[q]
        self.rr[q] = (i + 1) % self.NDS
        prod = f"d_{q}_{i}"
        if self.cnt[prod] > 0:
            self._wait(q, {prod: self.cnt[prod]})
        return prod

    def dma(self, q, out, in_, reads=(), writes=(), **kw):
        prod = self._dsem(q)
        self._wait(q, self._deps(reads, writes))
        inst = self.eng[q].dma_start(out=out, in_=in_, **kw)
        self.cnt[prod] += 16
        inst.then_inc(self.sem[prod], 16)
        self._mark(prod, reads, writes)
        self.ninst += 1
        return inst

    def idma(self, reads=(), writes=(), **kw):
        prod = self._dsem("pool")
        self._wait("pool", self._deps(reads, writes))
        inst = self.eng["pool"].indirect_dma_start(**kw)
        self.cnt[prod] += 16
        inst.then_inc(self.sem[prod], 16)
        self._mark(prod, reads, writes)
        self.ninst += 1
        return inst

    def mark(self, name):
        MARKS.append((name, dict(self.cnt)))

    def barrier(self):
        deps = {p: c for p, c in self.cnt.items() if c > 0}
        for e in self.eng:
            self._wait(e, deps)

    def finish(self):
        self.barrier()


def build_nc(stage=99):
    nc = bass.Bass("TRN2", target_bir_lowering=False)

    def din(name, shape, dt=F32):
        return nc.dram_tensor(name, list(shape), dt, kind="ExternalInput").ap()

    seq = din("seq", [T, D])
    xown = din("xown", [OWN, D])
    cmod = din("cmod", [128, 16, 2])
    w_ada = din("w_ada", [6, 128, 16, 512])
    b_ada = din("b_ada", [1, 3072])
    mod_in = nc.dram_tensor("mod_in", [2, 3072], F32).ap()
    mod_out = nc.dram_tensor("mod_out", [8, 3072], F32).ap()
    nrm = din("nrm", [3, D])
    wA = din("wA", [20, 128, 16, 128])
    wB = din("wB", [32, 128, 16, 128])
    convw = din("convw", [10, 128, 9])
    cmask = din("cmask", [9, 128, 128])
    rwp = din("rwp", [2, 128, 9])
    wlb = din("wlb", [2, 128, 128])
    alb = din("alb", [2, 128, 128])
    glb = din("glb", [2, 160, 128])
    hlb = din("hlb", [2, 128, 4])
    hgn = din("hgn", [128, 1])
    selq = din("selq", [128, 4])
    wup = din("wup", [16, 128, 16, 128])
    wo = din("wo", [4, 128, 16, 512])
    wq = din("wq", [16, 128, 16, 128])
    k1T = din("k1T", [128, 128])
    k2T = din("k2T", [128, 128])
    peer_u = din("peer_u", [16384, D])
    peer_v = din("peer_v", [16384, D])
    SIMTAIL = bool(os.environ.get("K_SIMTAIL"))
    if SIMTAIL:
        ag_ref = din("ag_ref", [2048, 4096])
    out_d = nc.dram_tensor("out", [OWN, D], F32, kind="ExternalOutput").ap()
    dbg = nc.dram_tensor("dbg", [128, 8192], F32, kind="ExternalOutput").ap() if stage < 99 else None
    modD = nc.dram_tensor("modD", [2, 6 * D], F32).ap()
    zT = nc.dram_tensor("zT", [20 * 128, T], F32).ap()
    gT = nc.dram_tensor("gT", [32 * 128, OWN], F32).ap()
    ag_in_rw = [nc.dram_tensor(f"ag_in_rw{p}", [256, 512], F32).ap() for p in range(8)]
    ag_out_rw = [nc.dram_tensor(f"ag_out_rw{p}", [1024, 512], F32).ap() for p in range(8)]
    ag_in_h = [[nc.dram_tensor(f"ag_in_h{hh}_{p}", [128, 512], F32).ap() for p in range(8)] for hh in range(2)]
    ag_out_h = [[nc.dram_tensor(f"ag_out_h{hh}_{p}", [512, 512], F32).ap() for p in range(8)] for hh in range(2)]
    x1D = nc.dram_tensor("x1D", [OWN, D], F32).ap()
    hx2D = nc.dram_tensor("hx2D", [OWN, D], F32).ap()
    qD = nc.dram_tensor("qD", [D, OWN], F32).ap()
    uvD = nc.dram_tensor("uvD", [16384, 2 * D], BF16).ap()

    top = ExitStack()
    with top:
        S = Sch(nc, top)

        uid = [0]

        def sb(es, name, shape, dt=F32):
            uid[0] += 1
            return es.enter_context(nc.sbuf_tensor(f"{name}_{uid[0]}", list(shape), dt))

        def ps(es, name, shape, dt=F32):
            uid[0] += 1
            return es.enter_context(nc.psum_tensor(f"{name}_{uid[0]}", list(shape), dt))

        qs = ["sp", "act"]
        qi = [0]

        def nq():
            qi[0] += 1
            return qs[qi[0] % 2]

        identf = sb(top, "identf", [128, 128]); b_const = Buf()
        identb = sb(top, "identb", [128, 128], BF16)
        S.op("pool", lambda e: e.memset(identf[:], 0.0), writes=[b_const])
        S.op("pool", lambda e: e.affine_select(out=identf[:], in_=identf[:], pattern=[[-1, 128]], compare_op=ALU.not_equal, fill=1.0, base=0, channel_multiplier=1), reads=[b_const], writes=[b_const])
        S.op("dve", lambda e: e.tensor_copy(out=identb[:], in_=identf[:]), reads=[b_const], writes=[b_const])

        with ExitStack() as ph:
            cm = sb(ph, "cm", [128, 16, 2]); b_cm = Buf()
            cmb = sb(ph, "cmb", [128, 16, 2], BF16)
            ones2 = sb(ph, "ones2", [1, 2])
            wst = [sb(ph, f"wada{i}", [128, 16, 512]) for i in range(2)]; b_wst = [Buf(), Buf()]
            wsb = [sb(ph, f"wadab{i}", [128, 16, 512], BF16) for i in range(2)]; b_wsb = [Buf(), Buf()]
            brow = [sb(ph, f"brow{i}", [1, 512]) for i in range(2)]; b_brow = [Buf(), Buf()]
            mrow = [sb(ph, f"mrow{i}", [2, 512]) for i in range(2)]; b_mrow = [Buf(), Buf()]
            pm = [ps(ph, f"pm{i}", [2, 512]) for i in range(2)]; b_pm = [PBuf(), PBuf()]
            b_modD = Buf()
            S.dma("sp", cm[:], cmod, writes=[b_cm])
            S.op("act", lambda e: e.activation(out=cm[:], in_=cm[:], func=AF.Silu), reads=[b_cm], writes=[b_cm])
            S.op("dve", lambda e: e.memset(ones2[:], 1.0), writes=[b_cm])
            S.op("dve", lambda e: e.tensor_copy(out=cmb[:], in_=cm[:]), reads=[b_cm], writes=[b_cm])
            b_modin = Buf()
            for ch in range(6):
                i = ch % 2
                S.dma("sp" if ch % 2 == 0 else "act", wst[i][:, 0:8, :], w_ada[ch][:, 0:8, :], writes=[b_wst[i]])
                S.dma("pool", wst[i][:, 8:16, :], w_ada[ch][:, 8:16, :], writes=[b_wst[i]])
                S.dma("sp", brow[i][:], b_ada[0:1, ch * 512:(ch + 1) * 512], writes=[b_brow[i]])
                S.op("dve", lambda e: e.tensor_copy(out=wsb[i][:, 0:8, :], in_=wst[i][:, 0:8, :]), reads=[b_wst[i]], writes=[b_wsb[i]])
                S.op("pool", lambda e: e.tensor_copy(out=wsb[i][:, 8:16, :], in_=wst[i][:, 8:16, :]), reads=[b_wst[i]], writes=[b_wsb[i]])
                for k in range(16):
                    S.op("pe", lambda e: e.matmul(pm[i][:, :], lhsT=cmb[:, k, :], rhs=wsb[i][:, k, :], start=(k == 0), stop=False), reads=[b_cm, b_wsb[i]], writes=[b_pm[i]])
                S.op("pe", lambda e: e.matmul(pm[i][:, :], lhsT=ones2[0:1, :], rhs=brow[i][0:1, :], start=False, stop=True), reads=[b_cm, b_brow[i]], writes=[b_pm[i]])
                S.op("act", lambda e: e.activation(out=mrow[i][:], in_=pm[i][:], func=AF.Copy), reads=[b_pm[i]], writes=[b_mrow[i]])
                S.dma("sp", mod_in[:, ch * 512:(ch + 1) * 512], mrow[i][:], reads=[b_mrow[i]], writes=[b_modin])
            b_modout = Buf()
            S._wait("pool", S._deps([b_modin], [b_modout], "pool"))
            nc.gpsimd.collective_compute("AllGather", ALU.bypass, replica_groups=[[0, 1, 2, 3], [4, 5, 6, 7]], ins=[mod_in], outs=[mod_out]).then_inc(S.sem["cc"], 1)
            S.cnt["cc"] += 1
            b_modout.w = ("cc", S.cnt["cc"])
            for r in range(4):
                S.dma("sp", modD[:, 3072 * r:3072 * (r + 1)], mod_out[2 * r:2 * r + 2, :], reads=[b_modout], writes=[b_modD])
            S.barrier()

        S.mark("p0_adaLN")

        def bc_load(q, dst, src_row, bufs_w, reads=()):
            S.dma(q, dst, src_row.to_broadcast([128, src_row.shape[1]]), reads=list(reads), writes=bufs_w)

        def make_AB(ph, row, nrow, sc_off, sh_off, tag):
            A = sb(ph, "A" + tag, [128, D]); Bt = sb(ph, "B" + tag, [128, D]); bA = Buf(); bB = Buf()
            bc_load("sp", A[:], modD[row:row + 1, sc_off:sc_off + D], [bA], reads=[b_modD])
            bc_load("act", Bt[:], nrm[nrow:nrow + 1, :], [bB])
            S.op("dve", lambda e: e.scalar_tensor_tensor(out=A[:], in0=A[:], scalar=1.0, in1=Bt[:], op0=ALU.add, op1=ALU.mult), reads=[bA, bB], writes=[bA])
            bc_load("sp", Bt[:], modD[row:row + 1, sh_off:sh_off + D], [bB], reads=[b_modD, bA])
            return A, Bt, bA, bB

        def norm_tiles(ph, src, ntiles, ABsel, dstT, tag, src_buf=None, store=None):
            xt = [sb(ph, f"xt{tag}{i}", [128, D]) for i in range(2)]; b_xt = [Buf(), Buf()]
            hb = [sb(ph, f"hb{tag}{i}", [128, D], BF16) for i in range(2)]; b_hb = [Buf(), Buf()]
            st = [sb(ph, f"st{tag}{i}", [128, 2]) for i in range(2)]; b_st = [Buf(), Buf()]
            pT = [ps(ph, f"pT{tag}{i}", [128, 512], BF16) for i in range(2)]; b_pT = [PBuf(), PBuf()]
            b_dst = Buf()

            def tile_gen(tl):
                i = tl % 2
                A, Bt, bA, bB = ABsel(tl)
                S.dma("sp", xt[i][:, 0:D // 2], src[tl * 128:(tl + 1) * 128, 0:D // 2], reads=[src_buf] if src_buf is not None else [], writes=[b_xt[i]])
                S.dma("pool", xt[i][:, D // 2:D], src[tl * 128:(tl + 1) * 128, D // 2:D], reads=[src_buf] if src_buf is not None else [], writes=[b_xt[i]])
                S.op("dve", lambda e: e.memset(st[i][:], 0.0), writes=[b_st[i]])
                S.op("act", lambda e: e.activation(out=hb[i][:], in_=xt[i][:], func=AF.Square, accum_out=st[i][:, 0:1]), reads=[b_xt[i]], writes=[b_hb[i], b_st[i]])
                yield
                S.op("act", lambda e: e.activation(out=st[i][:, 1:2], in_=st[i][:, 0:1], func=AF.Sqrt, scale=1.0 / D, bias=EPS), reads=[b_st[i]], writes=[b_st[i]])
                yield
                S.op("dve", lambda e: e.reciprocal(out=st[i][:, 1:2], in_=st[i][:, 1:2]), reads=[b_st[i]], writes=[b_st[i]])
                S.op("dve", lambda e: e.scalar_tensor_tensor(out=xt[i][:], in0=xt[i][:], scalar=st[i][:, 1:2], in1=A[:], op0=ALU.mult, op1=ALU.mult), reads=[b_xt[i], b_st[i], bA], writes=[b_xt[i]])
                yield
                if store is None:
                    S.op("pool", lambda e: e.tensor_tensor(out=hb[i][:], in0=xt[i][:], in1=Bt[:], op=ALU.add), reads=[b_xt[i], bB], writes=[b_hb[i]])
                else:
                    S.op("pool", lambda e: e.tensor_tensor(out=xt[i][:], in0=xt[i][:], in1=Bt[:], op=ALU.add), reads=[b_xt[i], bB], writes=[b_xt[i]])
                    S.dma("act", store[0][tl * 128:(tl + 1) * 128, :], xt[i][:], reads=[b_xt[i]], writes=[store[1]])
                    S.op("pool", lambda e: e.tensor_copy(out=hb[i][:], in_=xt[i][:]), reads=[b_xt[i]], writes=[b_hb[i]])
                yield
                for kq in range(4):
                    j = kq % 2
                    for kk in range(4):
                        k = kq * 4 + kk
                        S.op("pe", lambda e: e.transpose(out=pT[j][:, kk * 128:(kk + 1) * 128], in_=hb[i][:, k * 128:(k + 1) * 128], identity=identb[:]), reads=[b_hb[i], b_const], writes=[b_pT[j]])
                    S.op("act" if kq % 2 == 0 else "dve", lambda e: (e.activation(out=dstT[:, kq * 4:(kq + 1) * 4, tl * 128:(tl + 1) * 128], in_=pT[j][:, :].rearrange("p (a b) -> p a b", a=4), func=AF.Copy) if kq % 2 == 0 else e.tensor_copy(out=dstT[:, kq * 4:(kq + 1) * 4, tl * 128:(tl + 1) * 128], in_=pT[j][:, :].rearrange("p (a b) -> p a b", a=4))), reads=[b_pT[j]], writes=[b_dst])
                    yield

            for t0_ in range(0, ntiles, 2):
                gens = [tile_gen(t_) for t_ in range(t0_, min(t0_ + 2, ntiles))]
                while gens:
                    for g_ in list(gens):
                        try:
                            next(g_)
                        except StopIteration:
                            gens.remove(g_)
            return b_dst

        def project(ph, wsrc, nchunks, hT, b_hT, ntok, dstD, b_dstD, tag):
            wf = [sb(ph, f"wf{tag}{i}", [128, 16, 128]) for i in range(3)]; b_wf = [Buf() for _ in range(3)]
            wb = [sb(ph, f"wb{tag}{i}", [128, 16, 128], BF16) for i in range(3)]; b_wb = [Buf() for _ in range(3)]
            ze = [sb(ph, f"ze{tag}{i}", [128, 512]) for i in range(3)]; b_ze = [Buf() for _ in range(3)]
            pz = [ps(ph, f"pz{tag}{i}", [128, 512]) for i in range(3)]; b_pz = [PBuf() for _ in range(3)]
            it = 0
            for j in range(nchunks):
                i = j % 3
                S.dma("sp", wf[i][:], wsrc[j], writes=[b_wf[i]])
                S.op("pool" if j % 2 else "dve", lambda e: e.tensor_copy(out=wb[i][:], in_=wf[i][:]), reads=[b_wf[i]], writes=[b_wb[i]])
                for t0 in range(0, ntok, 512):
                    n = min(512, ntok - t0)
                    pi = it % 3
                    it += 1
                    for k in range(16):
                        S.op("pe", lambda e: e.matmul(pz[pi][:, 0:n], lhsT=wb[i][:, k, :], rhs=hT[:, k, t0:t0 + n], start=(k == 0), stop=(k == 15)), reads=[b_wb[i], b_hT], writes=[b_pz[pi]])
                    S.op("act", lambda e: e.activation(out=ze[pi][:, 0:n], in_=pz[pi][:, 0:n], func=AF.Copy), reads=[b_pz[pi]], writes=[b_ze[pi]])
                    S.dma("act", dstD[j * 128:(j + 1) * 128, t0:t0 + n], ze[pi][:, 0:n], reads=[b_ze[pi]], writes=[b_dstD])

        b_zT = Buf(); b_gT = Buf()
        with ExitStack() as ph:
            hTo = sb(ph, "hTo", [128, 16, OWN], BF16)
            with ExitStack() as ph2:
                A, Bt, bA, bB = make_AB(ph2, 0, 0, 1 * D, 0 * D, "o")
                b_hTo = norm_tiles(ph2, xown, 8, lambda tl: (A, Bt, bA, bB), hTo, "o")
                S.barrier()
            S.mark("p1b_norm_own")
            project(ph, wB, 32, hTo, b_hTo, OWN, gT, b_gT, "g")
            S.barrier()
            S.mark("p2b_gate_proj")
        with ExitStack() as ph:
          if not SIMTAIL:
            hT = sb(ph, "hT", [128, 16, T], BF16)
            with ExitStack() as ph2:
                Ax, Bx, bAx, bBx = make_AB(ph2, 0, 0, 1 * D, 0 * D, "x")
                Ac, Bc, bAc, bBc = make_AB(ph2, 1, 0, 1 * D, 0 * D, "c")
                b_hT = norm_tiles(ph2, seq, 34, lambda tl: (Ac, Bc, bAc, bBc) if tl < 2 else (Ax, Bx, bAx, bBx), hT, "a")
                S.barrier()
            S.mark("p1_norm_all")
            project(ph, wA, 20, hT, b_hT, T, zT, b_zT, "z")
            S.barrier()
            S.mark("p2_head_proj")
        with ExitStack() as ph:
            PADW = 65
            zr = [sb(ph, f"zr{i}", [128, T]) for i in range(2)]; b_zr = [Buf(), Buf()]
            zc = [sb(ph, f"zc{i}", [128, T]) for i in range(2)]; b_zc = [Buf(), Buf()]
            cw = [sb(ph, f"cw{i}", [128, 9]) for i in range(2)]; b_cw = [Buf(), Buf()]
            zp = [[sb(ph, f"zp{i}{v}", [128, 4096 + 2 * PADW], BF16) for v in range(3)] for i in range(2)]; b_zp = [[Buf() for _ in range(3)] for _ in range(2)]
            dgc = [[sb(ph, f"dgc{i}{t_}", [128, 128], BF16) for t_ in range(9)] for i in range(2)]; b_dgc = [Buf(), Buf()]
            pcv = [ps(ph, f"pcv{i}", [128, 512]) for i in range(2)]; b_pcv = [PBuf(), PBuf()]
            for i in range(2):
                for v in range(3):
                    S.op("pool", lambda e: e.memset(zp[i][v][:, 0:PADW], 0.0), writes=[b_zp[i][v]])
                    S.op("pool", lambda e: e.memset(zp[i][v][:, PADW + 4096:], 0.0), writes=[b_zp[i][v]])
            it = 0
            for j in range(0 if SIMTAIL else 10):
                i = j % 2
                S.dma("sp", zr[i][:], zT[j * 128:(j + 1) * 128, :], reads=[b_zT], writes=[b_zr[i]])
                S.dma("sp", cw[i][:], convw[j], writes=[b_cw[i]])
                xmid = [zp[i][v][:, PADW:PADW + 4096] for v in range(3)]
                S.op("act", lambda e: e.activation(out=xmid[0], in_=zr[i][:, 256:T], func=AF.Copy), reads=[b_zr[i]], writes=[b_zp[i][0]])
                S.op("dve", lambda e: e.tensor_copy(out=xmid[1], in_=xmid[0]), reads=[b_zp[i][0]], writes=[b_zp[i][1]])
                S.op("dve", lambda e: e.tensor_copy(out=xmid[2], in_=xmid[0]), reads=[b_zp[i][0]], writes=[b_zp[i][2]])
                S.op("dve", lambda e: e.memset(xmid[1].rearrange("p (r c) -> p r c", c=64)[:, :, 63:64], 0.0), reads=[b_zp[i][1]], writes=[b_zp[i][1]])
                S.op("dve", lambda e: e.memset(xmid[2].rearrange("p (r c) -> p r c", c=64)[:, :, 0:1], 0.0), reads=[b_zp[i][2]], writes=[b_zp[i][2]])
                for tap in range(9):
                    S.op("act", lambda e: e.activation(out=dgc[i][tap][:], in_=identf[:], func=AF.Copy, scale=cw[i][:, tap:tap + 1]), reads=[b_const, b_cw[i]], writes=[b_dgc[i]])
                for tb in range(8):
                    pi = it % 2
                    it += 1
                    n_ = 0
                    for dy in (-1, 0, 1):
                        for dx in (-1, 0, 1):
                            tap = (dy + 1) * 3 + (dx + 1)
                            v = {0: 0, -1: 1, 1: 2}[dx]
                            s0 = PADW + tb * 512 + 64 * dy + dx
                            S.op("pe", lambda e: e.matmul(pcv[pi][:, :], lhsT=dgc[i][tap][:], rhs=zp[i][v][:, s0:s0 + 512], start=(n_ == 0), stop=(n_ == 8)), reads=[b_dgc[i], b_zp[i][v]], writes=[b_pcv[pi]])
                            n_ += 1
                    if pi == 0:
                        S.op("act", lambda e: e.activation(out=zc[i][:, 256 + tb * 512:256 + (tb + 1) * 512], in_=pcv[pi][:, :], func=AF.Copy), reads=[b_pcv[pi]], writes=[b_zc[i]])
                    else:
                        S.op("dve", lambda e: e.tensor_copy(out=zc[i][:, 256 + tb * 512:256 + (tb + 1) * 512], in_=pcv[pi][:, :]), reads=[b_pcv[pi]], writes=[b_zc[i]])
                S.op("dve", lambda e: e.tensor_scalar(out=zc[i][:, 0:256], in0=zr[i][:, 0:256], scalar1=cw[i][:, 4:5], scalar2=None, op0=ALU.mult), reads=[b_zr[i], b_cw[i]], writes=[b_zc[i]])
                S.op("dve", lambda e: e.scalar_tensor_tensor(out=zc[i][:, 1:256], in0=zr[i][:, 0:255], scalar=cw[i][:, 3:4], in1=zc[i][:, 1:256], op0=ALU.mult, op1=ALU.add), reads=[b_zr[i], b_cw[i], b_zc[i]], writes=[b_zc[i]])
                S.op("dve", lambda e: e.scalar_tensor_tensor(out=zc[i][:, 0:255], in0=zr[i][:, 1:256], scalar=cw[i][:, 5:6], in1=zc[i][:, 0:255], op0=ALU.mult, op1=ALU.add), reads=[b_zr[i], b_cw[i], b_zc[i]], writes=[b_zc[i]])
                S.dma("act", zT[j * 128:(j + 1) * 128, :], zc[i][:], reads=[b_zc[i], b_zr[i]], writes=[b_zT])
            S.barrier()

        S.mark("p2c_conv")
        if stage <= 2:
            with ExitStack() as ph:
                dt_ = sb(ph, "dbgt", [128, 8192]); bd = Buf()
                S.dma("sp", dt_[:, 0:T], zT[0:128, :], reads=[b_zT], writes=[bd])
                S.dma("sp", dt_[:, T:T + 1024], gT[0:128, :], reads=[b_gT], writes=[bd])
                S.dma("sp", dt_[:, 5376:5376 + 2048], zT[10 * 128:11 * 128, 0:2048], reads=[b_zT], writes=[bd])
                S.dma("sp", dt_[0:2, 7424:7424 + 512], modD[:, 0:512], reads=[b_modD], writes=[bd])
                S.dma("sp", dbg, dt_[:], reads=[bd])
                S.finish()
            return nc
        C0 = float(np.exp(-0.5))
        b_agin = Buf()
        msk = sb(top, "msk", [128, 9, 128]); b_msk = Buf()
        S.dma("sp", msk[:], cmask.rearrange("m p q -> p m q"), writes=[b_msk])
        bdones = msk[:, 6, :]

        def run(gens):
            gens = list(gens)
            while gens:
                for g_ in list(gens):
                    try:
                        next(g_)
                    except StopIteration:
                        gens.remove(g_)

        order = [list(range(NCH)), [3, 2, 1, 0] + list(range(NCH - 1, 3, -1))]

        b_tab = Buf()
        cst = {"i": 0}

        def cbufs(es):
            return ([sb(es, f"cf{i}", [128, D]) for i in range(2)], [sb(es, f"cb{i}", [128, D], BF16) for i in range(2)], [Buf(), Buf()], [Buf(), Buf()])

        def conv_tiles(cb_, n):
            cf, cb, b_cf, b_cb = cb_
            for _ in range(n):
                t = cst["i"]
                if t >= 256:
                    return
                cst["i"] += 1
                src, c0 = (peer_u, 0) if t < 128 else (peer_v, D)
                i = t % 2
                rs = slice((t % 128) * 128, (t % 128) * 128 + 128)
                S.dma("sp", cf[i][:], src[rs, :], writes=[b_cf[i]])
                S.op("pool", lambda e: e.tensor_copy(out=cb[i][:], in_=cf[i][:]), reads=[b_cf[i]], writes=[b_cb[i]])
                S.dma("pool", uvD[rs, c0:c0 + D], cb[i][:], reads=[b_cb[i]], writes=[b_tab])

        def v3(ap, a):
            return ap.rearrange("p (a b) -> p a b", a=a)

        def rwkv_hp(hp):
            with ExitStack() as php:
                BK = [sb(php, f"BK{d}", [128, NCH, 128], BF16) for d in range(2)]
                AR = [sb(php, f"AR{d}", [128, NCH, 128], BF16) for d in range(2)]
                gam = [sb(php, f"gam{d}", [128, NCH]) for d in range(2)]
                vTb = sb(php, "vTb", [128, T], BF16)
                bonT = sb(php, "bonT", [128, 4096], BF16); ggT = sb(php, "ggT", [128, 4096], BF16)
                Oacc = sb(php, "Oacc", [64, 64, 128])
                prm = sb(php, "prm", [128, 12]); wl = sb(php, "wl", [128, 128]); al = sb(php, "al", [128, 128])
                gl0 = sb(php, "gl0", [128, 128]); gl1 = sb(php, "gl1", [32, 128])
                b_str = Buf(); b_O = Buf(); b_prm = Buf(); b_bon = Buf()
                S.dma("sp", prm[:, 0:9], rwp[hp], writes=[b_prm])
                S.dma("act", wl[:], wlb[hp], writes=[b_prm])
                S.dma("sp", al[:], alb[hp], writes=[b_prm])
                S.dma("act", gl0[:], glb[hp, 0:128, :], writes=[b_prm])
                S.dma("sp", gl1[:], glb[hp, 128:160, :], writes=[b_prm])
                S.op("dve", lambda e: e.tensor_scalar(out=prm[:, 9:10], in0=prm[:, 5:6], scalar1=-1.0, scalar2=1.0, op0=ALU.mult, op1=ALU.add), reads=[b_prm], writes=[b_prm])
                S.op("pool", lambda e: e.memset(Oacc[:], 0.0), writes=[b_O])
                with ExitStack() as ph:
                    TB = 256
                    tl = {}

                    cur_par = [0]

                    def tt(name, p=128, dt=F32):
                        key = (name, cur_par[0])
                        if key not in tl:
                            tl[key] = (sb(ph, "s_" + name, [p, TB], dt), Buf())
                        return tl[key]
                    pAs = [ps(ph, f"pA{i}", [128, 512]) for i in range(2)]; pBs = [ps(ph, f"pB{i}", [128, 512]) for i in range(2)]
                    pCs = [ps(ph, f"pC{i}", [128, 512]) for i in range(2)]; pDs = [ps(ph, f"pD{i}", [128, 512]) for i in range(2)]
                    b_pAs = [PBuf(), PBuf()]; b_pBs = [PBuf(), PBuf()]; b_pCs = [PBuf(), PBuf()]; b_pDs = [PBuf(), PBuf()]
                    def blkgen(blk):
                        t0 = blk * TB
                        c0 = blk * 4
                        cur_par[0] = blk % 2
                        pA, pB, pC, pD = pAs[blk % 2], pBs[blk % 2], pCs[blk % 2], pDs[blk % 2]
                        b_pA, b_pB, b_pC, b_pD = b_pAs[blk % 2], b_pBs[blk % 2], b_pCs[blk % 2], b_pDs[blk % 2]
                        b_pC2 = b_pC
                        rows = [3 * hp, 3 * hp + 1, 3 * hp + 2, 6, 7, 8, 9]
                        nm = ["r", "k", "v", "L0", "L1", "L2", "L3"]
                        for rr, n_ in zip(rows, nm):
                            t_, b_ = tt(n_)
                            S.dma("sp", t_[:], zT[rr * 128:(rr + 1) * 128, t0:t0 + TB], reads=[b_zT], writes=[b_])
                        (r_, b_r), (k_, b_k), (v_, b_v) = tt("r"), tt("k"), tt("v")
                        (L0, b_L0), (L1, b_L1), (L2, b_L2), (L3, b_L3) = tt("L0"), tt("L1"), tt("L2"), tt("L3")
                        yield
                        cur_par[0] = blk % 2
                        th, b_th = tt("th")
                        S.op("act", lambda e: e.activation(out=th[:], in_=L0[:], func=AF.Tanh), reads=[b_L0], writes=[b_th])
                        for d in range(2):
                            S.op("pe", lambda e: e.matmul(pA[:, d * 256:(d + 1) * 256], lhsT=wl[64 * d:64 * d + 64, :], rhs=th[64 * d:64 * d + 64, :], start=True, stop=True), reads=[b_prm, b_th], writes=[b_pA], pos=(64 * d, 0))
                            S.op("pe", lambda e: e.matmul(pB[:, d * 256:(d + 1) * 256], lhsT=al[64 * d:64 * d + 64, :], rhs=L1[64 * d:64 * d + 64, :], start=True, stop=True), reads=[b_prm, b_L1], writes=[b_pB], pos=(64 * d, 0))
                        s2, b_s2 = tt("s2"); s3, b_s3 = tt("s3")
                        S.op("act", lambda e: e.activation(out=s2[:], in_=L2[:], func=AF.Sigmoid), reads=[b_L2], writes=[b_s2])
                        S.op("act", lambda e: e.activation(out=s3[0:32, :], in_=L3[0:32, :], func=AF.Sigmoid), reads=[b_L3], writes=[b_s3])
                        S.op("pe", lambda e: e.matmul(pC[:, 0:256], lhsT=gl0[:, :], rhs=s2[:, :], start=True, stop=False), reads=[b_prm, b_s2], writes=[b_pC])
                        S.op("pe", lambda e: e.matmul(pC[:, 0:256], lhsT=gl1[0:32, :], rhs=s3[0:32, :], start=False, stop=True), reads=[b_prm, b_s3], writes=[b_pC])
                        yield
                        cur_par[0] = blk % 2
                        sg = []; aa = []
                        for d in range(2):
                            sgd, b_sgd = tt(f"sg{d}"); ad, b_ad = tt(f"a{d}")
                            S.op("act", lambda e: e.activation(out=sgd[:], in_=pA[:, d * 256:(d + 1) * 256], func=AF.Sigmoid, bias=prm[:, d:d + 1]), reads=[b_pA, b_prm], writes=[b_sgd])
                            S.op("act", lambda e: e.activation(out=ad[:], in_=pB[:, d * 256:(d + 1) * 256], func=AF.Sigmoid, bias=prm[:, 2 + d:3 + d]), reads=[b_pB, b_prm], writes=[b_ad])
                            sg.append((sgd, b_sgd)); aa.append((ad, b_ad))
                        yield
                        cur_par[0] = blk % 2
                        kkr, b_kkr = tt("kkr"); sq, b_sq = tt("sq"); nr, b_nr = tt("nr"); kk, b_kk = tt("kk")
                        S.op("dve", lambda e: e.tensor_scalar(out=kkr[:], in0=k_[:], scalar1=prm[:, 4:5], scalar2=None, op0=ALU.mult), reads=[b_k, b_prm], writes=[b_kkr])
                        S.op("pool", lambda e: e.tensor_tensor(out=sq[:], in0=kkr[:], in1=kkr[:], op=ALU.mult), reads=[b_kkr], writes=[b_sq])
                        S.op("pe", lambda e: e.matmul(pC[:, 256:512], lhsT=bdones, rhs=sq[:, :], start=True, stop=True), reads=[b_msk, b_sq], writes=[b_pC2])
                        S.op("act", lambda e: e.activation(out=nr[:], in_=pC[:, 256:512], func=AF.Sqrt), reads=[b_pC2], writes=[b_nr])
                        S.op("dve", lambda e: e.tensor_scalar_max(out=nr[:], in0=nr[:], scalar1=1e-12), reads=[b_nr], writes=[b_nr])
                        S.op("dve", lambda e: e.reciprocal(out=nr[:], in_=nr[:]), reads=[b_nr], writes=[b_nr])
                        S.op("pool", lambda e: e.tensor_tensor(out=kk[:], in0=kkr[:], in1=nr[:], op=ALU.mult), reads=[b_kkr, b_nr], writes=[b_kk])
                        yield
                        cur_par[0] = blk % 2
                        kd = []; kka = []
                        for d in range(2):
                            ad, b_ad = aa[d]
                            tm, b_tm = tt("tm"); kdd, b_kdd = tt(f"kd{d}"); kkad, b_kkad = tt(f"kka{d}")
                            S.op("dve", lambda e: e.tensor_scalar(out=tm[:], in0=ad[:], scalar1=prm[:, 5:6], scalar2=prm[:, 9:10], op0=ALU.mult, op1=ALU.add), reads=[b_ad, b_prm], writes=[b_tm])
                            S.op("pool", lambda e: e.tensor_tensor(out=kdd[:], in0=tm[:], in1=k_[:], op=ALU.mult), reads=[b_tm, b_k], writes=[b_kdd])
                            S.op("pool", lambda e: e.tensor_tensor(out=kkad[:], in0=kk[:], in1=ad[:], op=ALU.mult), reads=[b_kk, b_ad], writes=[b_kkad])
                            kd.append((kdd, b_kdd)); kka.append((kkad, b_kkad))
                        yield
                        cur_par[0] = blk % 2
                        for d in range(2):
                            sgd, b_sgd = sg[d]
                            cs, b_cs = tt(f"cs{d}"); ce, b_ce = tt(f"ce{d}"); ci, b_ci = tt(f"ci{d}")
                            for c in range(4):
                                S.op("dve", lambda e: e.tensor_tensor_scan(out=cs[:, c * 64:(c + 1) * 64], data0=sgd[:, c * 64:(c + 1) * 64], data1=sgd[:, c * 64:(c + 1) * 64], initial=0.0, op0=ALU.add, op1=ALU.bypass), reads=[b_sgd], writes=[b_cs])
                            if d == 0:
                                S.op("pool", lambda e: e.tensor_tensor(out=ce[:], in0=cs[:], in1=sgd[:], op=ALU.subtract), reads=[b_cs, b_sgd], writes=[b_ce])
                                ciu, b_ciu = cs, b_cs
                            else:
                                S.op("dve", lambda e: e.tensor_tensor(out=v3(ce[:, :], 4), in0=v3(cs[:, :], 4)[:, :, 63:64].to_broadcast([128, 4, 64]), in1=v3(cs[:, :], 4), op=ALU.subtract), reads=[b_cs], writes=[b_ce])
                                S.op("pool", lambda e: e.tensor_tensor(out=ci[:], in0=ce[:], in1=sgd[:], op=ALU.add), reads=[b_ce, b_sgd], writes=[b_ci])
                                ciu, b_ciu = ci, b_ci
                            Ein, b_Ein = tt("Ein"); Eex, b_Eex = tt("Eex"); Einv, b_Einv = tt("Einv")
                            S.op("act", lambda e: e.activation(out=Ein[:], in_=ciu[:], func=AF.Exp, scale=-C0), reads=[b_ciu], writes=[b_Ein])
                            S.op("act", lambda e: e.activation(out=Eex[:], in_=ce[:], func=AF.Exp, scale=-C0), reads=[b_ce], writes=[b_Eex])
                            S.op("act", lambda e: e.activation(out=Einv[:], in_=ciu[:], func=AF.Exp, scale=C0), reads=[b_ciu], writes=[b_Einv])
                            S.op("act", lambda e: e.activation(out=gam[d][:, c0:c0 + 4], in_=v3(cs[:, :], 4)[:, :, 63], func=AF.Exp, scale=-C0), reads=[b_cs], writes=[b_str])
                            kdd, b_kdd = kd[d]; kkad, b_kkad = kka[d]
                            S.op("dve", lambda e: e.tensor_tensor(out=AR[d][:, c0:c0 + 4, 0:64], in0=v3(kk[:, :], 4), in1=v3(Eex[:, :], 4), op=ALU.mult), reads=[b_kk, b_Eex], writes=[b_str])
                            S.op("pool", lambda e: e.tensor_tensor(out=AR[d][:, c0:c0 + 4, 64:128], in0=v3(r_[:, :], 4), in1=v3(Ein[:, :], 4), op=ALU.mult), reads=[b_r, b_Ein], writes=[b_str])
                            S.op("dve", lambda e: e.scalar_tensor_tensor(out=BK[d][:, c0:c0 + 4, 0:64], in0=v3(kkad[:, :], 4), scalar=-1.0, in1=v3(Einv[:, :], 4), op0=ALU.mult, op1=ALU.mult), reads=[b_kkad, b_Einv], writes=[b_str])
                            S.op("pool", lambda e: e.tensor_tensor(out=BK[d][:, c0:c0 + 4, 64:128], in0=v3(kdd[:, :], 4), in1=v3(Einv[:, :], 4), op=ALU.mult), reads=[b_kdd, b_Einv], writes=[b_str])
                        yield
                        cur_par[0] = blk % 2
                        S.op("act", lambda e: e.activation(out=vTb[:, t0:t0 + TB], in_=v_[:], func=AF.Copy), reads=[b_v], writes=[b_str])
                        if blk >= 1:
                            tx0 = t0 - 256
                            rk, b_rk = tt("rk"); kds, b_kds = tt("kds")
                            S.op("dve", lambda e: e.tensor_scalar(out=rk[:], in0=r_[:], scalar1=prm[:, 6:7], scalar2=None, op0=ALU.mult), reads=[b_r, b_prm], writes=[b_rk])
                            S.op("pool", lambda e: e.tensor_tensor(out=kds[:], in0=kd[0][0][:], in1=kd[1][0][:], op=ALU.add), reads=[kd[0][1], kd[1][1]], writes=[b_kds])
                            S.op("pool", lambda e: e.tensor_tensor(out=kds[:], in0=kds[:], in1=rk[:], op=ALU.mult), reads=[b_kds, b_rk], writes=[b_kds])
                            S.op("pe", lambda e: e.matmul(pD[:, 0:256], lhsT=bdones, rhs=kds[:, :], start=True, stop=True), reads=[b_msk, b_kds], writes=[b_pD])
                            S.op("dve", lambda e: e.tensor_tensor(out=bonT[:, tx0:tx0 + TB], in0=pD[:, 0:256], in1=v_[:], op=ALU.mult), reads=[b_pD, b_v], writes=[b_bon])
                            S.op("act", lambda e: e.activation(out=ggT[:, tx0:tx0 + TB], in_=pC[:, 0:256], func=AF.Copy), reads=[b_pC], writes=[b_bon])
                    for b0 in range(0, T // TB, 2):
                        run([blkgen(b_) for b_ in range(b0, min(b0 + 2, T // TB))])
                    S.barrier()
                S.mark(f"rw{hp}_streams")
                if os.environ.get("K_PHASE") == "streams":
                    return
                with ExitStack() as ph:
                    bank = [[ps(ph, f"bk{d}{i}", [128, 512]) for i in range(3)] for d in range(2)]
                    bankb = [ps(ph, f"bkb{d}", [128, 1024], BF16) for d in range(2)]
                    PM = [v3(bank[d][0][:, 0:256], 2) for d in range(2)]
                    PX = [bank[d][0][:, 256:384] for d in range(2)]
                    PY = [bank[d][0][:, 384:512] for d in range(2)]
                    PN = [[bank[d][1][:, 128 * i:128 * (i + 1)] for i in range(3)] for d in range(2)]
                    PW = [bank[d][2][:, 256:320] for d in range(2)]
                    PS_ = [bank[d][2][:, 320:384] for d in range(2)]
                    PU = [v3(bank[d][2][0:64, 0:128], 2) for d in range(2)]
                    PO = [v3(bank[d][2][0:64, 128:256], 2) for d in range(2)]
                    PVt = [v3(bankb[d][:, 0:128], 2) for d in range(2)]
                    PBt = [v3(bankb[d][:, 128:256], 2) for d in range(2)]
                    nb = lambda: Buf()
                    b_bank = [[PBuf() for _ in range(4)] for _ in range(2)]
                    b_PM = [b_bank[d][0] for d in range(2)]; b_PX = b_PM; b_PY = b_PM
                    b_PN = [[b_bank[d][1]] * 3 for d in range(2)]
                    b_PW = [b_bank[d][2] for d in range(2)]; b_PS = b_PW; b_PU = b_PW; b_PO = b_PW
                    b_PVt = [b_bank[d][3] for d in range(2)]; b_PBt = b_PVt
                    MS = [[sb(ph, f"MS{d}{p}", [128, 2, 128], BF16) for p in range(2)] for d in range(2)]; b_MS = [[nb(), nb()] for _ in range(2)]
                    Xs = [[sb(ph, f"Xs{d}{p}", [128, 128], BF16) for p in range(2)] for d in range(2)]; b_Xs = [[nb(), nb()] for _ in range(2)]
                    Ys = [[sb(ph, f"Ys{d}{p}", [128, 128], BF16) for p in range(2)] for d in range(2)]; b_Ys = [[nb(), nb()] for _ in range(2)]
                    Rs = [[sb(ph, f"Rs{d}{p}", [128, 128], BF16) for p in range(2)] for d in range(2)]; b_Rs = [[nb(), nb()] for _ in range(2)]
                    Yp = [sb(ph, f"Yp{d}", [128, 128]) for d in range(2)]; b_Yp = [nb(), nb()]
                    TT = [[sb(ph, f"TT{d}{p}", [128, 128], BF16) for p in range(2)] for d in range(2)]; b_TT = [[nb(), nb()] for _ in range(2)]
                    UV = [[[sb(ph, f"UV{d}{p}{j}", [128, 64], BF16) for j in range(2)] for p in range(2)] for d in range(2)]
                    b_UV = [[[nb(), nb()] for _ in range(2)] for _ in range(2)]
                    BKt = [[sb(ph, f"BKt{d}{p}", [128, 2, 64], BF16) for p in range(2)] for d in range(2)]; b_BKt = [[nb(), nb()] for _ in range(2)]
                    W0 = [sb(ph, f"W0{d}", [128, 64], BF16) for d in range(2)]; b_W0 = [nb(), nb()]
                    ST = [[sb(ph, f"ST{d}{p}", [128, 64], BF16) for p in range(2)] for d in range(2)]; b_ST = [[nb(), nb()] for _ in range(2)]
                    cvb = cbufs(ph)
                    for d in range(2):
                        S.op("dve", lambda e: e.memset(bank[d][0][:, 256:512], 0.0), writes=[b_PX[d]])
                        S.op("pool", lambda e: e.memset(ST[d][0][:], 0.0), writes=[b_ST[d][0]])
                        for p in range(2):
                            for j in range(2):
                                S.op("pool", lambda e: e.memset(UV[d][p][j][:], 0.0), writes=[b_UV[d][p][j]])

                    def prep(d, i):
                        c = order[d][i]; par = i % 2
                        ARc = AR[d][:, c, :]; BKc = BK[d][:, c, :]
                        for j in range(2):
                            p0 = 64 * j
                            S.op("pe", lambda e: e.matmul(PM[d][:, j, :], lhsT=BKc[p0:p0 + 64, :], rhs=ARc[p0:p0 + 64, :], start=True, stop=True), reads=[b_str], writes=[b_PM[d]], pos=(p0, 0))
                            S.op("pe", lambda e: e.matmul(PX[d][p0:p0 + 64, p0:p0 + 64], lhsT=BKc[p0:p0 + 64, 0:64], rhs=ARc[p0:p0 + 64, 0:64], start=True, stop=True), reads=[b_str], writes=[b_PX[d]], pos=(p0, p0))
                            S.op("pe", lambda e: e.matmul(PY[d][p0:p0 + 64, p0:p0 + 64], lhsT=ARc[p0:p0 + 64, 0:64], rhs=BKc[p0:p0 + 64, 0:64], start=True, stop=True), reads=[b_str], writes=[b_PY[d]], pos=(p0, p0))
                            S.op("pe", lambda e: e.transpose(out=PVt[d][64:128, j, :], in_=vTb[p0:p0 + 64, c * 64:(c + 1) * 64], identity=identb[p0:p0 + 64, p0:p0 + 64]), reads=[b_str, b_const], writes=[b_PVt[d]], pos=(p0, 64))
                            S.op("pe", lambda e: e.transpose(out=PBt[d][:, j, :], in_=BKc[p0:p0 + 64, :], identity=identb[p0:p0 + 64, p0:p0 + 64]), reads=[b_str, b_const], writes=[b_PBt[d]], pos=(p0, 0))
                        yield
                        S.op("dve", lambda e: e.tensor_tensor(out=MS[d][par][:], in0=PM[d], in1=msk[:, d, :].unsqueeze(1).to_broadcast([128, 2, 128]), op=ALU.mult), reads=[b_PM[d], b_msk], writes=[b_MS[d][par]])
                        S.op("dve", lambda e: e.tensor_tensor(out=Xs[d][0][:], in0=PX[d], in1=msk[:, 2 + d, :], op=ALU.mult), reads=[b_PX[d], b_msk], writes=[b_Xs[d][0]])
                        S.op("dve", lambda e: e.tensor_tensor(out=Ys[d][0][:], in0=PY[d], in1=msk[:, 4 + d, :], op=ALU.mult), reads=[b_PY[d], b_msk], writes=[b_Ys[d][0]])
                        S.op("pool", lambda e: e.tensor_tensor(out=Rs[d][0][:], in0=identb[:], in1=Xs[d][0][:], op=ALU.subtract), reads=[b_Xs[d][0], b_const], writes=[b_Rs[d][0]])
                        for j in range(2):
                            S.op("act", lambda e: e.activation(out=UV[d][par][j][64:128, :], in_=PVt[d][64:128, j, :], func=AF.Copy), reads=[b_PVt[d]], writes=[b_UV[d][par][j]])
                        S.op("act", lambda e: e.activation(out=BKt[d][par][:], in_=PBt[d], func=AF.Copy), reads=[b_PBt[d]], writes=[b_BKt[d][par]])
                        yield
                        xi = 0; ri = 0
                        for lvl in range(1, 7):
                            if lvl <= 4:
                                S.op("pe", lambda e: e.matmul(PN[d][0], lhsT=Ys[d][xi][:], rhs=Xs[d][xi][:], start=True, stop=True), reads=[b_Ys[d][xi], b_Xs[d][xi]], writes=[b_PN[d][0]])
                            if lvl <= 5:
                                S.op("pe", lambda e: e.matmul(PN[d][1], lhsT=Xs[d][xi][:], rhs=Ys[d][xi][:], start=True, stop=True), reads=[b_Ys[d][xi], b_Xs[d][xi]], writes=[b_PN[d][1]])
                            if lvl >= 2:
                                S.op("pe", lambda e: e.matmul(PN[d][2], lhsT=identb[:], rhs=Rs[d][ri][:], start=True, stop=False), reads=[b_const, b_Rs[d][ri]], writes=[b_PN[d][2]])
                                S.op("pe", lambda e: e.matmul(PN[d][2], lhsT=Ys[d][xi][:], rhs=Rs[d][ri][:], start=False, stop=True), reads=[b_Ys[d][xi], b_Rs[d][ri]], writes=[b_PN[d][2]])
                            yield
                            if lvl <= 4:
                                S.op("act", lambda e: e.activation(out=Xs[d][1 - xi][:], in_=PN[d][0], func=AF.Copy), reads=[b_PN[d][0]], writes=[b_Xs[d][1 - xi]])
                            if lvl <= 5:
                                S.op("act", lambda e: e.activation(out=Ys[d][1 - xi][:], in_=PN[d][1], func=AF.Copy), reads=[b_PN[d][1]], writes=[b_Ys[d][1 - xi]])
                            if lvl >= 2:
                                if lvl == 6:
                                    S.op("act", lambda e: e.activation(out=TT[d][par][:], in_=PN[d][2], func=AF.Copy), reads=[b_PN[d][2]], writes=[b_TT[d][par]])
                                else:
                                    S.op("act", lambda e: e.activation(out=Rs[d][1 - ri][:], in_=PN[d][2], func=AF.Copy), reads=[b_PN[d][2]], writes=[b_Rs[d][1 - ri]])
                                ri = 1 - ri
                            xi = 1 - xi
                            yield

                    def step(d, i):
                        c = order[d][i]; par = i % 2; cur = i % 2; nxt = 1 - cur
                        isx = c >= 4; xc = c - 4
                        ARc = AR[d][:, c, :]
                        for j in range(2):
                            p0 = 64 * j
                            S.op("pe", lambda e: e.matmul(PW[d][p0:p0 + 64, :], lhsT=ARc[p0:p0 + 64, 0:64], rhs=ST[d][cur][p0:p0 + 64, :], start=True, stop=False), reads=[b_str, b_ST[d][cur]], writes=[b_PW[d]], pos=(p0, p0))
                            S.op("pe", lambda e: e.matmul(PW[d][p0:p0 + 64, :], lhsT=MS[d][par][64:128, j, 0:64], rhs=UV[d][par][j][64:128, :], start=False, stop=True), reads=[b_MS[d][par], b_UV[d][par][j]], writes=[b_PW[d]], pos=(64, p0))
                        S.op("dve", lambda e: e.tensor_copy(out=W0[d][:], in_=PW[d]), reads=[b_PW[d]], writes=[b_W0[d]])
                        yield
                        for j in range(2):
                            p0 = 64 * j
                            S.op("pe", lambda e: e.matmul(PU[d][:, j, :], lhsT=TT[d][par][p0:p0 + 64, p0:p0 + 64], rhs=W0[d][p0:p0 + 64, :], start=True, stop=True), reads=[b_TT[d][par], b_W0[d]], writes=[b_PU[d]], pos=(p0, 0))
                        for j in range(2):
                            S.op("dve", lambda e: e.tensor_copy(out=UV[d][par][j][0:64, :], in_=PU[d][:, j, :]), reads=[b_PU[d]], writes=[b_UV[d][par][j]])
                        yield
                        if isx:
                            for j in range(2):
                                p0 = 64 * j
                                S.op("pe", lambda e: e.matmul(PO[d][:, j, :], lhsT=ARc[p0:p0 + 64, 64:128], rhs=ST[d][cur][p0:p0 + 64, :], start=True, stop=False), reads=[b_str, b_ST[d][cur]], writes=[b_PO[d]], pos=(p0, 0))
                                S.op("pe", lambda e: e.matmul(PO[d][:, j, :], lhsT=MS[d][par][:, j, 64:128], rhs=UV[d][par][j][:, :], start=False, stop=True), reads=[b_MS[d][par], b_UV[d][par][j]], writes=[b_PO[d]])
                            S.op("dve", lambda e: e.tensor_tensor(out=v3(Oacc[:, xc, :], 2), in0=PO[d], in1=v3(Oacc[:, xc, :], 2), op=ALU.add), reads=[b_PO[d], b_O], writes=[b_O])
                        for j in range(2):
                            p0 = 64 * j
                            S.op("pe", lambda e: e.matmul(PS_[d][p0:p0 + 64, :], lhsT=identb[p0:p0 + 64, p0:p0 + 64], rhs=ST[d][cur][p0:p0 + 64, :], start=True, stop=False), reads=[b_const, b_ST[d][cur]], writes=[b_PS[d]], pos=(p0, p0))
                            S.op("pe", lambda e: e.matmul(PS_[d][p0:p0 + 64, :], lhsT=BKt[d][par][:, j, :], rhs=UV[d][par][j][:, :], start=False, stop=True), reads=[b_BKt[d][par], b_UV[d][par][j]], writes=[b_PS[d]], pos=(0, p0))
                        S.op("dve", lambda e: e.tensor_scalar(out=ST[d][nxt][:], in0=PS_[d], scalar1=gam[d][:, c:c + 1], scalar2=None, op0=ALU.mult), reads=[b_PS[d], b_str], writes=[b_ST[d][nxt]])
                        yield

                    if os.environ.get("K_PHASE") == "prep":
                        ns = int(os.environ.get("K_NS", 99))
                        for g_ in (prep(0, 0), prep(1, 0)):
                            for _ in range(ns):
                                try:
                                    next(g_)
                                except StopIteration:
                                    break
                        S.barrier()
                        return
                    if os.environ.get("K_PHASE") != "noprep":
                        run([prep(0, 0), prep(1, 0)])
                    for i in range(int(os.environ.get("K_STEPS", NCH))):
                        gs = [step(0, i), step(1, i)]
                        if i + 1 < NCH:
                            gs += [prep(0, i + 1), prep(1, i + 1)]
                        run(gs)
                        conv_tiles(cvb, 2)
                    S.barrier()
                S.mark(f"rw{hp}_scan")
                with ExitStack() as ph:
                    sm = sb(ph, "sm", [64, 128]); sm2 = sb(ph, "sm2", [64, 128]); b_sm = Buf()
                    sqb = sb(ph, "sqb", [64, 64, 128]); b_sqb = Buf()
                    onT = sb(ph, "onT", [128, 4096]); b_onT = Buf()
                    pR = [ps(ph, f"pR{i}", [128, 512]) for i in range(2)]; b_pR = [PBuf(), PBuf()]
                    O3 = Oacc[:].rearrange("p a (j v) -> p (a j) v", j=2)
                    S.op("dve", lambda e: e.tensor_reduce(out=sm[:], in_=O3, axis=AX.X, op=ALU.add), reads=[b_O], writes=[b_sm])
                    S.op("pool", lambda e: e.tensor_tensor(out=sqb[:], in0=Oacc[:], in1=Oacc[:], op=ALU.mult), reads=[b_O], writes=[b_sqb])
                    S.op("dve", lambda e: e.tensor_reduce(out=sm2[:], in_=sqb[:].rearrange("p a (j v) -> p (a j) v", j=2), axis=AX.X, op=ALU.add), reads=[b_sqb], writes=[b_sm])
                    S.op("dve", lambda e: e.tensor_scalar(out=sm[:], in0=sm[:], scalar1=1.0 / 64, scalar2=None, op0=ALU.mult), reads=[b_sm], writes=[b_sm])
                    S.op("dve", lambda e: e.scalar_tensor_tensor(out=sm2[:], in0=sm2[:], scalar=1.0 / 64, in1=sm2[:], op0=ALU.mult, op1=ALU.bypass), reads=[b_sm], writes=[b_sm])
                    mu2 = sb(ph, "mu2", [64, 128])
                    S.op("dve", lambda e: e.tensor_tensor(out=mu2[:], in0=sm[:], in1=sm[:], op=ALU.mult), reads=[b_sm], writes=[b_sm])
                    S.op("dve", lambda e: e.tensor_tensor(out=sm2[:], in0=sm2[:], in1=mu2[:], op=ALU.subtract), reads=[b_sm], writes=[b_sm])
                    S.op("act", lambda e: e.activation(out=sm2[:], in_=sm2[:], func=AF.Sqrt, bias=64e-5), reads=[b_sm], writes=[b_sm])
                    S.op("dve", lambda e: e.reciprocal(out=sm2[:], in_=sm2[:]), reads=[b_sm], writes=[b_sm])
                    S.op("dve", lambda e: e.tensor_tensor(out=O3, in0=O3, in1=sm[:].unsqueeze(2).to_broadcast([64, 128, 64]), op=ALU.subtract), reads=[b_O, b_sm], writes=[b_O])
                    S.op("dve", lambda e: e.tensor_tensor(out=O3, in0=O3, in1=sm2[:].unsqueeze(2).to_broadcast([64, 128, 64]), op=ALU.mult), reads=[b_O, b_sm], writes=[b_O])
                    for g8 in range(8):
                        pi = g8 % 2
                        for q in range(8):
                            xc = g8 * 8 + q
                            S.op("pe", lambda e: e.transpose(out=pR[pi][:, q * 64:(q + 1) * 64], in_=Oacc[:, xc, :], identity=identf[0:64, 0:64]), reads=[b_O, b_const], writes=[b_pR[pi]])
                        S.op("act", lambda e: e.activation(out=onT[:, g8 * 512:(g8 + 1) * 512], in_=pR[pi][:, :], func=AF.Copy), reads=[b_pR[pi]], writes=[b_onT])
                    S.op("dve", lambda e: e.tensor_scalar(out=onT[:], in0=onT[:], scalar1=prm[:, 7:8], scalar2=prm[:, 8:9], op0=ALU.mult, op1=ALU.add), reads=[b_onT, b_prm], writes=[b_onT])
                    S.op("pool", lambda e: e.tensor_tensor(out=onT[:], in0=onT[:], in1=bonT[:], op=ALU.add), reads=[b_onT, b_bon], writes=[b_onT])
                    S.op("dve", lambda e: e.tensor_tensor(out=onT[:], in0=onT[:], in1=ggT[:], op=ALU.mult), reads=[b_onT, b_bon], writes=[b_onT])
                    for p in range(8):
                        S.dma(nq(), ag_in_rw[p][hp * 128:(hp + 1) * 128, :], onT[:, 512 * p:512 * (p + 1)], reads=[b_onT], writes=[b_agin])
                    S.barrier()

        b_agin_h = [Buf(), Buf()]
        b_agout_rw = Buf(); b_agout_h = [Buf(), Buf()]

        def gather(ins_, outs_, b_in, b_out):
            S._wait("pool", S._deps([b_in], [b_out], "pool"))
            for p in range(8):
                nc.gpsimd.collective_compute("AllGather", ALU.bypass, replica_groups=[[0, 1, 2, 3], [4, 5, 6, 7]], ins=[ins_[p]], outs=[outs_[p]]).then_inc(S.sem["cc"], 1)
                S.cnt["cc"] += 1
            b_out.w = ("cc", S.cnt["cc"])
            b_out.r = {}

        for hp in range(0 if SIMTAIL else int(os.environ.get("K_HP", 2))):
            rwkv_hp(hp)
            S.mark(f"rw{hp}_readout")
        if not SIMTAIL:
            gather(ag_in_rw, ag_out_rw, b_agin, b_agout_rw)
        def hgrn_head(hh):
            with ExitStack() as php:
                QD = [sb(php, f"QD{d}", [128, T], BF16) for d in range(2)]
                KD = [sb(php, f"KD{d}", [128, T], BF16) for d in range(2)]
                QG = [sb(php, f"QG{d}", [128, T], BF16) for d in range(2)]
                KL = [sb(php, f"KL{d}", [128, T], BF16) for d in range(2)]
                iTb = sb(php, "iTb", [128, T], BF16)
                gmh = [sb(php, f"gmh{d}", [128, NCH]) for d in range(2)]
                sog = sb(php, "sog", [128, 4096])
                Oh = sb(php, "Oh", [64, 64, 128])
                hpr = sb(php, "hpr", [128, 12])
                b_str = Buf(); b_O = Buf(); b_hp = Buf(); b_sog = Buf()
                S.dma("sp", hpr[:, 0:4], hlb[hh], writes=[b_hp])
                S.dma("act", hpr[:, 4:5], hgn, writes=[b_hp])
                S.op("dve", lambda e: e.tensor_tensor(out=hpr[:, 5:7], in0=hpr[:, 0:2], in1=hpr[:, 2:4], op=ALU.subtract), reads=[b_hp], writes=[b_hp])
                S.op("act", lambda e: e.activation(out=hpr[:, 5:7], in_=hpr[:, 5:7], func=AF.Sigmoid), reads=[b_hp], writes=[b_hp])
                S.op("dve", lambda e: e.tensor_scalar(out=hpr[:, 7:9], in0=hpr[:, 5:7], scalar1=-1.0, scalar2=1.0, op0=ALU.mult, op1=ALU.add), reads=[b_hp], writes=[b_hp])
                S.op("pool", lambda e: e.memset(Oh[:], 0.0), writes=[b_O])
                with ExitStack() as ph:
                    TB = 256
                    tl = {}

                    cur_par = [0]

                    def tt(name, p=128, dt=F32):
                        key = (name, cur_par[0])
                        if key not in tl:
                            tl[key] = (sb(ph, "h_" + name, [p, TB], dt), Buf())
                        return tl[key]
                    def blkgen(blk):
                        t0 = blk * TB
                        c0 = blk * 4
                        cur_par[0] = blk % 2
                        for s_i, n_ in enumerate(["q", "ff", "fb", "i", "og"]):
                            t_, b_ = tt(n_)
                            rr = 10 + 5 * hh + s_i
                            S.dma("sp", t_[:], zT[rr * 128:(rr + 1) * 128, t0:t0 + TB], reads=[b_zT], writes=[b_])
                        (q_, b_q), (i_, b_i), (og_, b_og) = tt("q"), tt("i"), tt("og")
                        yield
                        cur_par[0] = blk % 2
                        qs, b_qs = tt("qs")
                        S.op("act", lambda e: e.activation(out=qs[:], in_=q_[:], func=AF.Silu), reads=[b_q], writes=[b_qs])
                        if blk >= 1:
                            S.op("act", lambda e: e.activation(out=sog[:, t0 - 256:t0 - 256 + TB], in_=og_[:], func=AF.Silu), reads=[b_og], writes=[b_sog])
                        S.op("pool", lambda e: e.tensor_copy(out=iTb[:, t0:t0 + TB], in_=i_[:]), reads=[b_i], writes=[b_str])
                        for d in range(2):
                            yield
                            cur_par[0] = blk % 2
                            f_, b_f = tt("ff" if d == 0 else "fb")
                            sgf, b_sgf = tt("sgf"); fg, b_fg = tt("fg"); lf, b_lf = tt("lf"); kk_, b_kk = tt("kk_")
                            cs, b_cs = tt("cs"); ce, b_ce = tt("ce"); ci2, b_ci2 = tt("ci2")
                            S.op("act", lambda e: e.activation(out=sgf[:], in_=f_[:], func=AF.Sigmoid), reads=[b_f], writes=[b_sgf])
                            S.op("dve", lambda e: e.tensor_scalar(out=fg[:], in0=sgf[:], scalar1=hpr[:, 7 + d:8 + d], scalar2=hpr[:, 5 + d:6 + d], op0=ALU.mult, op1=ALU.add), reads=[b_sgf, b_hp], writes=[b_fg])
                            S.op("act", lambda e: e.activation(out=lf[:], in_=fg[:], func=AF.Ln), reads=[b_fg], writes=[b_lf])
                            S.op("pool", lambda e: e.tensor_scalar(out=kk_[:], in0=fg[:], scalar1=-1.0, scalar2=1.0, op0=ALU.mult, op1=ALU.add), reads=[b_fg], writes=[b_kk])
                            for c in range(4):
                                S.op("dve", lambda e: e.tensor_tensor_scan(out=cs[:, c * 64:(c + 1) * 64], data0=lf[:, c * 64:(c + 1) * 64], data1=lf[:, c * 64:(c + 1) * 64], initial=0.0, op0=ALU.add, op1=ALU.bypass), reads=[b_lf], writes=[b_cs])
                            if d == 0:
                                ci, b_ci = cs, b_cs
                                iref, ilast = 32, 63
                            else:
                                S.op("dve", lambda e: e.tensor_tensor(out=v3(ce[:, :], 4), in0=v3(cs[:, :], 4)[:, :, 63:64].to_broadcast([128, 4, 64]), in1=v3(cs[:, :], 4), op=ALU.subtract), reads=[b_cs], writes=[b_ce])
                                S.op("pool", lambda e: e.tensor_tensor(out=ci2[:], in0=ce[:], in1=lf[:], op=ALU.add), reads=[b_ce, b_lf], writes=[b_ci2])
                                ci, b_ci = ci2, b_ci2
                                iref, ilast = 31, 0
                            dr, b_dr = tt("dr"); dl, b_dl = tt("dl")
                            E1, b_E1 = tt("E1"); E2, b_E2 = tt("E2"); E3, b_E3 = tt("E3"); E4, b_E4 = tt("E4")
                            S.op("dve", lambda e: e.tensor_tensor(out=v3(dr[:, :], 4), in0=v3(ci[:, :], 4), in1=v3(ci[:, :], 4)[:, :, iref:iref + 1].to_broadcast([128, 4, 64]), op=ALU.subtract), reads=[b_ci], writes=[b_dr])
                            S.op("dve", lambda e: e.tensor_tensor(out=v3(dl[:, :], 4), in0=v3(ci[:, :], 4), in1=v3(ci[:, :], 4)[:, :, ilast:ilast + 1].to_broadcast([128, 4, 64]), op=ALU.subtract), reads=[b_ci], writes=[b_dl])
                            S.op("act", lambda e: e.activation(out=E1[:], in_=dr[:], func=AF.Exp), reads=[b_dr], writes=[b_E1])
                            S.op("act", lambda e: e.activation(out=E2[:], in_=dr[:], func=AF.Exp, scale=-1.0), reads=[b_dr], writes=[b_E2])
                            S.op("act", lambda e: e.activation(out=E3[:], in_=ci[:], func=AF.Exp), reads=[b_ci], writes=[b_E3])
                            S.op("act", lambda e: e.activation(out=E4[:], in_=dl[:], func=AF.Exp, scale=-1.0), reads=[b_dl], writes=[b_E4])
                            S.op("act", lambda e: e.activation(out=gmh[d][:, c0:c0 + 4], in_=v3(ci[:, :], 4)[:, :, ilast], func=AF.Exp), reads=[b_ci], writes=[b_str])
                            S.op("dve", lambda e: e.tensor_tensor(out=QD[d][:, t0:t0 + TB], in0=qs[:], in1=E1[:], op=ALU.mult), reads=[b_qs, b_E1], writes=[b_str])
                            S.op("pool", lambda e: e.tensor_tensor(out=KD[d][:, t0:t0 + TB], in0=kk_[:], in1=E2[:], op=ALU.mult), reads=[b_kk, b_E2], writes=[b_str])
                            S.op("dve", lambda e: e.tensor_tensor(out=QG[d][:, t0:t0 + TB], in0=qs[:], in1=E3[:], op=ALU.mult), reads=[b_qs, b_E3], writes=[b_str])
                            S.op("pool", lambda e: e.tensor_tensor(out=KL[d][:, t0:t0 + TB], in0=kk_[:], in1=E4[:], op=ALU.mult), reads=[b_kk, b_E4], writes=[b_str])
                    for b0 in range(0, T // TB, 2):
                        run([blkgen(b_) for b_ in range(b0, min(b0 + 2, T // TB))])
                    S.barrier()
                with ExitStack() as ph:
                    bA = [ps(ph, f"hbA{d}", [128, 512]) for d in range(2)]
                    bB = [ps(ph, f"hbB{d}", [128, 512]) for d in range(2)]
                    bC = [ps(ph, f"hbC{d}", [128, 1024], BF16) for d in range(2)]
                    b_bA = [PBuf(), PBuf()]; b_bB = [PBuf(), PBuf()]; b_bC = [PBuf(), PBuf()]
                    PSc = [bA[d][0:64, 0:64] for d in range(2)]
                    PO = [bA[d][0:64, 64:192] for d in range(2)]
                    PSn = [bB[d][:, 0:128] for d in range(2)]
                    PVt = [bC[d][0:64, 0:128] for d in range(2)]
                    PKt = [bC[d][0:64, 128:256] for d in range(2)]
                    S32 = [sb(ph, f"S32{d}", [128, 128]) for d in range(2)]; b_S32 = [Buf(), Buf()]
                    Sb = [[sb(ph, f"Sb{d}{p}", [128, 128], BF16) for p in range(2)] for d in range(2)]; b_Sb = [[Buf(), Buf()] for _ in range(2)]
                    Msc = [[sb(ph, f"Msc{d}{p}", [64, 64], BF16) for p in range(2)] for d in range(2)]; b_Msc = [[Buf(), Buf()] for _ in range(2)]
                    Vt = [[sb(ph, f"Vt{d}{p}", [64, 128], BF16) for p in range(2)] for d in range(2)]; b_Vt = [[Buf(), Buf()] for _ in range(2)]
                    KLt = [[sb(ph, f"KLt{d}{p}", [64, 128], BF16) for p in range(2)] for d in range(2)]; b_KLt = [[Buf(), Buf()] for _ in range(2)]
                    cvb = cbufs(ph)
                    for d in range(2):
                        S.op("dve", lambda e: e.memset(S32[d][:], 0.0), writes=[b_S32[d]])
                        S.op("pool", lambda e: e.memset(Sb[d][0][:], 0.0), writes=[b_Sb[d][0]])

                    def prep(d, i):
                        c = order[d][i]; par = i % 2
                        sl = slice(c * 64, (c + 1) * 64)
                        S.op("pe", lambda e: e.matmul(PSc[d], lhsT=KD[d][:, sl], rhs=QD[d][:, sl], start=True, stop=True), reads=[b_str], writes=[b_bA[d]])
                        S.op("pe", lambda e: e.transpose(out=PVt[d], in_=iTb[:, sl], identity=identb[:]), reads=[b_str, b_const], writes=[b_bC[d]])
                        S.op("pe", lambda e: e.transpose(out=PKt[d], in_=KL[d][:, sl], identity=identb[:]), reads=[b_str, b_const], writes=[b_bC[d]])
                        yield
                        S.op("dve", lambda e: e.tensor_tensor(out=Msc[d][par][:], in0=PSc[d], in1=msk[0:64, 7 + d, 0:64], op=ALU.mult), reads=[b_bA[d], b_msk], writes=[b_Msc[d][par]])
                        S.op("act", lambda e: e.activation(out=Vt[d][par][:], in_=PVt[d], func=AF.Copy), reads=[b_bC[d]], writes=[b_Vt[d][par]])
                        S.op("act", lambda e: e.activation(out=KLt[d][par][:], in_=PKt[d], func=AF.Copy), reads=[b_bC[d]], writes=[b_KLt[d][par]])
                        yield

                    def step(d, i):
                        c = order[d][i]; par = i % 2; cur = i % 2; nxt = 1 - cur
                        sl = slice(c * 64, (c + 1) * 64)
                        isx = c >= 4; xc = c - 4
                        if isx:
                            S.op("pe", lambda e: e.matmul(PO[d], lhsT=Msc[d][par][:], rhs=Vt[d][par][:], start=True, stop=False), reads=[b_Msc[d][par], b_Vt[d][par]], writes=[b_bA[d]])
                            S.op("pe", lambda e: e.matmul(PO[d], lhsT=QG[d][:, sl], rhs=Sb[d][cur][:], start=False, stop=True), reads=[b_str, b_Sb[d][cur]], writes=[b_bA[d]])
                            S.op("dve", lambda e: e.tensor_tensor(out=Oh[:, xc, :], in0=PO[d], in1=Oh[:, xc, :], op=ALU.add), reads=[b_bA[d], b_O], writes=[b_O])
                        S.op("pe", lambda e: e.matmul(PSn[d], lhsT=KLt[d][par][:], rhs=Vt[d][par][:], start=True, stop=True), reads=[b_KLt[d][par], b_Vt[d][par]], writes=[b_bB[d]])
                        yield
                        S.op("dve", lambda e: e.scalar_tensor_tensor(out=S32[d][:], in0=S32[d][:], scalar=gmh[d][:, c:c + 1], in1=PSn[d], op0=ALU.mult, op1=ALU.add), reads=[b_S32[d], b_str, b_bB[d]], writes=[b_S32[d]])
                        S.op("act", lambda e: e.activation(out=Sb[d][nxt][:], in_=S32[d][:], func=AF.Copy), reads=[b_S32[d]], writes=[b_Sb[d][nxt]])
                        yield

                    run([prep(0, 0), prep(1, 0)])
                    for i in range(NCH):
                        gs = [step(0, i), step(1, i)]
                        if i + 1 < NCH:
                            gs += [prep(0, i + 1), prep(1, i + 1)]
                        run(gs)
                        conv_tiles(cvb, 1)
                    S.barrier()
                with ExitStack() as ph:
                    sqb = sb(ph, "hsq", [64, 64, 128]); b_sqb = Buf()
                    ssm = sb(ph, "hss", [64, 64]); b_ss = Buf()
                    ohT = sb(ph, "ohT", [128, 4096]); b_ohT = Buf()
                    pR = [ps(ph, f"hpR{i}", [128, 512]) for i in range(2)]; b_pR = [PBuf(), PBuf()]
                    S.op("pool", lambda e: e.tensor_tensor(out=sqb[:], in0=Oh[:], in1=Oh[:], op=ALU.mult), reads=[b_O], writes=[b_sqb])
                    S.op("dve", lambda e: e.tensor_reduce(out=ssm[:], in_=sqb[:], axis=AX.X, op=ALU.add), reads=[b_sqb], writes=[b_ss])
                    S.op("act", lambda e: e.activation(out=ssm[:], in_=ssm[:], func=AF.Sqrt, scale=1.0 / 128, bias=EPS), reads=[b_ss], writes=[b_ss])
                    S.op("dve", lambda e: e.reciprocal(out=ssm[:], in_=ssm[:]), reads=[b_ss], writes=[b_ss])
                    S.op("dve", lambda e: e.tensor_tensor(out=Oh[:], in0=Oh[:], in1=ssm[:].unsqueeze(2).to_broadcast([64, 64, 128]), op=ALU.mult), reads=[b_O, b_ss], writes=[b_O])
                    for g8 in range(8):
                        pi = g8 % 2
                        for q in range(8):
                            xc = g8 * 8 + q
                            S.op("pe", lambda e: e.transpose(out=pR[pi][:, q * 64:(q + 1) * 64], in_=Oh[:, xc, :], identity=identf[0:64, 0:64]), reads=[b_O, b_const], writes=[b_pR[pi]])
                        S.op("act", lambda e: e.activation(out=ohT[:, g8 * 512:(g8 + 1) * 512], in_=pR[pi][:, :], func=AF.Copy), reads=[b_pR[pi]], writes=[b_ohT])
                    S.op("dve", lambda e: e.scalar_tensor_tensor(out=ohT[:], in0=ohT[:], scalar=hpr[:, 4:5], in1=sog[:], op0=ALU.mult, op1=ALU.mult), reads=[b_ohT, b_hp, b_sog], writes=[b_ohT])
                    for p in range(8):
                        S.dma(nq(), ag_in_h[hh][p][:, :], ohT[:, 512 * p:512 * (p + 1)], reads=[b_ohT], writes=[b_agin_h[hh]])
                    S.barrier()

        for hh in range(0 if SIMTAIL else 2):
            hgrn_head(hh)
            gather(ag_in_h[hh], ag_out_h[hh], b_agin_h[hh], b_agout_h[hh])
            S.mark(f"hg{hh}_all")
        if SIMTAIL:
            for p in range(8):
                for r in range(4):
                    S.dma("sp", ag_out_rw[p][256 * r:256 * r + 256, :], ag_ref[512 * r:512 * r + 256, 512 * p:512 * (p + 1)], writes=[b_agout_rw])
                    for hh in range(2):
                        S.dma("sp", ag_out_h[hh][p][128 * r:128 * r + 128, :], ag_ref[512 * r + 256 + 128 * hh:512 * r + 384 + 128 * hh, 512 * p:512 * (p + 1)], writes=[b_agout_h[hh]])

        b_x1D = Buf(); b_hx2D = Buf(); b_qD = Buf(); b_out = Buf()
        with ExitStack() as pht:
            mT = sb(pht, "mT", [128, 16, OWN], BF16); b_mT = Buf()
            with ExitStack() as ph:
                yTb = sb(ph, "yTb", [128, 16, OWN], BF16); b_yT = Buf()
                sel = sb(ph, "sel", [128, 4]); b_sel = Buf()
                S.dma("sp", sel[:], selq, writes=[b_sel])
                ld = [sb(ph, f"yl{i}", [128, 4096]) for i in range(2)]; b_ld = [Buf(), Buf()]
                ya = [sb(ph, f"ya{i}", [128, OWN]) for i in range(2)]; b_ya = [Buf(), Buf()]
                for kc in range(16):
                    i = kc % 2
                    kk_ = kc % 8
                    for p in range(8):
                        if kc < 8:
                            src_, bsrc = ag_out_rw[p][256 * (kk_ // 2) + 128 * (kk_ % 2):256 * (kk_ // 2) + 128 * (kk_ % 2) + 128, :], b_agout_rw
                        else:
                            src_, bsrc = ag_out_h[kk_ % 2][p][128 * (kk_ // 2):128 * (kk_ // 2) + 128, :], b_agout_h[kk_ % 2]
                        S.dma(nq(), ld[i][:, 512 * p:512 * (p + 1)], src_, reads=[bsrc], writes=[b_ld[i]])
                    S.op("dve", lambda e: e.tensor_scalar(out=ya[i][:], in0=ld[i][:, 0:OWN], scalar1=sel[:, 0:1], scalar2=None, op0=ALU.mult), reads=[b_ld[i], b_sel], writes=[b_ya[i]])
                    for q in range(1, 4):
                        o_ = yTb[:, kc, :] if q == 3 else ya[i][:]
                        S.op("dve", lambda e: e.scalar_tensor_tensor(out=o_, in0=ld[i][:, q * OWN:(q + 1) * OWN], scalar=sel[:, q:q + 1], in1=ya[i][:], op0=ALU.mult, op1=ALU.add), reads=[b_ld[i], b_sel, b_ya[i]], writes=[b_yT] if q == 3 else [b_ya[i]])
                wuf = [sb(ph, f"wuf{i}", [128, 16, 128]) for i in range(2)]; b_wuf = [Buf(), Buf()]
                wub = [sb(ph, f"wub{i}", [128, 16, 128], BF16) for i in range(2)]; b_wub = [Buf(), Buf()]
                gl = [[sb(ph, f"gl{i}{br}", [128, 512]) for br in range(2)] for i in range(2)]; b_gl = [[Buf(), Buf()] for _ in range(2)]
                pU = [[ps(ph, f"pU{i}{br}", [128, 512]) for br in range(2)] for i in range(2)]; b_pU = [[PBuf(), PBuf()] for _ in range(2)]
                it = 0
                for j in range(16):
                    i = j % 2
                    S.dma(nq(), wuf[i][:], wup[j], writes=[b_wuf[i]])
                    S.op("pool", lambda e: e.tensor_copy(out=wub[i][:], in_=wuf[i][:]), reads=[b_wuf[i]], writes=[b_wub[i]])
                    for tb in range(2):
                        t0 = tb * 512
                        pi = it % 2
                        it += 1
                        for br in range(2):
                            for kc in range(8):
                                S.op("pe", lambda e: e.matmul(pU[pi][br][:, :], lhsT=wub[i][:, 8 * br + kc, :], rhs=yTb[:, 8 * br + kc, t0:t0 + 512], start=(kc == 0), stop=(kc == 7)), reads=[b_wub[i], b_yT], writes=[b_pU[pi][br]])
                            S.dma(nq(), gl[pi][br][:], gT[(16 * br + j) * 128:(16 * br + j + 1) * 128, t0:t0 + 512], reads=[b_gT], writes=[b_gl[pi][br]])
                            S.op("act", lambda e: e.activation(out=gl[pi][br][:], in_=gl[pi][br][:], func=AF.Sigmoid), reads=[b_gl[pi][br]], writes=[b_gl[pi][br]])
                            S.op("dve", lambda e: e.tensor_tensor(out=gl[pi][br][:], in0=pU[pi][br][:, :], in1=gl[pi][br][:], op=ALU.mult), reads=[b_pU[pi][br], b_gl[pi][br]], writes=[b_gl[pi][br]])
                        S.op("pool", lambda e: e.tensor_tensor(out=mT[:, j, t0:t0 + 512], in0=gl[pi][0][:], in1=gl[pi][1][:], op=ALU.add), reads=[b_gl[pi][0], b_gl[pi][1]], writes=[b_mT])
                S.barrier()
            S.mark("tailA1_up_merge")
            hx2T = sb(pht, "hx2T", [128, 16, OWN], BF16)
            with ExitStack() as ph:
                wof = sb(ph, "wof", [128, 16, 512]); b_wof = Buf()
                wob = [sb(ph, f"wob{n}", [128, 16, 512], BF16) for n in range(4)]; b_wob = Buf()
                for n in range(4):
                    S.dma(nq(), wof[:], wo[n], writes=[b_wof])
                    S.op("pool" if n % 2 else "dve", lambda e: e.tensor_copy(out=wob[n][:], in_=wof[:]), reads=[b_wof], writes=[b_wob])
                gmb = sb(ph, "gmb", [128, D]); b_gmb = Buf()
                bc_load("sp", gmb[:], modD[0:1, 2 * D:3 * D], [b_gmb], reads=[b_modD])
                xt = [sb(ph, f"xt2{i}", [128, D]) for i in range(2)]; b_xt = [Buf(), Buf()]
                o1 = [sb(ph, f"o1{i}", [128, D]) for i in range(2)]; b_o1 = [Buf(), Buf()]
                pO = [ps(ph, f"pO{n}", [128, 512]) for n in range(4)]; b_pO = [PBuf() for _ in range(4)]
                for tl in range(8):
                    i = tl % 2
                    S.dma(nq(), xt[i][:], xown[tl * 128:(tl + 1) * 128, :], writes=[b_xt[i]])
                    for n in range(4):
                        for k in range(16):
                            S.op("pe", lambda e: e.matmul(pO[n][:, :], lhsT=mT[:, k, tl * 128:(tl + 1) * 128], rhs=wob[n][:, k, :], start=(k == 0), stop=(k == 15)), reads=[b_mT, b_wob], writes=[b_pO[n]])
                        S.op("dve", lambda e: e.tensor_tensor(out=o1[i][:, n * 512:(n + 1) * 512], in0=pO[n][:, :], in1=gmb[:, n * 512:(n + 1) * 512], op=ALU.mult), reads=[b_pO[n], b_gmb], writes=[b_o1[i]])
                    S.op("pool", lambda e: e.tensor_tensor(out=o1[i][:], in0=o1[i][:], in1=xt[i][:], op=ALU.add), reads=[b_o1[i], b_xt[i]], writes=[b_o1[i]])
                    S.dma(nq(), x1D[tl * 128:(tl + 1) * 128, :], o1[i][:], reads=[b_o1[i]], writes=[b_x1D])
                S.barrier()
            with ExitStack() as ph:
                A2, B2, bA2, bB2 = make_AB(ph, 0, 1, 4 * D, 3 * D, "f")
                b_hx2T = norm_tiles(ph, x1D, 8, lambda tl: (A2, B2, bA2, bB2), hx2T, "f", src_buf=b_x1D, store=(hx2D, b_hx2D))
                S.barrier()
            with ExitStack() as ph:
                project(ph, wq, 16, hx2T, b_hx2T, OWN, qD, b_qD, "q")
                S.barrier()
        S.mark("tailA2B_out_norm_q")
        with ExitStack() as ph:
            cvb = cbufs(ph)
            conv_tiles(cvb, 512)
            S.barrier()
        with ExitStack() as ph:
            kT = sb(ph, "kT", [128, 2, 128]); b_kT = Buf()
            S.dma("sp", kT[:, 0, :], k1T, writes=[b_kT])
            S.dma("act", kT[:, 1, :], k2T, writes=[b_kT])
            gfb = sb(ph, "gfb", [128, D]); nfb = sb(ph, "nfb", [128, D]); b_cb = Buf()
            bc_load("sp", gfb[:], modD[0:1, 5 * D:6 * D], [b_cb], reads=[b_modD])
            bc_load("act", nfb[:], nrm[2:3, :], [b_cb])
            qt = sb(ph, "qt", [128, 16, 128]); b_qt = Buf()
            sc = sb(ph, "sc", [128, 16, 128]); b_sc = Buf()
            sc2 = sb(ph, "sc2", [128, 256]); b_sc2 = Buf()
            v16 = sb(ph, "v16", [128, 16, 16]); i16 = sb(ph, "i16", [128, 16, 16]); iu = sb(ph, "iu", [128, 16], U32); b_v16 = Buf(); b_iu = Buf()
            cand = sb(ph, "cand", [128, 8, 256]); ecand = sb(ph, "ecand", [128, 8, 256]); b_cand = Buf(); b_ecand = Buf()
            best = sb(ph, "best", [128, 8, 16]); b_best = Buf()
            eid = sb(ph, "eid", [128, 128]); eidi = sb(ph, "eidi", [128, 128], I32); b_eid = Buf()
            gate = sb(ph, "gate", [128, 8, 16]); gs = sb(ph, "gs", [128, 8]); b_gate = Buf()
            dots = sb(ph, "dots", [128, 128]); coef = sb(ph, "coef", [128, 128]); b_dots = Buf(); b_coef = Buf()
            b_dsl = [Buf() for _ in range(128)]; b_csl = [Buf() for _ in range(128)]
            hx2 = sb(ph, "hx2", [128, D]); b_hx2 = Buf()
            hx2b = sb(ph, "hx2b", [128, D], BF16); b_hx2b = Buf()
            x1t = sb(ph, "x1t", [128, D]); b_x1t = Buf()
            acc = sb(ph, "acc", [128, D]); b_acc = Buf()
            junk = sb(ph, "junk", [128, D], BF16); b_junk = Buf()
            junk2 = [sb(ph, f"junkd{i}", [128, D], BF16) for i in range(2)]; b_junk2 = [Buf(), Buf()]
            NB = 8
            gb = [sb(ph, f"gb{i}", [128, 2 * D], BF16) for i in range(NB)]; b_gb = [Buf() for _ in range(NB)]
            dg = [sb(ph, f"dg{i}", [128, 128], BF16) for i in range(8)]; b_dg = [Buf() for _ in range(8)]
            pacc = [ps(ph, f"pacc{n}", [128, 512]) for n in range(4)]; b_pacc = [PBuf() for _ in range(4)]
            st2 = sb(ph, "st2", [128, 2]); b_st2 = Buf()
            psc = [ps(ph, f"psc{i}", [128, 512]) for i in range(2)]; b_psc = [PBuf(), PBuf()]
            gi = 0
            qDv = qD.rearrange("(j p) t -> p j t", p=128)
            for tl in range(OWN // 128):
                tsl = slice(tl * 128, (tl + 1) * 128)
                S.dma("sp", qt[:], qDv[:, :, tsl], reads=[b_qD], writes=[b_qt])
                S.dma("act", hx2[:], hx2D[tsl, :], reads=[b_hx2D], writes=[b_hx2])
                S.op("pool", lambda e: e.tensor_copy(out=hx2b[:], in_=hx2[:]), reads=[b_hx2], writes=[b_hx2b])
                S.dma("sp", x1t[:], x1D[tsl, :], reads=[b_x1D], writes=[b_x1t])
                for g4 in range(4):
                    pi = g4 % 2
                    for u in range(4):
                        hh = g4 * 4 + u
                        S.op("pe", lambda e: e.matmul(psc[pi][:, u * 128:(u + 1) * 128], lhsT=qt[:, hh, :], rhs=kT[:, hh % 2, :], start=True, stop=True), reads=[b_qt, b_kT], writes=[b_psc[pi]])
                    S.op("dve", lambda e: e.tensor_copy(out=sc[:, g4 * 4:(g4 + 1) * 4, :], in_=v3(psc[pi][:, :], 4)), reads=[b_psc[pi]], writes=[b_sc])
                for hh in range(16):
                    S.op("dve", lambda e: e.max(out=v16[:, hh, 0:8], in_=sc[:, hh, :]), reads=[b_sc], writes=[b_v16])
                    S.op("dve", lambda e: e.max_index(out=iu[:, 0:8], in_max=v16[:, hh, 0:8], in_values=sc[:, hh, :]), reads=[b_sc, b_v16], writes=[b_iu])
                    S.op("dve", lambda e: e.match_replace(out=sc2[:, 0:128], in_to_replace=v16[:, hh, 0:8], in_values=sc[:, hh, :], imm_value=-1e30), reads=[b_sc, b_v16], writes=[b_sc2])
                    S.op("dve", lambda e: e.max(out=v16[:, hh, 8:16], in_=sc2[:, 0:128]), reads=[b_sc2], writes=[b_v16])
                    S.op("dve", lambda e: e.max_index(out=iu[:, 8:16], in_max=v16[:, hh, 8:16], in_values=sc2[:, 0:128]), reads=[b_sc2, b_v16], writes=[b_iu])
                    S.op("dve", lambda e: e.tensor_copy(out=i16[:, hh, :], in_=iu[:, :]), reads=[b_iu], writes=[b_v16])
                for h in range(8):
                    S.op("dve", lambda e: e.tensor_tensor(out=cand[:, h, :].rearrange("p (a b) -> p a b", a=16), in0=v16[:, 2 * h, :].unsqueeze(2).to_broadcast([128, 16, 16]), in1=v16[:, 2 * h + 1, :].unsqueeze(1).to_broadcast([128, 16, 16]), op=ALU.add), reads=[b_v16], writes=[b_cand])
                    S.op("dve", lambda e: e.scalar_tensor_tensor(out=ecand[:, h, :].rearrange("p (a b) -> p a b", a=16), in0=i16[:, 2 * h, :].unsqueeze(2).to_broadcast([128, 16, 16]), scalar=128.0, in1=i16[:, 2 * h + 1, :].unsqueeze(1).to_broadcast([128, 16, 16]), op0=ALU.mult, op1=ALU.add), reads=[b_v16], writes=[b_ecand])
                    S.op("dve", lambda e: e.max(out=best[:, h, 0:8], in_=cand[:, h, :]), reads=[b_cand], writes=[b_best])
                    S.op("dve", lambda e: e.match_replace(out=sc2[:, :], in_to_replace=best[:, h, 0:8], in_values=cand[:, h, :], imm_value=-1e30), reads=[b_cand, b_best], writes=[b_sc2])
                    S.op("dve", lambda e: e.max(out=best[:, h, 8:16], in_=sc2[:, :]), reads=[b_sc2], writes=[b_best])
                S.op("dve", lambda e: e.memset(eid[:], 0.0), writes=[b_eid])
                for h in range(8):
                    for n in range(16):
                        S.op("dve", lambda e: e.scalar_tensor_tensor(out=sc2[:, :], in0=cand[:, h, :], scalar=best[:, h, n:n + 1], in1=ecand[:, h, :], op0=ALU.is_equal, op1=ALU.mult, accum_out=eid[:, h * 16 + n:h * 16 + n + 1]), reads=[b_cand, b_ecand, b_best], writes=[b_sc2, b_eid])
                S.op("dve", lambda e: e.tensor_scalar_min(out=eid[:], in0=eid[:], scalar1=16383.0), reads=[b_eid], writes=[b_eid])
                S.op("dve", lambda e: e.tensor_copy(out=eidi[:], in_=eid[:]), reads=[b_eid], writes=[b_eid])
                S.op("dve", lambda e: e.tensor_tensor(out=gate[:], in0=best[:], in1=best[:, :, 0:1].to_broadcast([128, 8, 16]), op=ALU.subtract), reads=[b_best], writes=[b_gate])
                S.op("act", lambda e: e.activation(out=gate[:], in_=gate[:], func=AF.Exp), reads=[b_gate], writes=[b_gate])
                S.op("dve", lambda e: e.tensor_reduce(out=gs[:], in_=gate[:], axis=AX.X, op=ALU.add), reads=[b_gate], writes=[b_gate])
                S.op("dve", lambda e: e.reciprocal(out=gs[:], in_=gs[:]), reads=[b_gate], writes=[b_gate])
                S.op("dve", lambda e: e.tensor_tensor(out=gate[:], in0=gate[:], in1=gs[:].unsqueeze(2).to_broadcast([128, 8, 16]), op=ALU.mult), reads=[b_gate], writes=[b_gate])
                S.op("pool", lambda e: e.memset(dots[:], 0.0), writes=b_dsl)
                gflat = gate[:].rearrange("p a b -> p (a b)")
                for s_ in range(128):
                    bi = gi % NB
                    gi += 1
                    dj = s_ % 8
                    S.idma(out=gb[bi][:], out_offset=None, in_=uvD, in_offset=bass.IndirectOffsetOnAxis(ap=eidi[:, s_:s_ + 1], axis=0), reads=[b_eid, b_tab], writes=[b_gb[bi]])
                    S.op("dve", lambda e: e.scalar_tensor_tensor(out=junk2[s_ % 2][:], in0=gb[bi][:, 0:D], scalar=1.0, in1=hx2b[:], op0=ALU.mult, op1=ALU.mult, accum_out=dots[:, s_:s_ + 1]), reads=[b_gb[bi], b_hx2b], writes=[b_junk2[s_ % 2], b_dsl[s_]])
                    S.op("act", lambda e: e.activation(out=coef[:, s_:s_ + 1], in_=dots[:, s_:s_ + 1], func=AF.Gelu), reads=[b_dsl[s_]], writes=[b_csl[s_]])
                    S.op("act", lambda e: e.activation(out=coef[:, s_:s_ + 1], in_=coef[:, s_:s_ + 1], func=AF.Copy, scale=gflat[:, s_:s_ + 1]), reads=[b_csl[s_], b_gate], writes=[b_csl[s_]])
                    S.op("act", lambda e: e.activation(out=dg[dj][:], in_=identf[:], func=AF.Copy, scale=coef[:, s_:s_ + 1]), reads=[b_const, b_csl[s_]], writes=[b_dg[dj]])
                    for n in range(4):
                        S.op("pe", lambda e: e.matmul(pacc[n][:, :], lhsT=dg[dj][:], rhs=gb[bi][:, D + n * 512:D + (n + 1) * 512], start=(s_ == 0), stop=(s_ == 127)), reads=[b_dg[dj], b_gb[bi]], writes=[b_pacc[n]])
                for n in range(4):
                    S.op("dve", lambda e: e.tensor_tensor(out=acc[:, n * 512:(n + 1) * 512], in0=pacc[n][:, :], in1=gfb[:, n * 512:(n + 1) * 512], op=ALU.mult), reads=[b_pacc[n], b_cb], writes=[b_acc])
                S.op("pool", lambda e: e.tensor_tensor(out=acc[:], in0=acc[:], in1=x1t[:], op=ALU.add), reads=[b_acc, b_x1t], writes=[b_acc])
                S.op("dve", lambda e: e.memset(st2[:], 0.0), writes=[b_st2])
                S.op("act", lambda e: e.activation(out=junk[:], in_=acc[:], func=AF.Square, accum_out=st2[:, 0:1]), reads=[b_acc], writes=[b_junk, b_st2])
                S.op("act", lambda e: e.activation(out=st2[:, 1:2], in_=st2[:, 0:1], func=AF.Sqrt, scale=1.0 / D, bias=EPS), reads=[b_st2], writes=[b_st2])
                S.op("dve", lambda e: e.reciprocal(out=st2[:, 1:2], in_=st2[:, 1:2]), reads=[b_st2], writes=[b_st2])
                S.op("dve", lambda e: e.scalar_tensor_tensor(out=acc[:], in0=acc[:], scalar=st2[:, 1:2], in1=nfb[:], op0=ALU.mult, op1=ALU.mult), reads=[b_acc, b_st2, b_cb], writes=[b_acc])
                S.dma("sp", out_d[tsl, :], acc[:], reads=[b_acc], writes=[b_out])
            S.barrier()
        S.mark("peer")
        S.finish()
    return nc


_CACHE = {}


def _prep(inputs):
    f = lambda a: np.ascontiguousarray(np.asarray(a, dtype=np.float32))
    x = f(inputs["x"]); ctx = f(inputs["ctx"]); c = f(inputs["c"]); c_ctx = f(inputs["c_ctx"])
    w_in = f(inputs["w_in"])[0]
    w_ada_all = np.ascontiguousarray(f(inputs["w_ada"])[0].reshape(16, 128, 24, 512).transpose(2, 1, 0, 3))
    b_ada_all = f(inputs["b_ada"]).reshape(1, -1)
    nrm = np.stack([f(inputs["norm_mix"])[0], f(inputs["norm_ffn"])[0], f(inputs["norm_final"])])
    rw_conv = f(inputs["rw_conv"])[0].reshape(9, -1)

    def arr_w(cols):
        w = w_in[:, cols]
        n = w.shape[1] // 128
        return np.ascontiguousarray(w.reshape(16, 128, n, 128).transpose(2, 1, 0, 3))

    s_ = np.arange(128)[:, None] % 64; t_ = np.arange(128)[None, :] % 64
    rb = np.arange(128)[:, None] // 64; cb = np.arange(128)[None, :] // 64
    cm = np.zeros((9, 128, 128), np.float32)
    cm[0] = np.where(cb == 0, s_ < t_, s_ <= t_)
    cm[1] = np.where(cb == 0, s_ > t_, s_ >= t_)
    cm[2] = -1.0 * ((rb == cb) & (s_ < t_))
    cm[3] = -1.0 * ((rb == cb) & (s_ > t_))
    cm[4] = -1.0 * ((rb == cb) & (t_ < s_))
    cm[5] = -1.0 * ((rb == cb) & (t_ > s_))
    cm[6] = (rb == cb)
    cm[7] = (s_ <= t_) & (rb == 0) & (cb == 0)
    cm[8] = (s_ >= t_) & (rb == 0) & (cb == 0)
    g_ = lambda n: f(inputs[n])[0]
    rw_w0, rw_a0 = g_("rw_w0"), g_("rw_a0")
    rw_kk, rw_ka, rw_rk = g_("rw_k_k"), g_("rw_k_a"), g_("rw_r_k").reshape(-1)
    rw_lnw, rw_lnb = g_("rw_ln_w"), g_("rw_ln_b")
    wlb_f = g_("rw_w_lora_b").reshape(128, 1024); alb_f = g_("rw_a_lora_b").reshape(128, 1024); glb_f = g_("rw_g_lora_b")
    maps = []
    for core in range(8):
        b, g = core // 4, core % 4
        own = 256 * g + np.arange(256)
        cols = []
        for hp in range(2):
            for base in (0, 1024, 2048):
                cols.append(base + own[hp * 128:(hp + 1) * 128])
        lora = 3072 + np.arange(416)
        cols.append(lora[0:128]); cols.append(lora[128:256]); cols.append(lora[256:384])
        rwcols = np.concatenate(cols + [lora[384:416]])
        hg = []
        for hh in range(2):
            for s in range(5):
                hg.append(3488 + s * 1024 + own[hh * 128:(hh + 1) * 128])
        wfull = np.zeros((2048, 20 * 128), np.float32)
        wfull[:, 0:9 * 128] = w_in[:, np.concatenate(cols)]
        wfull[:, 9 * 128:9 * 128 + 32] = w_in[:, lora[384:416]]
        wfull[:, 10 * 128:] = w_in[:, np.concatenate(hg)]
        wA = np.ascontiguousarray(wfull.reshape(16, 128, 20, 128).transpose(2, 1, 0, 3))
        convw = np.zeros((10, 128, 9), np.float32)
        cc = np.concatenate(cols)
        convw[0:9] = rw_conv[:, cc].T.reshape(9, 128, 9)
        convw[9, 0:32] = rw_conv[:, lora[384:416]].T
        m = {
            "seq": np.concatenate([ctx[b], x[b]], axis=0),
            "xown": x[b, 1024 * g:1024 * (g + 1)],
            "cmod": np.ascontiguousarray(np.stack([c[b], c_ctx], axis=-1).reshape(16, 128, 2).transpose(1, 0, 2)),
            "w_ada": w_ada_all[6 * g:6 * g + 6], "b_ada": np.ascontiguousarray(b_ada_all[:, 3072 * g:3072 * (g + 1)]), "nrm": nrm, "wA": wA,
            "wB": None, "convw": convw, "cmask": cm,
        }
        rwp = np.zeros((2, 128, 9), np.float32)
        for hp in range(2):
            ch = own[hp * 128:(hp + 1) * 128]
            rwp[hp] = np.stack([rw_w0[0, ch], rw_w0[1, ch], rw_a0[0, ch], rw_a0[1, ch], rw_kk[ch], rw_ka[ch], rw_rk[ch], rw_lnw[ch], rw_lnb[ch]], axis=1)
        m["rwp"] = rwp
        hl = f(inputs["hg_lb"])
        m["hlb"] = np.ascontiguousarray(np.stack([np.stack([hl[0, 0, own[hh * 128:(hh + 1) * 128]], hl[0, 1, own[hh * 128:(hh + 1) * 128]], hl[1, 0, own[hh * 128:(hh + 1) * 128]], hl[1, 1, own[hh * 128:(hh + 1) * 128]]], axis=1) for hh in range(2)]))
        m["hgn"] = np.ascontiguousarray(f(inputs["hg_norm"])[0].reshape(128, 1))
        m["wlb"] = np.ascontiguousarray(np.stack([wlb_f[:, own[hp * 128:(hp + 1) * 128]] for hp in range(2)]))
        m["alb"] = np.ascontiguousarray(np.stack([alb_f[:, own[hp * 128:(hp + 1) * 128]] for hp in range(2)]))
        m["glb"] = np.ascontiguousarray(np.stack([glb_f[:, own[hp * 128:(hp + 1) * 128]] for hp in range(2)]))
        maps.append(m)
    wB = arr_w(8608 + np.arange(4096))
    wupr = f(inputs["w_up_rw"])[0]; wuph = f(inputs["w_up_hg"])[0]
    wcat = np.concatenate([wupr, wuph], axis=0)
    wup = np.ascontiguousarray(wcat.reshape(16, 128, 16, 128).transpose(2, 1, 0, 3))
    wo = np.ascontiguousarray(f(inputs["w_out"])[0].reshape(16, 128, 4, 512).transpose(2, 1, 0, 3))
    wq = np.ascontiguousarray(f(inputs["peer_wq"])[0].reshape(16, 128, 16, 128).transpose(2, 1, 0, 3))
    k1T = np.ascontiguousarray(f(inputs["peer_k1"])[0].T); k2T = np.ascontiguousarray(f(inputs["peer_k2"])[0].T)
    pu = f(inputs["peer_u"])[0]; pv = f(inputs["peer_v"])[0]
    for core, m in enumerate(maps):
        m["wB"] = wB
        sq = np.zeros((128, 4), np.float32); sq[:, core % 4] = 1.0
        m.update(selq=sq, wup=wup, wo=wo, wq=wq, k1T=k1T, k2T=k2T, peer_u=pu, peer_v=pv)
    return maps


def kernel(**inputs):
    maps = _prep(inputs)
    nc = build_nc(STAGE)
    res = run_bass_kernel_spmd(nc, maps, core_ids=list(range(8)))
    _CACHE["res"] = res
    out = np.zeros((2, 4096, D), np.float32)
    for core in range(8):
        b, g = core // 4, core % 4
        out[b, 1024 * g:1024 * (g + 1)] = res.results[core]["out"]
    return out
```

```python
import os
import numpy as np
from contextlib import ExitStack
import concourse.bass as bass
import concourse.mybir as mybir
from concourse.bass_utils import run_bass_kernel_spmd

F32 = mybir.dt.float32
BF16 = mybir.dt.bfloat16
I32 = mybir.dt.int32
U32 = mybir.dt.uint32
AF = mybir.ActivationFunctionType
ALU = mybir.AluOpType
AX = mybir.AxisListType

D = 2048
T = 4352
NCH = 68
OWN = 1024
EPS = 1e-6
STAGE = 99
MARKS = []


class Buf:
    __slots__ = ("w", "r", "excl")

    def __init__(self, excl=False):
        self.w = None
        self.r = {}
        self.excl = excl


def PBuf():
    return Buf(True)


class Sch:
    def __init__(self, nc, es):
        self.nc = nc
        self.eng = {"pe": nc.tensor, "act": nc.scalar, "dve": nc.vector, "pool": nc.gpsimd, "sp": nc.sync}
        self.sem = {}
        self.cnt = {}
        self.NDS = 16
        names = ["pe", "act", "dve", "pool", "cc"] + [f"d_{q}_{i}" for q in ("sp", "act", "pool") for i in range(self.NDS)]
        for p in names:
            self.sem[p] = es.enter_context(nc.semaphore("s_" + p))
            self.cnt[p] = 0
        self.rr = {"sp": 0, "act": 0, "pool": 0}
        self.pe_pos = (0, 0)
        self.seen = {e: {} for e in self.eng}
        self.ninst = 0

    def _deps(self, reads, writes, e=None):
        deps = {}
        for b in reads:
            if b.w is not None and deps.get(b.w[0], 0) < b.w[1]:
                deps[b.w[0]] = b.w[1]
            if b.excl:
                for p, c in b.r.items():
                    if p != e and deps.get(p, 0) < c:
                        deps[p] = c
        for b in writes:
            if b.w is not None and deps.get(b.w[0], 0) < b.w[1]:
                deps[b.w[0]] = b.w[1]
            for p, c in b.r.items():
                if deps.get(p, 0) < c:
                    deps[p] = c
        return deps

    def _wait(self, e, deps):
        eng = self.eng[e]
        seen = self.seen[e]
        for p, c in deps.items():
            if p == "pe" and e == "pe":
                continue
            if seen.get(p, 0) >= c:
                continue
            eng.wait_ge(self.sem[p], c)
            seen[p] = c

    def _mark(self, prod, reads, writes):
        c = self.cnt[prod]
        for b in reads:
            if b.r.get(prod, 0) < c:
                b.r[prod] = c
        for b in writes:
            b.w = (prod, c)
            b.r = {}

    def op(self, e, fn, reads=(), writes=(), pos=(0, 0)):
        if e == "pe":
            if pos != self.pe_pos and self.cnt["pe"] > 0 and self.seen["pe"].get("pe_self", 0) < self.cnt["pe"]:
                self.eng["pe"].wait_ge(self.sem["pe"], self.cnt["pe"])
                self.seen["pe"]["pe_self"] = self.cnt["pe"]
            self.pe_pos = pos
        self._wait(e, self._deps(reads, writes, e))
        inst = fn(self.eng[e])
        self.cnt[e] += 1
        inst.then_inc(self.sem[e], 1)
        self._mark(e, reads, writes)
        self.ninst += 1
        return inst

    def _dsem(self, q):
        i = self.rr[q]
        self.rr[q] = (i + 1) % self.NDS
        prod = f"d_{q}_{i}"
        if self.cnt[prod] > 0:
            self._wait(q, {prod: self.cnt[prod]})
        return prod

    def dma(self, q, out, in_, reads=(), writes=(), **kw):
        prod = self._dsem(q)
        self._wait(q, self._deps(reads, writes))
        inst = self.eng[q].dma_start(out=out, in_=in_, **kw)
        self.cnt[prod] += 16
        inst.then_inc(self.sem[prod], 16)
        self._mark(prod, reads, writes)
        self.ninst += 1
        return inst

    def idma(self, reads=(), writes=(), **kw):
        prod = self._dsem("pool")
        self._wait("pool", self._deps(reads, writes))
        inst = self.eng["pool"].indirect_dma_start(**kw)
        self.cnt[prod] += 16
        inst.then_inc(self.sem[prod], 16)
        self._mark(prod, reads, writes)
        self.ninst += 1
        return inst

    def mark(self, name):
        MARKS.append((name, dict(self.cnt)))

    def barrier(self):
        deps = {p: c for p, c in self.cnt.items() if c > 0}
        for e in self.eng:
            self._wait(e, deps)

    def finish(self):
        self.barrier()


def build_nc(stage=99):
    nc = bass.Bass("TRN2", target_bir_lowering=False)

    def din(name, shape, dt=F32):
        return nc.dram_tensor(name, list(shape), dt, kind="ExternalInput").ap()

    seq = din("seq", [T, D])
    xown = din("xown", [OWN, D])
    cmod = din("cmod", [128, 16, 2])
    w_ada = din("w_ada", [6, 128, 16, 512])
    b_ada = din("b_ada", [1, 3072])
    mod_in = nc.dram_tensor("mod_in", [2, 3072], F32).ap()
    mod_out = nc.dram_tensor("mod_out", [8, 3072], F32).ap()
    nrm = din("nrm", [3, D])
    wA = din("wA", [20, 128, 16, 128])
    wB = din("wB", [32, 128, 16, 128])
    convw = din("convw", [10, 128, 9])
    cmask = din("cmask", [9, 128, 128])
    rwp = din("rwp", [2, 128, 9])
    wlb = din("wlb", [2, 128, 128])
    alb = din("alb", [2, 128, 128])
    glb = din("glb", [2, 160, 128])
    hlb = din("hlb", [2, 128, 4])
    hgn = din("hgn", [128, 1])
    selq = din("selq", [128, 4])
    wup = din("wup", [16, 128, 16, 128])
    wo = din("wo", [4, 128, 16, 512])
    wq = din("wq", [16, 128, 16, 128])
    k1T = din("k1T", [128, 128])
    k2T = din("k2T", [128, 128])
    peer_u = din("peer_u", [16384, D])
    peer_v = din("peer_v", [16384, D])
    SIMTAIL = bool(os.environ.get("K_SIMTAIL"))
    if SIMTAIL:
        ag_ref = din("ag_ref", [2048, 4096])
    out_d = nc.dram_tensor("out", [OWN, D], F32, kind="ExternalOutput").ap()
    dbg = nc.dram_tensor("dbg", [128, 8192], F32, kind="ExternalOutput").ap() if stage < 99 else None
    modD = nc.dram_tensor("modD", [2, 6 * D], F32).ap()
    zT = nc.dram_tensor("zT", [20 * 128, T], F32).ap()
    gT = nc.dram_tensor("gT", [32 * 128, OWN], F32).ap()
    ag_in_rw = [nc.dram_tensor(f"ag_in_rw{p}", [256, 512], F32).ap() for p in range(8)]
    ag_out_rw = [nc.dram_tensor(f"ag_out_rw{p}", [1024, 512], F32).ap() for p in range(8)]
    ag_in_h = [[nc.dram_tensor(f"ag_in_h{hh}_{p}", [128, 512], F32).ap() for p in range(8)] for hh in range(2)]
    ag_out_h = [[nc.dram_tensor(f"ag_out_h{hh}_{p}", [512, 512], F32).ap() for p in range(8)] for hh in range(2)]
    x1D = nc.dram_tensor("x1D", [OWN, D], F32).ap()
    hx2D = nc.dram_tensor("hx2D", [OWN, D], F32).ap()
    qD = nc.dram_tensor("qD", [D, OWN], F32).ap()
    uvD = nc.dram_tensor("uvD", [16384, 2 * D], BF16).ap()

    top = ExitStack()
    with top:
        S = Sch(nc, top)

        uid = [0]

        def sb(es, name, shape, dt=F32):
            uid[0] += 1
            return es.enter_context(nc.sbuf_tensor(f"{name}_{uid[0]}", list(shape), dt))

        def ps(es, name, shape, dt=F32):
            uid[0] += 1
            return es.enter_context(nc.psum_tensor(f"{name}_{uid[0]}", list(shape), dt))

        qs = ["sp", "act"]
        qi = [0]

        def nq():
            qi[0] += 1
            return qs[qi[0] % 2]

        identf = sb(top, "identf", [128, 128]); b_const = Buf()
        identb = sb(top, "identb", [128, 128], BF16)
        S.op("pool", lambda e: e.memset(identf[:], 0.0), writes=[b_const])
        S.op("pool", lambda e: e.affine_select(out=identf[:], in_=identf[:], pattern=[[-1, 128]], compare_op=ALU.not_equal, fill=1.0, base=0, channel_multiplier=1), reads=[b_const], writes=[b_const])
        S.op("dve", lambda e: e.tensor_copy(out=identb[:], in_=identf[:]), reads=[b_const], writes=[b_const])

        with ExitStack() as ph:
            cm = sb(ph, "cm", [128, 16, 2]); b_cm = Buf()
            cmb = sb(ph, "cmb", [128, 16, 2], BF16)
            ones2 = sb(ph, "ones2", [1, 2])
            wst = [sb(ph, f"wada{i}", [128, 16, 512]) for i in range(2)]; b_wst = [Buf(), Buf()]
            wsb = [sb(ph, f"wadab{i}", [128, 16, 512], BF16) for i in range(2)]; b_wsb = [Buf(), Buf()]
            brow = [sb(ph, f"brow{i}", [1, 512]) for i in range(2)]; b_brow = [Buf(), Buf()]
            mrow = [sb(ph, f"mrow{i}", [2, 512]) for i in range(2)]; b_mrow = [Buf(), Buf()]
            pm = [ps(ph, f"pm{i}", [2, 512]) for i in range(2)]; b_pm = [PBuf(), PBuf()]
            b_modD = Buf()
            S.dma("sp", cm[:], cmod, writes=[b_cm])
            S.op("act", lambda e: e.activation(out=cm[:], in_=cm[:], func=AF.Silu), reads=[b_cm], writes=[b_cm])
            S.op("dve", lambda e: e.memset(ones2[:], 1.0), writes=[b_cm])
            S.op("dve", lambda e: e.tensor_copy(out=cmb[:], in_=cm[:]), reads=[b_cm], writes=[b_cm])
            b_modin = Buf()
            for ch in range(6):
                i = ch % 2
                S.dma("sp" if ch % 2 == 0 else "act", wst[i][:], w_ada[ch], writes=[b_wst[i]])
                S.dma("sp", brow[i][:], b_ada[0:1, ch * 512:(ch + 1) * 512], writes=[b_brow[i]])
                S.op("dve", lambda e: e.tensor_copy(out=wsb[i][:, 0:8, :], in_=wst[i][:, 0:8, :]), reads=[b_wst[i]], writes=[b_wsb[i]])
                S.op("pool", lambda e: e.tensor_copy(out=wsb[i][:, 8:16, :], in_=wst[i][:, 8:16, :]), reads=[b_wst[i]], writes=[b_wsb[i]])
                for k in range(16):
                    S.op("pe", lambda e: e.matmul(pm[i][:, :], lhsT=cmb[:, k, :], rhs=wsb[i][:, k, :], start=(k == 0), stop=False), reads=[b_cm, b_wsb[i]], writes=[b_pm[i]])
                S.op("pe", lambda e: e.matmul(pm[i][:, :], lhsT=ones2[0:1, :], rhs=brow[i][0:1, :], start=False, stop=True), reads=[b_cm, b_brow[i]], writes=[b_pm[i]])
                S.op("act", lambda e: e.activation(out=mrow[i][:], in_=pm[i][:], func=AF.Copy), reads=[b_pm[i]], writes=[b_mrow[i]])
                S.dma("sp", mod_in[:, ch * 512:(ch + 1) * 512], mrow[i][:], reads=[b_mrow[i]], writes=[b_modin])
            b_modout = Buf()
            S._wait("pool", S._deps([b_modin], [b_modout], "pool"))
            nc.gpsimd.collective_compute("AllGather", ALU.bypass, replica_groups=[[0, 1, 2, 3], [4, 5, 6, 7]], ins=[mod_in], outs=[mod_out]).then_inc(S.sem["cc"], 1)
            S.cnt["cc"] += 1
            b_modout.w = ("cc", S.cnt["cc"])
            for r in range(4):
                S.dma("sp", modD[:, 3072 * r:3072 * (r + 1)], mod_out[2 * r:2 * r + 2, :], reads=[b_modout], writes=[b_modD])
            S.barrier()

        S.mark("p0_adaLN")

        def bc_load(q, dst, src_row, bufs_w, reads=()):
            S.dma(q, dst, src_row.to_broadcast([128, src_row.shape[1]]), reads=list(reads), writes=bufs_w)

        def make_AB(ph, row, nrow, sc_off, sh_off, tag):
            A = sb(ph, "A" + tag, [128, D]); Bt = sb(ph, "B" + tag, [128, D]); bA = Buf(); bB = Buf()
            bc_load("sp", A[:], modD[row:row + 1, sc_off:sc_off + D], [bA], reads=[b_modD])
            bc_load("act", Bt[:], nrm[nrow:nrow + 1, :], [bB])
            S.op("dve", lambda e: e.scalar_tensor_tensor(out=A[:], in0=A[:], scalar=1.0, in1=Bt[:], op0=ALU.add, op1=ALU.mult), reads=[bA, bB], writes=[bA])
            bc_load("sp", Bt[:], modD[row:row + 1, sh_off:sh_off + D], [bB], reads=[b_modD, bA])
            return A, Bt, bA, bB

        def norm_tiles(ph, src, ntiles, ABsel, dstT, tag, src_buf=None, store=None):
            xt = [sb(ph, f"xt{tag}{i}", [128, D]) for i in range(2)]; b_xt = [Buf(), Buf()]
            hb = [sb(ph, f"hb{tag}{i}", [128, D], BF16) for i in range(2)]; b_hb = [Buf(), Buf()]
            st = [sb(ph, f"st{tag}{i}", [128, 2]) for i in range(2)]; b_st = [Buf(), Buf()]
            pT = [ps(ph, f"pT{tag}{i}", [128, 512], BF16) for i in range(2)]; b_pT = [PBuf(), PBuf()]
            b_dst = Buf()

            def tile_gen(tl):
                i = tl % 2
                A, Bt, bA, bB = ABsel(tl)
                S.dma("sp", xt[i][:], src[tl * 128:(tl + 1) * 128, :], reads=[src_buf] if src_buf is not None else [], writes=[b_xt[i]])
                S.op("dve", lambda e: e.memset(st[i][:], 0.0), writes=[b_st[i]])
                S.op("act", lambda e: e.activation(out=hb[i][:], in_=xt[i][:], func=AF.Square, accum_out=st[i][:, 0:1]), reads=[b_xt[i]], writes=[b_hb[i], b_st[i]])
                yield
                S.op("act", lambda e: e.activation(out=st[i][:, 1:2], in_=st[i][:, 0:1], func=AF.Sqrt, scale=1.0 / D, bias=EPS), reads=[b_st[i]], writes=[b_st[i]])
                yield
                S.op("dve", lambda e: e.reciprocal(out=st[i][:, 1:2], in_=st[i][:, 1:2]), reads=[b_st[i]], writes=[b_st[i]])
                S.op("dve", lambda e: e.scalar_tensor_tensor(out=xt[i][:], in0=xt[i][:], scalar=st[i][:, 1:2], in1=A[:], op0=ALU.mult, op1=ALU.mult), reads=[b_xt[i], b_st[i], bA], writes=[b_xt[i]])
                yield
                if store is None:
                    S.op("pool", lambda e: e.tensor_tensor(out=hb[i][:], in0=xt[i][:], in1=Bt[:], op=ALU.add), reads=[b_xt[i], bB], writes=[b_hb[i]])
                else:
                    S.op("pool", lambda e: e.tensor_tensor(out=xt[i][:], in0=xt[i][:], in1=Bt[:], op=ALU.add), reads=[b_xt[i], bB], writes=[b_xt[i]])
                    S.dma("act", store[0][tl * 128:(tl + 1) * 128, :], xt[i][:], reads=[b_xt[i]], writes=[store[1]])
                    S.op("pool", lambda e: e.tensor_copy(out=hb[i][:], in_=xt[i][:]), reads=[b_xt[i]], writes=[b_hb[i]])
                yield
                for kq in range(4):
                    j = kq % 2
                    for kk in range(4):
                        k = kq * 4 + kk
                        S.op("pe", lambda e: e.transpose(out=pT[j][:, kk * 128:(kk + 1) * 128], in_=hb[i][:, k * 128:(k + 1) * 128], identity=identb[:]), reads=[b_hb[i], b_const], writes=[b_pT[j]])
                    S.op("act" if kq % 2 == 0 else "dve", lambda e: (e.activation(out=dstT[:, kq * 4:(kq + 1) * 4, tl * 128:(tl + 1) * 128], in_=pT[j][:, :].rearrange("p (a b) -> p a b", a=4), func=AF.Copy) if kq % 2 == 0 else e.tensor_copy(out=dstT[:, kq * 4:(kq + 1) * 4, tl * 128:(tl + 1) * 128], in_=pT[j][:, :].rearrange("p (a b) -> p a b", a=4))), reads=[b_pT[j]], writes=[b_dst])
                    yield

            for t0_ in range(0, ntiles, 2):
                gens = [tile_gen(t_) for t_ in range(t0_, min(t0_ + 2, ntiles))]
                while gens:
                    for g_ in list(gens):
                        try:
                            next(g_)
                        except StopIteration:
                            gens.remove(g_)
            return b_dst

        def project(ph, wsrc, nchunks, hT, b_hT, ntok, dstD, b_dstD, tag):
            wf = [sb(ph, f"wf{tag}{i}", [128, 16, 128]) for i in range(3)]; b_wf = [Buf() for _ in range(3)]
            wb = [sb(ph, f"wb{tag}{i}", [128, 16, 128], BF16) for i in range(3)]; b_wb = [Buf() for _ in range(3)]
            ze = [sb(ph, f"ze{tag}{i}", [128, 512]) for i in range(3)]; b_ze = [Buf() for _ in range(3)]
            pz = [ps(ph, f"pz{tag}{i}", [128, 512]) for i in range(3)]; b_pz = [PBuf() for _ in range(3)]
            it = 0
            for j in range(nchunks):
                i = j % 3
                S.dma("sp", wf[i][:], wsrc[j], writes=[b_wf[i]])
                S.op("pool" if j % 2 else "dve", lambda e: e.tensor_copy(out=wb[i][:], in_=wf[i][:]), reads=[b_wf[i]], writes=[b_wb[i]])
                for t0 in range(0, ntok, 512):
                    n = min(512, ntok - t0)
                    pi = it % 3
                    it += 1
                    for k in range(16):
                        S.op("pe", lambda e: e.matmul(pz[pi][:, 0:n], lhsT=wb[i][:, k, :], rhs=hT[:, k, t0:t0 + n], start=(k == 0), stop=(k == 15)), reads=[b_wb[i], b_hT], writes=[b_pz[pi]])
                    S.op("act", lambda e: e.activation(out=ze[pi][:, 0:n], in_=pz[pi][:, 0:n], func=AF.Copy), reads=[b_pz[pi]], writes=[b_ze[pi]])
                    S.dma("act", dstD[j * 128:(j + 1) * 128, t0:t0 + n], ze[pi][:, 0:n], reads=[b_ze[pi]], writes=[b_dstD])

        b_zT = Buf(); b_gT = Buf()
        with ExitStack() as ph:
            hTo = sb(ph, "hTo", [128, 16, OWN], BF16)
            with ExitStack() as ph2:
                A, Bt, bA, bB = make_AB(ph2, 0, 0, 1 * D, 0 * D, "o")
                b_hTo = norm_tiles(ph2, xown, 8, lambda tl: (A, Bt, bA, bB), hTo, "o")
                S.barrier()
            S.mark("p1b_norm_own")
            project(ph, wB, 32, hTo, b_hTo, OWN, gT, b_gT, "g")
            S.barrier()
            S.mark("p2b_gate_proj")
        with ExitStack() as ph:
          if not SIMTAIL:
            hT = sb(ph, "hT", [128, 16, T], BF16)
            with ExitStack() as ph2:
                Ax, Bx, bAx, bBx = make_AB(ph2, 0, 0, 1 * D, 0 * D, "x")
                Ac, Bc, bAc, bBc = make_AB(ph2, 1, 0, 1 * D, 0 * D, "c")
                b_hT = norm_tiles(ph2, seq, 34, lambda tl: (Ac, Bc, bAc, bBc) if tl < 2 else (Ax, Bx, bAx, bBx), hT, "a")
                S.barrier()
            S.mark("p1_norm_all")
            project(ph, wA, 20, hT, b_hT, T, zT, b_zT, "z")
            S.barrier()
            S.mark("p2_head_proj")
        with ExitStack() as ph:
            PADW = 65
            zr = [sb(ph, f"zr{i}", [128, T]) for i in range(2)]; b_zr = [Buf(), Buf()]
            zc = [sb(ph, f"zc{i}", [128, T]) for i in range(2)]; b_zc = [Buf(), Buf()]
            cw = [sb(ph, f"cw{i}", [128, 9]) for i in range(2)]; b_cw = [Buf(), Buf()]
            zp = [[sb(ph, f"zp{i}{v}", [128, 4096 + 2 * PADW], BF16) for v in range(3)] for i in range(2)]; b_zp = [[Buf() for _ in range(3)] for _ in range(2)]
            dgc = [[sb(ph, f"dgc{i}{t_}", [128, 128], BF16) for t_ in range(9)] for i in range(2)]; b_dgc = [Buf(), Buf()]
            pcv = [ps(ph, f"pcv{i}", [128, 512]) for i in range(2)]; b_pcv = [PBuf(), PBuf()]
            for i in range(2):
                for v in range(3):
                    S.op("pool", lambda e: e.memset(zp[i][v][:, 0:PADW], 0.0), writes=[b_zp[i][v]])
                    S.op("pool", lambda e: e.memset(zp[i][v][:, PADW + 4096:], 0.0), writes=[b_zp[i][v]])
            it = 0
            for j in range(0 if SIMTAIL else 10):
                i = j % 2
                S.dma("sp", zr[i][:], zT[j * 128:(j + 1) * 128, :], reads=[b_zT], writes=[b_zr[i]])
                S.dma("sp", cw[i][:], convw[j], writes=[b_cw[i]])
                xmid = [zp[i][v][:, PADW:PADW + 4096] for v in range(3)]
                S.op("act", lambda e: e.activation(out=xmid[0], in_=zr[i][:, 256:T], func=AF.Copy), reads=[b_zr[i]], writes=[b_zp[i][0]])
                S.op("dve", lambda e: e.tensor_copy(out=xmid[1], in_=xmid[0]), reads=[b_zp[i][0]], writes=[b_zp[i][1]])
                S.op("dve", lambda e: e.tensor_copy(out=xmid[2], in_=xmid[0]), reads=[b_zp[i][0]], writes=[b_zp[i][2]])
                S.op("dve", lambda e: e.memset(xmid[1].rearrange("p (r c) -> p r c", c=64)[:, :, 63:64], 0.0), reads=[b_zp[i][1]], writes=[b_zp[i][1]])
                S.op("dve", lambda e: e.memset(xmid[2].rearrange("p (r c) -> p r c", c=64)[:, :, 0:1], 0.0), reads=[b_zp[i][2]], writes=[b_zp[i][2]])
                for tap in range(9):
                    S.op("act", lambda e: e.activation(out=dgc[i][tap][:], in_=identf[:], func=AF.Copy, scale=cw[i][:, tap:tap + 1]), reads=[b_const, b_cw[i]], writes=[b_dgc[i]])
                for tb in range(8):
                    pi = it % 2
                    it += 1
                    n_ = 0
                    for dy in (-1, 0, 1):
                        for dx in (-1, 0, 1):
                            tap = (dy + 1) * 3 + (dx + 1)
                            v = {0: 0, -1: 1, 1: 2}[dx]
                            s0 = PADW + tb * 512 + 64 * dy + dx
                            S.op("pe", lambda e: e.matmul(pcv[pi][:, :], lhsT=dgc[i][tap][:], rhs=zp[i][v][:, s0:s0 + 512], start=(n_ == 0), stop=(n_ == 8)), reads=[b_dgc[i], b_zp[i][v]], writes=[b_pcv[pi]])
                            n_ += 1
                    if pi == 0:
                        S.op("act", lambda e: e.activation(out=zc[i][:, 256 + tb * 512:256 + (tb + 1) * 512], in_=pcv[pi][:, :], func=AF.Copy), reads=[b_pcv[pi]], writes=[b_zc[i]])
                    else:
                        S.op("dve", lambda e: e.tensor_copy(out=zc[i][:, 256 + tb * 512:256 + (tb + 1) * 512], in_=pcv[pi][:, :]), reads=[b_pcv[pi]], writes=[b_zc[i]])
                S.op("dve", lambda e: e.tensor_scalar(out=zc[i][:, 0:256], in0=zr[i][:, 0:256], scalar1=cw[i][:, 4:5], scalar2=None, op0=ALU.mult), reads=[b_zr[i], b_cw[i]], writes=[b_zc[i]])
                S.op("dve", lambda e: e.scalar_tensor_tensor(out=zc[i][:, 1:256], in0=zr[i][:, 0:255], scalar=cw[i][:, 3:4], in1=zc[i][:, 1:256], op0=ALU.mult, op1=ALU.add), reads=[b_zr[i], b_cw[i], b_zc[i]], writes=[b_zc[i]])
                S.op("dve", lambda e: e.scalar_tensor_tensor(out=zc[i][:, 0:255], in0=zr[i][:, 1:256], scalar=cw[i][:, 5:6], in1=zc[i][:, 0:255], op0=ALU.mult, op1=ALU.add), reads=[b_zr[i], b_cw[i], b_zc[i]], writes=[b_zc[i]])
                S.dma("act", zT[j * 128:(j + 1) * 128, :], zc[i][:], reads=[b_zc[i], b_zr[i]], writes=[b_zT])
            S.barrier()

        S.mark("p2c_conv")
        if stage <= 2:
            with ExitStack() as ph:
                dt_ = sb(ph, "dbgt", [128, 8192]); bd = Buf()
                S.dma("sp", dt_[:, 0:T], zT[0:128, :], reads=[b_zT], writes=[bd])
                S.dma("sp", dt_[:, T:T + 1024], gT[0:128, :], reads=[b_gT], writes=[bd])
                S.dma("sp", dt_[:, 5376:5376 + 2048], zT[10 * 128:11 * 128, 0:2048], reads=[b_zT], writes=[bd])
                S.dma("sp", dt_[0:2, 7424:7424 + 512], modD[:, 0:512], reads=[b_modD], writes=[bd])
                S.dma("sp", dbg, dt_[:], reads=[bd])
                S.finish()
            return nc
        C0 = float(np.exp(-0.5))
        b_agin = Buf()
        msk = sb(top, "msk", [128, 9, 128]); b_msk = Buf()
        S.dma("sp", msk[:], cmask.rearrange("m p q -> p m q"), writes=[b_msk])
        bdones = msk[:, 6, :]

        def run(gens):
            gens = list(gens)
            while gens:
                for g_ in list(gens):
                    try:
                        next(g_)
                    except StopIteration:
                        gens.remove(g_)

        order = [list(range(NCH)), [3, 2, 1, 0] + list(range(NCH - 1, 3, -1))]

        b_tab = Buf()
        cst = {"i": 0}

        def cbufs(es):
            return ([sb(es, f"cf{i}", [128, D]) for i in range(2)], [sb(es, f"cb{i}", [128, D], BF16) for i in range(2)], [Buf(), Buf()], [Buf(), Buf()])

        def conv_tiles(cb_, n):
            cf, cb, b_cf, b_cb = cb_
            for _ in range(n):
                t = cst["i"]
                if t >= 256:
                    return
                cst["i"] += 1
                src, c0 = (peer_u, 0) if t < 128 else (peer_v, D)
                i = t % 2
                rs = slice((t % 128) * 128, (t % 128) * 128 + 128)
                S.dma("sp", cf[i][:], src[rs, :], writes=[b_cf[i]])
                S.op("pool", lambda e: e.tensor_copy(out=cb[i][:], in_=cf[i][:]), reads=[b_cf[i]], writes=[b_cb[i]])
                S.dma("pool", uvD[rs, c0:c0 + D], cb[i][:], reads=[b_cb[i]], writes=[b_tab])

        def v3(ap, a):
            return ap.rearrange("p (a b) -> p a b", a=a)

        def rwkv_hp(hp):
            with ExitStack() as php:
                BK = [sb(php, f"BK{d}", [128, NCH, 128], BF16) for d in range(2)]
                AR = [sb(php, f"AR{d}", [128, NCH, 128], BF16) for d in range(2)]
                gam = [sb(php, f"gam{d}", [128, NCH]) for d in range(2)]
                vTb = sb(php, "vTb", [128, T], BF16)
                bonT = sb(php, "bonT", [128, 4096], BF16); ggT = sb(php, "ggT", [128, 4096], BF16)
                Oacc = sb(php, "Oacc", [64, 64, 128])
                prm = sb(php, "prm", [128, 12]); wl = sb(php, "wl", [128, 128]); al = sb(php, "al", [128, 128])
                gl0 = sb(php, "gl0", [128, 128]); gl1 = sb(php, "gl1", [32, 128])
                b_str = Buf(); b_O = Buf(); b_prm = Buf(); b_bon = Buf()
                S.dma("sp", prm[:, 0:9], rwp[hp], writes=[b_prm])
                S.dma("act", wl[:], wlb[hp], writes=[b_prm])
                S.dma("sp", al[:], alb[hp], writes=[b_prm])
                S.dma("act", gl0[:], glb[hp, 0:128, :], writes=[b_prm])
                S.dma("sp", gl1[:], glb[hp, 128:160, :], writes=[b_prm])
                S.op("dve", lambda e: e.tensor_scalar(out=prm[:, 9:10], in0=prm[:, 5:6], scalar1=-1.0, scalar2=1.0, op0=ALU.mult, op1=ALU.add), reads=[b_prm], writes=[b_prm])
                S.op("pool", lambda e: e.memset(Oacc[:], 0.0), writes=[b_O])
                with ExitStack() as ph:
                    TB = 256
                    tl = {}

                    cur_par = [0]

                    def tt(name, p=128, dt=F32):
                        key = (name, cur_par[0])
                        if key not in tl:
                            tl[key] = (sb(ph, "s_" + name, [p, TB], dt), Buf())
                        return tl[key]
                    pAs = [ps(ph, f"pA{i}", [128, 512]) for i in range(2)]; pBs = [ps(ph, f"pB{i}", [128, 512]) for i in range(2)]
                    pCs = [ps(ph, f"pC{i}", [128, 512]) for i in range(2)]; pDs = [ps(ph, f"pD{i}", [128, 512]) for i in range(2)]
                    b_pAs = [PBuf(), PBuf()]; b_pBs = [PBuf(), PBuf()]; b_pCs = [PBuf(), PBuf()]; b_pDs = [PBuf(), PBuf()]
                    def blkgen(blk):
                        t0 = blk * TB
                        c0 = blk * 4
                        cur_par[0] = blk % 2
                        pA, pB, pC, pD = pAs[blk % 2], pBs[blk % 2], pCs[blk % 2], pDs[blk % 2]
                        b_pA, b_pB, b_pC, b_pD = b_pAs[blk % 2], b_pBs[blk % 2], b_pCs[blk % 2], b_pDs[blk % 2]
                        b_pC2 = b_pC
                        rows = [3 * hp, 3 * hp + 1, 3 * hp + 2, 6, 7, 8, 9]
                        nm = ["r", "k", "v", "L0", "L1", "L2", "L3"]
                        for rr, n_ in zip(rows, nm):
                            t_, b_ = tt(n_)
                            S.dma("sp", t_[:], zT[rr * 128:(rr + 1) * 128, t0:t0 + TB], reads=[b_zT], writes=[b_])
                        (r_, b_r), (k_, b_k), (v_, b_v) = tt("r"), tt("k"), tt("v")
                        (L0, b_L0), (L1, b_L1), (L2, b_L2), (L3, b_L3) = tt("L0"), tt("L1"), tt("L2"), tt("L3")
                        yield
                        cur_par[0] = blk % 2
                        th, b_th = tt("th")
                        S.op("act", lambda e: e.activation(out=th[:], in_=L0[:], func=AF.Tanh), reads=[b_L0], writes=[b_th])
                        for d in range(2):
                            S.op("pe", lambda e: e.matmul(pA[:, d * 256:(d + 1) * 256], lhsT=wl[64 * d:64 * d + 64, :], rhs=th[64 * d:64 * d + 64, :], start=True, stop=True), reads=[b_prm, b_th], writes=[b_pA], pos=(64 * d, 0))
                            S.op("pe", lambda e: e.matmul(pB[:, d * 256:(d + 1) * 256], lhsT=al[64 * d:64 * d + 64, :], rhs=L1[64 * d:64 * d + 64, :], start=True, stop=True), reads=[b_prm, b_L1], writes=[b_pB], pos=(64 * d, 0))
                        s2, b_s2 = tt("s2"); s3, b_s3 = tt("s3")
                        S.op("act", lambda e: e.activation(out=s2[:], in_=L2[:], func=AF.Sigmoid), reads=[b_L2], writes=[b_s2])
                        S.op("act", lambda e: e.activation(out=s3[0:32, :], in_=L3[0:32, :], func=AF.Sigmoid), reads=[b_L3], writes=[b_s3])
                        S.op("pe", lambda e: e.matmul(pC[:, 0:256], lhsT=gl0[:, :], rhs=s2[:, :], start=True, stop=False), reads=[b_prm, b_s2], writes=[b_pC])
                        S.op("pe", lambda e: e.matmul(pC[:, 0:256], lhsT=gl1[0:32, :], rhs=s3[0:32, :], start=False, stop=True), reads=[b_prm, b_s3], writes=[b_pC])
                        yield
                        cur_par[0] = blk % 2
                        sg = []; aa = []
                        for d in range(2):
                            sgd, b_sgd = tt(f"sg{d}"); ad, b_ad = tt(f"a{d}")
                            S.op("act", lambda e: e.activation(out=sgd[:], in_=pA[:, d * 256:(d + 1) * 256], func=AF.Sigmoid, bias=prm[:, d:d + 1]), reads=[b_pA, b_prm], writes=[b_sgd])
                            S.op("act", lambda e: e.activation(out=ad[:], in_=pB[:, d * 256:(d + 1) * 256], func=AF.Sigmoid, bias=prm[:, 2 + d:3 + d]), reads=[b_pB, b_prm], writes=[b_ad])
                            sg.append((sgd, b_sgd)); aa.append((ad, b_ad))
                        yield
                        cur_par[0] = blk % 2
                        kkr, b_kkr = tt("kkr"); sq, b_sq = tt("sq"); nr, b_nr = tt("nr"); kk, b_kk = tt("kk")
                        S.op("dve", lambda e: e.tensor_scalar(out=kkr[:], in0=k_[:], scalar1=prm[:, 4:5], scalar2=None, op0=ALU.mult), reads=[b_k, b_prm], writes=[b_kkr])
                        S.op("pool", lambda e: e.tensor_tensor(out=sq[:], in0=kkr[:], in1=kkr[:], op=ALU.mult), reads=[b_kkr], writes=[b_sq])
                        S.op("pe", lambda e: e.matmul(pC[:, 256:512], lhsT=bdones, rhs=sq[:, :], start=True, stop=True), reads=[b_msk, b_sq], writes=[b_pC2])
                        S.op("act", lambda e: e.activation(out=nr[:], in_=pC[:, 256:512], func=AF.Sqrt), reads=[b_pC2], writes=[b_nr])
                        S.op("dve", lambda e: e.tensor_scalar_max(out=nr[:], in0=nr[:], scalar1=1e-12), reads=[b_nr], writes=[b_nr])
                        S.op("dve", lambda e: e.reciprocal(out=nr[:], in_=nr[:]), reads=[b_nr], writes=[b_nr])
                        S.op("pool", lambda e: e.tensor_tensor(out=kk[:], in0=kkr[:], in1=nr[:], op=ALU.mult), reads=[b_kkr, b_nr], writes=[b_kk])
                        yield
                        cur_par[0] = blk % 2
                        kd = []; kka = []
                        for d in range(2):
                            ad, b_ad = aa[d]
                            tm, b_tm = tt("tm"); kdd, b_kdd = tt(f"kd{d}"); kkad, b_kkad = tt(f"kka{d}")
                            S.op("dve", lambda e: e.tensor_scalar(out=tm[:], in0=ad[:], scalar1=prm[:, 5:6], scalar2=prm[:, 9:10], op0=ALU.mult, op1=ALU.add), reads=[b_ad, b_prm], writes=[b_tm])
                            S.op("pool", lambda e: e.tensor_tensor(out=kdd[:], in0=tm[:], in1=k_[:], op=ALU.mult), reads=[b_tm, b_k], writes=[b_kdd])
                            S.op("pool", lambda e: e.tensor_tensor(out=kkad[:], in0=kk[:], in1=ad[:], op=ALU.mult), reads=[b_kk, b_ad], writes=[b_kkad])
                            kd.append((kdd, b_kdd)); kka.append((kkad, b_kkad))
                        yield
                        cur_par[0] = blk % 2
                        for d in range(2):
                            sgd, b_sgd = sg[d]
                            cs, b_cs = tt(f"cs{d}"); ce, b_ce = tt(f"ce{d}"); ci, b_ci = tt(f"ci{d}")
                            for c in range(4):
                                S.op("dve", lambda e: e.tensor_tensor_scan(out=cs[:, c * 64:(c + 1) * 64], data0=sgd[:, c * 64:(c + 1) * 64], data1=sgd[:, c * 64:(c + 1) * 64], initial=0.0, op0=ALU.add, op1=ALU.bypass), reads=[b_sgd], writes=[b_cs])
                            if d == 0:
                                S.op("pool", lambda e: e.tensor_tensor(out=ce[:], in0=cs[:], in1=sgd[:], op=ALU.subtract), reads=[b_cs, b_sgd], writes=[b_ce])
                                ciu, b_ciu = cs, b_cs
                            else:
                                S.op("dve", lambda e: e.tensor_tensor(out=v3(ce[:, :], 4), in0=v3(cs[:, :], 4)[:, :, 63:64].to_broadcast([128, 4, 64]), in1=v3(cs[:, :], 4), op=ALU.subtract), reads=[b_cs], writes=[b_ce])
                                S.op("pool", lambda e: e.tensor_tensor(out=ci[:], in0=ce[:], in1=sgd[:], op=ALU.add), reads=[b_ce, b_sgd], writes=[b_ci])
                                ciu, b_ciu = ci, b_ci
                            Ein, b_Ein = tt("Ein"); Eex, b_Eex = tt("Eex"); Einv, b_Einv = tt("Einv")
                            S.op("act", lambda e: e.activation(out=Ein[:], in_=ciu[:], func=AF.Exp, scale=-C0), reads=[b_ciu], writes=[b_Ein])
                            S.op("act", lambda e: e.activation(out=Eex[:], in_=ce[:], func=AF.Exp, scale=-C0), reads=[b_ce], writes=[b_Eex])
                            S.op("act", lambda e: e.activation(out=Einv[:], in_=ciu[:], func=AF.Exp, scale=C0), reads=[b_ciu], writes=[b_Einv])
                            S.op("act", lambda e: e.activation(out=gam[d][:, c0:c0 + 4], in_=v3(cs[:, :], 4)[:, :, 63], func=AF.Exp, scale=-C0), reads=[b_cs], writes=[b_str])
                            kdd, b_kdd = kd[d]; kkad, b_kkad = kka[d]
                            S.op("dve", lambda e: e.tensor_tensor(out=AR[d][:, c0:c0 + 4, 0:64], in0=v3(kk[:, :], 4), in1=v3(Eex[:, :], 4), op=ALU.mult), reads=[b_kk, b_Eex], writes=[b_str])
                            S.op("pool", lambda e: e.tensor_tensor(out=AR[d][:, c0:c0 + 4, 64:128], in0=v3(r_[:, :], 4), in1=v3(Ein[:, :], 4), op=ALU.mult), reads=[b_r, b_Ein], writes=[b_str])
                            S.op("dve", lambda e: e.scalar_tensor_tensor(out=BK[d][:, c0:c0 + 4, 0:64], in0=v3(kkad[:, :], 4), scalar=-1.0, in1=v3(Einv[:, :], 4), op0=ALU.mult, op1=ALU.mult), reads=[b_kkad, b_Einv], writes=[b_str])
                            S.op("pool", lambda e: e.tensor_tensor(out=BK[d][:, c0:c0 + 4, 64:128], in0=v3(kdd[:, :], 4), in1=v3(Einv[:, :], 4), op=ALU.mult), reads=[b_kdd, b_Einv], writes=[b_str])
                        yield
                        cur_par[0] = blk % 2
                        S.op("act", lambda e: e.activation(out=vTb[:, t0:t0 + TB], in_=v_[:], func=AF.Copy), reads=[b_v], writes=[b_str])
                        if blk >= 1:
                            tx0 = t0 - 256
                            rk, b_rk = tt("rk"); kds, b_kds = tt("kds")
                            S.op("dve", lambda e: e.tensor_scalar(out=rk[:], in0=r_[:], scalar1=prm[:, 6:7], scalar2=None, op0=ALU.mult), reads=[b_r, b_prm], writes=[b_rk])
                            S.op("pool", lambda e: e.tensor_tensor(out=kds[:], in0=kd[0][0][:], in1=kd[1][0][:], op=ALU.add), reads=[kd[0][1], kd[1][1]], writes=[b_kds])
                            S.op("pool", lambda e: e.tensor_tensor(out=kds[:], in0=kds[:], in1=rk[:], op=ALU.mult), reads=[b_kds, b_rk], writes=[b_kds])
                            S.op("pe", lambda e: e.matmul(pD[:, 0:256], lhsT=bdones, rhs=kds[:, :], start=True, stop=True), reads=[b_msk, b_kds], writes=[b_pD])
                            S.op("dve", lambda e: e.tensor_tensor(out=bonT[:, tx0:tx0 + TB], in0=pD[:, 0:256], in1=v_[:], op=ALU.mult), reads=[b_pD, b_v], writes=[b_bon])
                            S.op("act", lambda e: e.activation(out=ggT[:, tx0:tx0 + TB], in_=pC[:, 0:256], func=AF.Copy), reads=[b_pC], writes=[b_bon])
                    for b0 in range(0, T // TB, 2):
                        run([blkgen(b_) for b_ in range(b0, min(b0 + 2, T // TB))])
                    S.barrier()
                S.mark(f"rw{hp}_streams")
                if os.environ.get("K_PHASE") == "streams":
                    return
                with ExitStack() as ph:
                    bank = [[ps(ph, f"bk{d}{i}", [128, 512]) for i in range(3)] for d in range(2)]
                    bankb = [ps(ph, f"bkb{d}", [128, 1024], BF16) for d in range(2)]
                    PM = [v3(bank[d][0][:, 0:256], 2) for d in range(2)]
                    PX = [bank[d][0][:, 256:384] for d in range(2)]
                    PY = [bank[d][0][:, 384:512] for d in range(2)]
                    PN = [[bank[d][1][:, 128 * i:128 * (i + 1)] for i in range(3)] for d in range(2)]
                    PW = [bank[d][2][:, 256:320] for d in range(2)]
                    PS_ = [bank[d][2][:, 320:384] for d in range(2)]
                    PU = [v3(bank[d][2][0:64, 0:128], 2) for d in range(2)]
                    PO = [v3(bank[d][2][0:64, 128:256], 2) for d in range(2)]
                    PVt = [v3(bankb[d][:, 0:128], 2) for d in range(2)]
                    PBt = [v3(bankb[d][:, 128:256], 2) for d in range(2)]
                    nb = lambda: Buf()
                    b_bank = [[PBuf() for _ in range(4)] for _ in range(2)]
                    b_PM = [b_bank[d][0] for d in range(2)]; b_PX = b_PM; b_PY = b_PM
                    b_PN = [[b_bank[d][1]] * 3 for d in range(2)]
                    b_PW = [b_bank[d][2] for d in range(2)]; b_PS = b_PW; b_PU = b_PW; b_PO = b_PW
                    b_PVt = [b_bank[d][3] for d in range(2)]; b_PBt = b_PVt
                    MS = [[sb(ph, f"MS{d}{p}", [128, 2, 128], BF16) for p in range(2)] for d in range(2)]; b_MS = [[nb(), nb()] for _ in range(2)]
                    Xs = [[sb(ph, f"Xs{d}{p}", [128, 128], BF16) for p in range(2)] for d in range(2)]; b_Xs = [[nb(), nb()] for _ in range(2)]
                    Ys = [[sb(ph, f"Ys{d}{p}", [128, 128], BF16) for p in range(2)] for d in range(2)]; b_Ys = [[nb(), nb()] for _ in range(2)]
                    Rs = [[sb(ph, f"Rs{d}{p}", [128, 128], BF16) for p in range(2)] for d in range(2)]; b_Rs = [[nb(), nb()] for _ in range(2)]
                    Yp = [sb(ph, f"Yp{d}", [128, 128]) for d in range(2)]; b_Yp = [nb(), nb()]
                    TT = [[sb(ph, f"TT{d}{p}", [128, 128], BF16) for p in range(2)] for d in range(2)]; b_TT = [[nb(), nb()] for _ in range(2)]
                    UV = [[[sb(ph, f"UV{d}{p}{j}", [128, 64], BF16) for j in range(2)] for p in range(2)] for d in range(2)]
                    b_UV = [[[nb(), nb()] for _ in range(2)] for _ in range(2)]
                    BKt = [[sb(ph, f"BKt{d}{p}", [128, 2, 64], BF16) for p in range(2)] for d in range(2)]; b_BKt = [[nb(), nb()] for _ in range(2)]
                    W0 = [sb(ph, f"W0{d}", [128, 64], BF16) for d in range(2)]; b_W0 = [nb(), nb()]
                    ST = [[sb(ph, f"ST{d}{p}", [128, 64], BF16) for p in range(2)] for d in range(2)]; b_ST = [[nb(), nb()] for _ in range(2)]
                    cvb = cbufs(ph)
                    for d in range(2):
                        S.op("dve", lambda e: e.memset(bank[d][0][:, 256:512], 0.0), writes=[b_PX[d]])
                        S.op("pool", lambda e: e.memset(ST[d][0][:], 0.0), writes=[b_ST[d][0]])
                        for p in range(2):
                            for j in range(2):
                                S.op("pool", lambda e: e.memset(UV[d][p][j][:], 0.0), writes=[b_UV[d][p][j]])

                    def prep(d, i):
                        c = order[d][i]; par = i % 2
                        ARc = AR[d][:, c, :]; BKc = BK[d][:, c, :]
                        for j in range(2):
                            p0 = 64 * j
                            S.op("pe", lambda e: e.matmul(PM[d][:, j, :], lhsT=BKc[p0:p0 + 64, :], rhs=ARc[p0:p0 + 64, :], start=True, stop=True), reads=[b_str], writes=[b_PM[d]], pos=(p0, 0))
                            S.op("pe", lambda e: e.matmul(PX[d][p0:p0 + 64, p0:p0 + 64], lhsT=BKc[p0:p0 + 64, 0:64], rhs=ARc[p0:p0 + 64, 0:64], start=True, stop=True), reads=[b_str], writes=[b_PX[d]], pos=(p0, p0))
                            S.op("pe", lambda e: e.matmul(PY[d][p0:p0 + 64, p0:p0 + 64], lhsT=ARc[p0:p0 + 64, 0:64], rhs=BKc[p0:p0 + 64, 0:64], start=True, stop=True), reads=[b_str], writes=[b_PY[d]], pos=(p0, p0))
                            S.op("pe", lambda e: e.transpose(out=PVt[d][64:128, j, :], in_=vTb[p0:p0 + 64, c * 64:(c + 1) * 64], identity=identb[p0:p0 + 64, p0:p0 + 64]), reads=[b_str, b_const], writes=[b_PVt[d]], pos=(p0, 64))
                            S.op("pe", lambda e: e.transpose(out=PBt[d][:, j, :], in_=BKc[p0:p0 + 64, :], identity=identb[p0:p0 + 64, p0:p0 + 64]), reads=[b_str, b_const], writes=[b_PBt[d]], pos=(p0, 0))
                        yield
                        S.op("dve", lambda e: e.tensor_tensor(out=MS[d][par][:], in0=PM[d], in1=msk[:, d, :].unsqueeze(1).to_broadcast([128, 2, 128]), op=ALU.mult), reads=[b_PM[d], b_msk], writes=[b_MS[d][par]])
                        S.op("dve", lambda e: e.tensor_tensor(out=Xs[d][0][:], in0=PX[d], in1=msk[:, 2 + d, :], op=ALU.mult), reads=[b_PX[d], b_msk], writes=[b_Xs[d][0]])
                        S.op("dve", lambda e: e.tensor_tensor(out=Ys[d][0][:], in0=PY[d], in1=msk[:, 4 + d, :], op=ALU.mult), reads=[b_PY[d], b_msk], writes=[b_Ys[d][0]])
                        S.op("pool", lambda e: e.tensor_tensor(out=Rs[d][0][:], in0=identb[:], in1=Xs[d][0][:], op=ALU.subtract), reads=[b_Xs[d][0], b_const], writes=[b_Rs[d][0]])
                        for j in range(2):
                            S.op("act", lambda e: e.activation(out=UV[d][par][j][64:128, :], in_=PVt[d][64:128, j, :], func=AF.Copy), reads=[b_PVt[d]], writes=[b_UV[d][par][j]])
                        S.op("act", lambda e: e.activation(out=BKt[d][par][:], in_=PBt[d], func=AF.Copy), reads=[b_PBt[d]], writes=[b_BKt[d][par]])
                        yield
                        xi = 0; ri = 0
                        for lvl in range(1, 7):
                            if lvl <= 4:
                                S.op("pe", lambda e: e.matmul(PN[d][0], lhsT=Ys[d][xi][:], rhs=Xs[d][xi][:], start=True, stop=True), reads=[b_Ys[d][xi], b_Xs[d][xi]], writes=[b_PN[d][0]])
                            if lvl <= 5:
                                S.op("pe", lambda e: e.matmul(PN[d][1], lhsT=Xs[d][xi][:], rhs=Ys[d][xi][:], start=True, stop=True), reads=[b_Ys[d][xi], b_Xs[d][xi]], writes=[b_PN[d][1]])
                            if lvl >= 2:
                                S.op("pe", lambda e: e.matmul(PN[d][2], lhsT=identb[:], rhs=Rs[d][ri][:], start=True, stop=False), reads=[b_const, b_Rs[d][ri]], writes=[b_PN[d][2]])
                                S.op("pe", lambda e: e.matmul(PN[d][2], lhsT=Ys[d][xi][:], rhs=Rs[d][ri][:], start=False, stop=True), reads=[b_Ys[d][xi], b_Rs[d][ri]], writes=[b_PN[d][2]])
                            yield
                            if lvl <= 4:
                                S.op("act", lambda e: e.activation(out=Xs[d][1 - xi][:], in_=PN[d][0], func=AF.Copy), reads=[b_PN[d][0]], writes=[b_Xs[d][1 - xi]])
                            if lvl <= 5:
                                S.op("act", lambda e: e.activation(out=Ys[d][1 - xi][:], in_=PN[d][1], func=AF.Copy), reads=[b_PN[d][1]], writes=[b_Ys[d][1 - xi]])
                            if lvl >= 2:
                                if lvl == 6:
                                    S.op("act", lambda e: e.activation(out=TT[d][par][:], in_=PN[d][2], func=AF.Copy), reads=[b_PN[d][2]], writes=[b_TT[d][par]])
                                else:
                                    S.op("act", lambda e: e.activation(out=Rs[d][1 - ri][:], in_=PN[d][2], func=AF.Copy), reads=[b_PN[d][2]], writes=[b_Rs[d][1 - ri]])
                                ri = 1 - ri
                            xi = 1 - xi
                            yield

                    def step(d, i):
                        c = order[d][i]; par = i % 2; cur = i % 2; nxt = 1 - cur
                        isx = c >= 4; xc = c - 4
                        ARc = AR[d][:, c, :]
                        for j in range(2):
                            p0 = 64 * j
                            S.op("pe", lambda e: e.matmul(PW[d][p0:p0 + 64, :], lhsT=ARc[p0:p0 + 64, 0:64], rhs=ST[d][cur][p0:p0 + 64, :], start=True, stop=False), reads=[b_str, b_ST[d][cur]], writes=[b_PW[d]], pos=(p0, p0))
                            S.op("pe", lambda e: e.matmul(PW[d][p0:p0 + 64, :], lhsT=MS[d][par][64:128, j, 0:64], rhs=UV[d][par][j][64:128, :], start=False, stop=True), reads=[b_MS[d][par], b_UV[d][par][j]], writes=[b_PW[d]], pos=(64, p0))
                        S.op("dve", lambda e: e.tensor_copy(out=W0[d][:], in_=PW[d]), reads=[b_PW[d]], writes=[b_W0[d]])
                        yield
                        for j in range(2):
                            p0 = 64 * j
                            S.op("pe", lambda e: e.matmul(PU[d][:, j, :], lhsT=TT[d][par][p0:p0 + 64, p0:p0 + 64], rhs=W0[d][p0:p0 + 64, :], start=True, stop=True), reads=[b_TT[d][par], b_W0[d]], writes=[b_PU[d]], pos=(p0, 0))
                        for j in range(2):
                            S.op("dve", lambda e: e.tensor_copy(out=UV[d][par][j][0:64, :], in_=PU[d][:, j, :]), reads=[b_PU[d]], writes=[b_UV[d][par][j]])
                        yield
                        if isx:
                            for j in range(2):
                                p0 = 64 * j
                                S.op("pe", lambda e: e.matmul(PO[d][:, j, :], lhsT=ARc[p0:p0 + 64, 64:128], rhs=ST[d][cur][p0:p0 + 64, :], start=True, stop=False), reads=[b_str, b_ST[d][cur]], writes=[b_PO[d]], pos=(p0, 0))
                                S.op("pe", lambda e: e.matmul(PO[d][:, j, :], lhsT=MS[d][par][:, j, 64:128], rhs=UV[d][par][j][:, :], start=False, stop=True), reads=[b_MS[d][par], b_UV[d][par][j]], writes=[b_PO[d]])
                            S.op("dve", lambda e: e.tensor_tensor(out=v3(Oacc[:, xc, :], 2), in0=PO[d], in1=v3(Oacc[:, xc, :], 2), op=ALU.add), reads=[b_PO[d], b_O], writes=[b_O])
                        for j in range(2):
                            p0 = 64 * j
                            S.op("pe", lambda e: e.matmul(PS_[d][p0:p0 + 64, :], lhsT=identb[p0:p0 + 64, p0:p0 + 64], rhs=ST[d][cur][p0:p0 + 64, :], start=True, stop=False), reads=[b_const, b_ST[d][cur]], writes=[b_PS[d]], pos=(p0, p0))
                            S.op("pe", lambda e: e.matmul(PS_[d][p0:p0 + 64, :], lhsT=BKt[d][par][:, j, :], rhs=UV[d][par][j][:, :], start=False, stop=True), reads=[b_BKt[d][par], b_UV[d][par][j]], writes=[b_PS[d]], pos=(0, p0))
                        S.op("dve", lambda e: e.tensor_scalar(out=ST[d][nxt][:], in0=PS_[d], scalar1=gam[d][:, c:c + 1], scalar2=None, op0=ALU.mult), reads=[b_PS[d], b_str], writes=[b_ST[d][nxt]])
                        yield

                    if os.environ.get("K_PHASE") == "prep":
                        ns = int(os.environ.get("K_NS", 99))
                        for g_ in (prep(0, 0), prep(1, 0)):
                            for _ in range(ns):
                                try:
                                    next(g_)
                                except StopIteration:
                                    break
                        S.barrier()
                        return
                    if os.environ.get("K_PHASE") != "noprep":
                        run([prep(0, 0), prep(1, 0)])
                    for i in range(int(os.environ.get("K_STEPS", NCH))):
                        gs = [step(0, i), step(1, i)]
                        if i + 1 < NCH:
                            gs += [prep(0, i + 1), prep(1, i + 1)]
                        run(gs)
                        conv_tiles(cvb, 2)
                    S.barrier()
                S.mark(f"rw{hp}_scan")
                with ExitStack() as ph:
                    sm = sb(ph, "sm", [64, 128]); sm2 = sb(ph, "sm2", [64, 128]); b_sm = Buf()
                    sqb = sb(ph, "sqb", [64, 64, 128]); b_sqb = Buf()
                    onT = sb(ph, "onT", [128, 4096]); b_onT = Buf()
                    pR = [ps(ph, f"pR{i}", [128, 512]) for i in range(2)]; b_pR = [PBuf(), PBuf()]
                    O3 = Oacc[:].rearrange("p a (j v) -> p (a j) v", j=2)
                    S.op("dve", lambda e: e.tensor_reduce(out=sm[:], in_=O3, axis=AX.X, op=ALU.add), reads=[b_O], writes=[b_sm])
                    S.op("pool", lambda e: e.tensor_tensor(out=sqb[:], in0=Oacc[:], in1=Oacc[:], op=ALU.mult), reads=[b_O], writes=[b_sqb])
                    S.op("dve", lambda e: e.tensor_reduce(out=sm2[:], in_=sqb[:].rearrange("p a (j v) -> p (a j) v", j=2), axis=AX.X, op=ALU.add), reads=[b_sqb], writes=[b_sm])
                    S.op("dve", lambda e: e.tensor_scalar(out=sm[:], in0=sm[:], scalar1=1.0 / 64, scalar2=None, op0=ALU.mult), reads=[b_sm], writes=[b_sm])
                    S.op("dve", lambda e: e.scalar_tensor_tensor(out=sm2[:], in0=sm2[:], scalar=1.0 / 64, in1=sm2[:], op0=ALU.mult, op1=ALU.bypass), reads=[b_sm], writes=[b_sm])
                    mu2 = sb(ph, "mu2", [64, 128])
                    S.op("dve", lambda e: e.tensor_tensor(out=mu2[:], in0=sm[:], in1=sm[:], op=ALU.mult), reads=[b_sm], writes=[b_sm])
                    S.op("dve", lambda e: e.tensor_tensor(out=sm2[:], in0=sm2[:], in1=mu2[:], op=ALU.subtract), reads=[b_sm], writes=[b_sm])
                    S.op("act", lambda e: e.activation(out=sm2[:], in_=sm2[:], func=AF.Sqrt, bias=64e-5), reads=[b_sm], writes=[b_sm])
                    S.op("dve", lambda e: e.reciprocal(out=sm2[:], in_=sm2[:]), reads=[b_sm], writes=[b_sm])
                    S.op("dve", lambda e: e.tensor_tensor(out=O3, in0=O3, in1=sm[:].unsqueeze(2).to_broadcast([64, 128, 64]), op=ALU.subtract), reads=[b_O, b_sm], writes=[b_O])
                    S.op("dve", lambda e: e.tensor_tensor(out=O3, in0=O3, in1=sm2[:].unsqueeze(2).to_broadcast([64, 128, 64]), op=ALU.mult), reads=[b_O, b_sm], writes=[b_O])
                    for g8 in range(8):
                        pi = g8 % 2
                        for q in range(8):
                            xc = g8 * 8 + q
                            S.op("pe", lambda e: e.transpose(out=pR[pi][:, q * 64:(q + 1) * 64], in_=Oacc[:, xc, :], identity=identf[0:64, 0:64]), reads=[b_O, b_const], writes=[b_pR[pi]])
                        S.op("act", lambda e: e.activation(out=onT[:, g8 * 512:(g8 + 1) * 512], in_=pR[pi][:, :], func=AF.Copy), reads=[b_pR[pi]], writes=[b_onT])
                    S.op("dve", lambda e: e.tensor_scalar(out=onT[:], in0=onT[:], scalar1=prm[:, 7:8], scalar2=prm[:, 8:9], op0=ALU.mult, op1=ALU.add), reads=[b_onT, b_prm], writes=[b_onT])
                    S.op("pool", lambda e: e.tensor_tensor(out=onT[:], in0=onT[:], in1=bonT[:], op=ALU.add), reads=[b_onT, b_bon], writes=[b_onT])
                    S.op("dve", lambda e: e.tensor_tensor(out=onT[:], in0=onT[:], in1=ggT[:], op=ALU.mult), reads=[b_onT, b_bon], writes=[b_onT])
                    for p in range(8):
                        S.dma(nq(), ag_in_rw[p][hp * 128:(hp + 1) * 128, :], onT[:, 512 * p:512 * (p + 1)], reads=[b_onT], writes=[b_agin])
                    S.barrier()

        b_agin_h = [Buf(), Buf()]
        b_agout_rw = Buf(); b_agout_h = [Buf(), Buf()]

        def gather(ins_, outs_, b_in, b_out):
            S._wait("pool", S._deps([b_in], [b_out], "pool"))
            for p in range(8):
                nc.gpsimd.collective_compute("AllGather", ALU.bypass, replica_groups=[[0, 1, 2, 3], [4, 5, 6, 7]], ins=[ins_[p]], outs=[outs_[p]]).then_inc(S.sem["cc"], 1)
                S.cnt["cc"] += 1
            b_out.w = ("cc", S.cnt["cc"])
            b_out.r = {}

        for hp in range(0 if SIMTAIL else int(os.environ.get("K_HP", 2))):
            rwkv_hp(hp)
            S.mark(f"rw{hp}_readout")
        if not SIMTAIL:
            gather(ag_in_rw, ag_out_rw, b_agin, b_agout_rw)
        def hgrn_head(hh):
            with ExitStack() as php:
                QD = [sb(php, f"QD{d}", [128, T], BF16) for d in range(2)]
                KD = [sb(php, f"KD{d}", [128, T], BF16) for d in range(2)]
                QG = [sb(php, f"QG{d}", [128, T], BF16) for d in range(2)]
                KL = [sb(php, f"KL{d}", [128, T], BF16) for d in range(2)]
                iTb = sb(php, "iTb", [128, T], BF16)
                gmh = [sb(php, f"gmh{d}", [128, NCH]) for d in range(2)]
                sog = sb(php, "sog", [128, 4096])
                Oh = sb(php, "Oh", [64, 64, 128])
                hpr = sb(php, "hpr", [128, 12])
                b_str = Buf(); b_O = Buf(); b_hp = Buf(); b_sog = Buf()
                S.dma("sp", hpr[:, 0:4], hlb[hh], writes=[b_hp])
                S.dma("act", hpr[:, 4:5], hgn, writes=[b_hp])
                S.op("dve", lambda e: e.tensor_tensor(out=hpr[:, 5:7], in0=hpr[:, 0:2], in1=hpr[:, 2:4], op=ALU.subtract), reads=[b_hp], writes=[b_hp])
                S.op("act", lambda e: e.activation(out=hpr[:, 5:7], in_=hpr[:, 5:7], func=AF.Sigmoid), reads=[b_hp], writes=[b_hp])
                S.op("dve", lambda e: e.tensor_scalar(out=hpr[:, 7:9], in0=hpr[:, 5:7], scalar1=-1.0, scalar2=1.0, op0=ALU.mult, op1=ALU.add), reads=[b_hp], writes=[b_hp])
                S.op("pool", lambda e: e.memset(Oh[:], 0.0), writes=[b_O])
                with ExitStack() as ph:
                    TB = 256
                    tl = {}

                    cur_par = [0]

                    def tt(name, p=128, dt=F32):
                        key = (name, cur_par[0])
                        if key not in tl:
                            tl[key] = (sb(ph, "h_" + name, [p, TB], dt), Buf())
                        return tl[key]
                    def blkgen(blk):
                        t0 = blk * TB
                        c0 = blk * 4
                        cur_par[0] = blk % 2
                        for s_i, n_ in enumerate(["q", "ff", "fb", "i", "og"]):
                            t_, b_ = tt(n_)
                            rr = 10 + 5 * hh + s_i
                            S.dma("sp", t_[:], zT[rr * 128:(rr + 1) * 128, t0:t0 + TB], reads=[b_zT], writes=[b_])
                        (q_, b_q), (i_, b_i), (og_, b_og) = tt("q"), tt("i"), tt("og")
                        yield
                        cur_par[0] = blk % 2
                        qs, b_qs = tt("qs")
                        S.op("act", lambda e: e.activation(out=qs[:], in_=q_[:], func=AF.Silu), reads=[b_q], writes=[b_qs])
                        if blk >= 1:
                            S.op("act", lambda e: e.activation(out=sog[:, t0 - 256:t0 - 256 + TB], in_=og_[:], func=AF.Silu), reads=[b_og], writes=[b_sog])
                        S.op("pool", lambda e: e.tensor_copy(out=iTb[:, t0:t0 + TB], in_=i_[:]), reads=[b_i], writes=[b_str])
                        for d in range(2):
                            yield
                            cur_par[0] = blk % 2
                            f_, b_f = tt("ff" if d == 0 else "fb")
                            sgf, b_sgf = tt("sgf"); fg, b_fg = tt("fg"); lf, b_lf = tt("lf"); kk_, b_kk = tt("kk_")
                            cs, b_cs = tt("cs"); ce, b_ce = tt("ce"); ci2, b_ci2 = tt("ci2")
                            S.op("act", lambda e: e.activation(out=sgf[:], in_=f_[:], func=AF.Sigmoid), reads=[b_f], writes=[b_sgf])
                            S.op("dve", lambda e: e.tensor_scalar(out=fg[:], in0=sgf[:], scalar1=hpr[:, 7 + d:8 + d], scalar2=hpr[:, 5 + d:6 + d], op0=ALU.mult, op1=ALU.add), reads=[b_sgf, b_hp], writes=[b_fg])
                            S.op("act", lambda e: e.activation(out=lf[:], in_=fg[:], func=AF.Ln), reads=[b_fg], writes=[b_lf])
                            S.op("pool", lambda e: e.tensor_scalar(out=kk_[:], in0=fg[:], scalar1=-1.0, scalar2=1.0, op0=ALU.mult, op1=ALU.add), reads=[b_fg], writes=[b_kk])
                            for c in range(4):
                                S.op("dve", lambda e: e.tensor_tensor_scan(out=cs[:, c * 64:(c + 1) * 64], data0=lf[:, c * 64:(c + 1) * 64], data1=lf[:, c * 64:(c + 1) * 64], initial=0.0, op0=ALU.add, op1=ALU.bypass), reads=[b_lf], writes=[b_cs])
                            if d == 0:
                                ci, b_ci = cs, b_cs
                                iref, ilast = 32, 63
                            else:
                                S.op("dve", lambda e: e.tensor_tensor(out=v3(ce[:, :], 4), in0=v3(cs[:, :], 4)[:, :, 63:64].to_broadcast([128, 4, 64]), in1=v3(cs[:, :], 4), op=ALU.subtract), reads=[b_cs], writes=[b_ce])
                                S.op("pool", lambda e: e.tensor_tensor(out=ci2[:], in0=ce[:], in1=lf[:], op=ALU.add), reads=[b_ce, b_lf], writes=[b_ci2])
                                ci, b_ci = ci2, b_ci2
                                iref, ilast = 31, 0
                            dr, b_dr = tt("dr"); dl, b_dl = tt("dl")
                            E1, b_E1 = tt("E1"); E2, b_E2 = tt("E2"); E3, b_E3 = tt("E3"); E4, b_E4 = tt("E4")
                            S.op("dve", lambda e: e.tensor_tensor(out=v3(dr[:, :], 4), in0=v3(ci[:, :], 4), in1=v3(ci[:, :], 4)[:, :, iref:iref + 1].to_broadcast([128, 4, 64]), op=ALU.subtract), reads=[b_ci], writes=[b_dr])
                            S.op("dve", lambda e: e.tensor_tensor(out=v3(dl[:, :], 4), in0=v3(ci[:, :], 4), in1=v3(ci[:, :], 4)[:, :, ilast:ilast + 1].to_broadcast([128, 4, 64]), op=ALU.subtract), reads=[b_ci], writes=[b_dl])
                            S.op("act", lambda e: e.activation(out=E1[:], in_=dr[:], func=AF.Exp), reads=[b_dr], writes=[b_E1])
                            S.op("act", lambda e: e.activation(out=E2[:], in_=dr[:], func=AF.Exp, scale=-1.0), reads=[b_dr], writes=[b_E2])
                            S.op("act", lambda e: e.activation(out=E3[:], in_=ci[:], func=AF.Exp), reads=[b_ci], writes=[b_E3])
                            S.op("act", lambda e: e.activation(out=E4[:], in_=dl[:], func=AF.Exp, scale=-1.0), reads=[b_dl], writes=[b_E4])
                            S.op("act", lambda e: e.activation(out=gmh[d][:, c0:c0 + 4], in_=v3(ci[:, :], 4)[:, :, ilast], func=AF.Exp), reads=[b_ci], writes=[b_str])
                            S.op("dve", lambda e: e.tensor_tensor(out=QD[d][:, t0:t0 + TB], in0=qs[:], in1=E1[:], op=ALU.mult), reads=[b_qs, b_E1], writes=[b_str])
                            S.op("pool", lambda e: e.tensor_tensor(out=KD[d][:, t0:t0 + TB], in0=kk_[:], in1=E2[:], op=ALU.mult), reads=[b_kk, b_E2], writes=[b_str])
                            S.op("dve", lambda e: e.tensor_tensor(out=QG[d][:, t0:t0 + TB], in0=qs[:], in1=E3[:], op=ALU.mult), reads=[b_qs, b_E3], writes=[b_str])
                            S.op("pool", lambda e: e.tensor_tensor(out=KL[d][:, t0:t0 + TB], in0=kk_[:], in1=E4[:], op=ALU.mult), reads=[b_kk, b_E4], writes=[b_str])
                    for b0 in range(0, T // TB, 2):
                        run([blkgen(b_) for b_ in range(b0, min(b0 + 2, T // TB))])
                    S.barrier()
                with ExitStack() as ph:
                    bA = [ps(ph, f"hbA{d}", [128, 512]) for d in range(2)]
                    bB = [ps(ph, f"hbB{d}", [128, 512]) for d in range(2)]
                    bC = [ps(ph, f"hbC{d}", [128, 1024], BF16) for d in range(2)]
                    b_bA = [PBuf(), PBuf()]; b_bB = [PBuf(), PBuf()]; b_bC = [PBuf(), PBuf()]
                    PSc = [bA[d][0:64, 0:64] for d in range(2)]
                    PO = [bA[d][0:64, 64:192] for d in range(2)]
                    PSn = [bB[d][:, 0:128] for d in range(2)]
                    PVt = [bC[d][0:64, 0:128] for d in range(2)]
                    PKt = [bC[d][0:64, 128:256] for d in range(2)]
                    S32 = [sb(ph, f"S32{d}", [128, 128]) for d in range(2)]; b_S32 = [Buf(), Buf()]
                    Sb = [[sb(ph, f"Sb{d}{p}", [128, 128], BF16) for p in range(2)] for d in range(2)]; b_Sb = [[Buf(), Buf()] for _ in range(2)]
                    Msc = [[sb(ph, f"Msc{d}{p}", [64, 64], BF16) for p in range(2)] for d in range(2)]; b_Msc = [[Buf(), Buf()] for _ in range(2)]
                    Vt = [[sb(ph, f"Vt{d}{p}", [64, 128], BF16) for p in range(2)] for d in range(2)]; b_Vt = [[Buf(), Buf()] for _ in range(2)]
                    KLt = [[sb(ph, f"KLt{d}{p}", [64, 128], BF16) for p in range(2)] for d in range(2)]; b_KLt = [[Buf(), Buf()] for _ in range(2)]
                    cvb = cbufs(ph)
                    for d in range(2):
                        S.op("dve", lambda e: e.memset(S32[d][:], 0.0), writes=[b_S32[d]])
                        S.op("pool", lambda e: e.memset(Sb[d][0][:], 0.0), writes=[b_Sb[d][0]])

                    def prep(d, i):
                        c = order[d][i]; par = i % 2
                        sl = slice(c * 64, (c + 1) * 64)
                        S.op("pe", lambda e: e.matmul(PSc[d], lhsT=KD[d][:, sl], rhs=QD[d][:, sl], start=True, stop=True), reads=[b_str], writes=[b_bA[d]])
                        S.op("pe", lambda e: e.transpose(out=PVt[d], in_=iTb[:, sl], identity=identb[:]), reads=[b_str, b_const], writes=[b_bC[d]])
                        S.op("pe", lambda e: e.transpose(out=PKt[d], in_=KL[d][:, sl], identity=identb[:]), reads=[b_str, b_const], writes=[b_bC[d]])
                        yield
                        S.op("dve", lambda e: e.tensor_tensor(out=Msc[d][par][:], in0=PSc[d], in1=msk[0:64, 7 + d, 0:64], op=ALU.mult), reads=[b_bA[d], b_msk], writes=[b_Msc[d][par]])
                        S.op("act", lambda e: e.activation(out=Vt[d][par][:], in_=PVt[d], func=AF.Copy), reads=[b_bC[d]], writes=[b_Vt[d][par]])
                        S.op("act", lambda e: e.activation(out=KLt[d][par][:], in_=PKt[d], func=AF.Copy), reads=[b_bC[d]], writes=[b_KLt[d][par]])
                        yield

                    def step(d, i):
                        c = order[d][i]; par = i % 2; cur = i % 2; nxt = 1 - cur
                        sl = slice(c * 64, (c + 1) * 64)
                        isx = c >= 4; xc = c - 4
                        if isx:
                            S.op("pe", lambda e: e.matmul(PO[d], lhsT=Msc[d][par][:], rhs=Vt[d][par][:], start=True, stop=False), reads=[b_Msc[d][par], b_Vt[d][par]], writes=[b_bA[d]])
                            S.op("pe", lambda e: e.matmul(PO[d], lhsT=QG[d][:, sl], rhs=Sb[d][cur][:], start=False, stop=True), reads=[b_str, b_Sb[d][cur]], writes=[b_bA[d]])
                            S.op("dve", lambda e: e.tensor_tensor(out=Oh[:, xc, :], in0=PO[d], in1=Oh[:, xc, :], op=ALU.add), reads=[b_bA[d], b_O], writes=[b_O])
                        S.op("pe", lambda e: e.matmul(PSn[d], lhsT=KLt[d][par][:], rhs=Vt[d][par][:], start=True, stop=True), reads=[b_KLt[d][par], b_Vt[d][par]], writes=[b_bB[d]])
                        yield
                        S.op("dve", lambda e: e.scalar_tensor_tensor(out=S32[d][:], in0=S32[d][:], scalar=gmh[d][:, c:c + 1], in1=PSn[d], op0=ALU.mult, op1=ALU.add), reads=[b_S32[d], b_str, b_bB[d]], writes=[b_S32[d]])
                        S.op("act", lambda e: e.activation(out=Sb[d][nxt][:], in_=S32[d][:], func=AF.Copy), reads=[b_S32[d]], writes=[b_Sb[d][nxt]])
                        yield

                    run([prep(0, 0), prep(1, 0)])
                    for i in range(NCH):
                        gs = [step(0, i), step(1, i)]
                        if i + 1 < NCH:
                            gs += [prep(0, i + 1), prep(1, i + 1)]
                        run(gs)
                        conv_tiles(cvb, 1)
                    S.barrier()
                with ExitStack() as ph:
                    sqb = sb(ph, "hsq", [64, 64, 128]); b_sqb = Buf()
                    ssm = sb(ph, "hss", [64, 64]); b_ss = Buf()
                    ohT = sb(ph, "ohT", [128, 4096]); b_ohT = Buf()
                    pR = [ps(ph, f"hpR{i}", [128, 512]) for i in range(2)]; b_pR = [PBuf(), PBuf()]
                    S.op("pool", lambda e: e.tensor_tensor(out=sqb[:], in0=Oh[:], in1=Oh[:], op=ALU.mult), reads=[b_O], writes=[b_sqb])
                    S.op("dve", lambda e: e.tensor_reduce(out=ssm[:], in_=sqb[:], axis=AX.X, op=ALU.add), reads=[b_sqb], writes=[b_ss])
                    S.op("act", lambda e: e.activation(out=ssm[:], in_=ssm[:], func=AF.Sqrt, scale=1.0 / 128, bias=EPS), reads=[b_ss], writes=[b_ss])
                    S.op("dve", lambda e: e.reciprocal(out=ssm[:], in_=ssm[:]), reads=[b_ss], writes=[b_ss])
                    S.op("dve", lambda e: e.tensor_tensor(out=Oh[:], in0=Oh[:], in1=ssm[:].unsqueeze(2).to_broadcast([64, 64, 128]), op=ALU.mult), reads=[b_O, b_ss], writes=[b_O])
                    for g8 in range(8):
                        pi = g8 % 2
                        for q in range(8):
                            xc = g8 * 8 + q
                            S.op("pe", lambda e: e.transpose(out=pR[pi][:, q * 64:(q + 1) * 64], in_=Oh[:, xc, :], identity=identf[0:64, 0:64]), reads=[b_O, b_const], writes=[b_pR[pi]])
                        S.op("act", lambda e: e.activation(out=ohT[:, g8 * 512:(g8 + 1) * 512], in_=pR[pi][:, :], func=AF.Copy), reads=[b_pR[pi]], writes=[b_ohT])
                    S.op("dve", lambda e: e.scalar_tensor_tensor(out=ohT[:], in0=ohT[:], scalar=hpr[:, 4:5], in1=sog[:], op0=ALU.mult, op1=ALU.mult), reads=[b_ohT, b_hp, b_sog], writes=[b_ohT])
                    for p in range(8):
                        S.dma(nq(), ag_in_h[hh][p][:, :], ohT[:, 512 * p:512 * (p + 1)], reads=[b_ohT], writes=[b_agin_h[hh]])
                    S.barrier()

        for hh in range(0 if SIMTAIL else 2):
            hgrn_head(hh)
            gather(ag_in_h[hh], ag_out_h[hh], b_agin_h[hh], b_agout_h[hh])
            S.mark(f"hg{hh}_all")
        if SIMTAIL:
            for p in range(8):
                for r in range(4):
                    S.dma("sp", ag_out_rw[p][256 * r:256 * r + 256, :], ag_ref[512 * r:512 * r + 256, 512 * p:512 * (p + 1)], writes=[b_agout_rw])
                    for hh in range(2):
                        S.dma("sp", ag_out_h[hh][p][128 * r:128 * r + 128, :], ag_ref[512 * r + 256 + 128 * hh:512 * r + 384 + 128 * hh, 512 * p:512 * (p + 1)], writes=[b_agout_h[hh]])

        b_x1D = Buf(); b_hx2D = Buf(); b_qD = Buf(); b_out = Buf()
        with ExitStack() as pht:
            mT = sb(pht, "mT", [128, 16, OWN], BF16); b_mT = Buf()
            with ExitStack() as ph:
                yTb = sb(ph, "yTb", [128, 16, OWN], BF16); b_yT = Buf()
                sel = sb(ph, "sel", [128, 4]); b_sel = Buf()
                S.dma("sp", sel[:], selq, writes=[b_sel])
                ld = [sb(ph, f"yl{i}", [128, 4096]) for i in range(2)]; b_ld = [Buf(), Buf()]
                ya = [sb(ph, f"ya{i}", [128, OWN]) for i in range(2)]; b_ya = [Buf(), Buf()]
                for kc in range(16):
                    i = kc % 2
                    kk_ = kc % 8
                    for p in range(8):
                        if kc < 8:
                            src_, bsrc = ag_out_rw[p][256 * (kk_ // 2) + 128 * (kk_ % 2):256 * (kk_ // 2) + 128 * (kk_ % 2) + 128, :], b_agout_rw
                        else:
                            src_, bsrc = ag_out_h[kk_ % 2][p][128 * (kk_ // 2):128 * (kk_ // 2) + 128, :], b_agout_h[kk_ % 2]
                        S.dma(nq(), ld[i][:, 512 * p:512 * (p + 1)], src_, reads=[bsrc], writes=[b_ld[i]])
                    S.op("dve", lambda e: e.tensor_scalar(out=ya[i][:], in0=ld[i][:, 0:OWN], scalar1=sel[:, 0:1], scalar2=None, op0=ALU.mult), reads=[b_ld[i], b_sel], writes=[b_ya[i]])
                    for q in range(1, 4):
                        o_ = yTb[:, kc, :] if q == 3 else ya[i][:]
                        S.op("dve", lambda e: e.scalar_tensor_tensor(out=o_, in0=ld[i][:, q * OWN:(q + 1) * OWN], scalar=sel[:, q:q + 1], in1=ya[i][:], op0=ALU.mult, op1=ALU.add), reads=[b_ld[i], b_sel, b_ya[i]], writes=[b_yT] if q == 3 else [b_ya[i]])
                wuf = [sb(ph, f"wuf{i}", [128, 16, 128]) for i in range(2)]; b_wuf = [Buf(), Buf()]
                wub = [sb(ph, f"wub{i}", [128, 16, 128], BF16) for i in range(2)]; b_wub = [Buf(), Buf()]
                gl = [[sb(ph, f"gl{i}{br}", [128, 512]) for br in range(2)] for i in range(2)]; b_gl = [[Buf(), Buf()] for _ in range(2)]
                pU = [[ps(ph, f"pU{i}{br}", [128, 512]) for br in range(2)] for i in range(2)]; b_pU = [[PBuf(), PBuf()] for _ in range(2)]
                it = 0
                for j in range(16):
                    i = j % 2
                    S.dma(nq(), wuf[i][:], wup[j], writes=[b_wuf[i]])
                    S.op("pool", lambda e: e.tensor_copy(out=wub[i][:], in_=wuf[i][:]), reads=[b_wuf[i]], writes=[b_wub[i]])
                    for tb in range(2):
                        t0 = tb * 512
                        pi = it % 2
                        it += 1
                        for br in range(2):
                            for kc in range(8):
                                S.op("pe", lambda e: e.matmul(pU[pi][br][:, :], lhsT=wub[i][:, 8 * br + kc, :], rhs=yTb[:, 8 * br + kc, t0:t0 + 512], start=(kc == 0), stop=(kc == 7)), reads=[b_wub[i], b_yT], writes=[b_pU[pi][br]])
                            S.dma(nq(), gl[pi][br][:], gT[(16 * br + j) * 128:(16 * br + j + 1) * 128, t0:t0 + 512], reads=[b_gT], writes=[b_gl[pi][br]])
                            S.op("act", lambda e: e.activation(out=gl[pi][br][:], in_=gl[pi][br][:], func=AF.Sigmoid), reads=[b_gl[pi][br]], writes=[b_gl[pi][br]])
                            S.op("dve", lambda e: e.tensor_tensor(out=gl[pi][br][:], in0=pU[pi][br][:, :], in1=gl[pi][br][:], op=ALU.mult), reads=[b_pU[pi][br], b_gl[pi][br]], writes=[b_gl[pi][br]])
                        S.op("pool", lambda e: e.tensor_tensor(out=mT[:, j, t0:t0 + 512], in0=gl[pi][0][:], in1=gl[pi][1][:], op=ALU.add), reads=[b_gl[pi][0], b_gl[pi][1]], writes=[b_mT])
                S.barrier()
            S.mark("tailA1_up_merge")
            hx2T = sb(pht, "hx2T", [128, 16, OWN], BF16)
            with ExitStack() as ph:
                wof = sb(ph, "wof", [128, 16, 512]); b_wof = Buf()
                wob = [sb(ph, f"wob{n}", [128, 16, 512], BF16) for n in range(4)]; b_wob = Buf()
                for n in range(4):
                    S.dma(nq(), wof[:], wo[n], writes=[b_wof])
                    S.op("pool" if n % 2 else "dve", lambda e: e.tensor_copy(out=wob[n][:], in_=wof[:]), reads=[b_wof], writes=[b_wob])
                gmb = sb(ph, "gmb", [128, D]); b_gmb = Buf()
                bc_load("sp", gmb[:], modD[0:1, 2 * D:3 * D], [b_gmb], reads=[b_modD])
                xt = [sb(ph, f"xt2{i}", [128, D]) for i in range(2)]; b_xt = [Buf(), Buf()]
                o1 = [sb(ph, f"o1{i}", [128, D]) for i in range(2)]; b_o1 = [Buf(), Buf()]
                pO = [ps(ph, f"pO{n}", [128, 512]) for n in range(4)]; b_pO = [PBuf() for _ in range(4)]
                for tl in range(8):
                    i = tl % 2
                    S.dma(nq(), xt[i][:], xown[tl * 128:(tl + 1) * 128, :], writes=[b_xt[i]])
                    for n in range(4):
                        for k in range(16):
                            S.op("pe", lambda e: e.matmul(pO[n][:, :], lhsT=mT[:, k, tl * 128:(tl + 1) * 128], rhs=wob[n][:, k, :], start=(k == 0), stop=(k == 15)), reads=[b_mT, b_wob], writes=[b_pO[n]])
                        S.op("dve", lambda e: e.tensor_tensor(out=o1[i][:, n * 512:(n + 1) * 512], in0=pO[n][:, :], in1=gmb[:, n * 512:(n + 1) * 512], op=ALU.mult), reads=[b_pO[n], b_gmb], writes=[b_o1[i]])
                    S.op("pool", lambda e: e.tensor_tensor(out=o1[i][:], in0=o1[i][:], in1=xt[i][:], op=ALU.add), reads=[b_o1[i], b_xt[i]], writes=[b_o1[i]])
                    S.dma(nq(), x1D[tl * 128:(tl + 1) * 128, :], o1[i][:], reads=[b_o1[i]], writes=[b_x1D])
                S.barrier()
            with ExitStack() as ph:
                A2, B2, bA2, bB2 = make_AB(ph, 0, 1, 4 * D, 3 * D, "f")
                b_hx2T = norm_tiles(ph, x1D, 8, lambda tl: (A2, B2, bA2, bB2), hx2T, "f", src_buf=b_x1D, store=(hx2D, b_hx2D))
                S.barrier()
            with ExitStack() as ph:
                project(ph, wq, 16, hx2T, b_hx2T, OWN, qD, b_qD, "q")
                S.barrier()
        S.mark("tailA2B_out_norm_q")
        with ExitStack() as ph:
            cvb = cbufs(ph)
            conv_tiles(cvb, 512)
            S.barrier()
        with ExitStack() as ph:
            kT = sb(ph, "kT", [128, 2, 128]); b_kT = Buf()
            S.dma("sp", kT[:, 0, :], k1T, writes=[b_kT])
            S.dma("act", kT[:, 1, :], k2T, writes=[b_kT])
            gfb = sb(ph, "gfb", [128, D]); nfb = sb(ph, "nfb", [128, D]); b_cb = Buf()
            bc_load("sp", gfb[:], modD[0:1, 5 * D:6 * D], [b_cb], reads=[b_modD])
            bc_load("act", nfb[:], nrm[2:3, :], [b_cb])
            qt = sb(ph, "qt", [128, 16, 128]); b_qt = Buf()
            sc = sb(ph, "sc", [128, 16, 128]); b_sc = Buf()
            sc2 = sb(ph, "sc2", [128, 256]); b_sc2 = Buf()
            v16 = sb(ph, "v16", [128, 16, 16]); i16 = sb(ph, "i16", [128, 16, 16]); iu = sb(ph, "iu", [128, 16], U32); b_v16 = Buf(); b_iu = Buf()
            cand = sb(ph, "cand", [128, 8, 256]); ecand = sb(ph, "ecand", [128, 8, 256]); b_cand = Buf(); b_ecand = Buf()
            best = sb(ph, "best", [128, 8, 16]); b_best = Buf()
            eid_ = [sb(ph, f"eid{i}", [128, 128]) for i in range(2)]; eidi_ = [sb(ph, f"eidi{i}", [128, 128], I32) for i in range(2)]; b_eid_ = [Buf(), Buf()]
            gate_ = [sb(ph, f"gate{i}", [128, 8, 16]) for i in range(2)]; gs_ = [sb(ph, f"gs{i}", [128, 8]) for i in range(2)]; b_gate_ = [Buf(), Buf()]
            dots = sb(ph, "dots", [128, 128]); coef = sb(ph, "coef", [128, 128]); b_dots = Buf(); b_coef = Buf()
            b_dsl = [Buf() for _ in range(128)]; b_csl = [Buf() for _ in range(128)]
            hx2_ = [sb(ph, f"hx2{i}", [128, D]) for i in range(2)]; b_hx2_ = [Buf(), Buf()]
            hx2b_ = [sb(ph, f"hx2b{i}", [128, D], BF16) for i in range(2)]; b_hx2b_ = [Buf(), Buf()]
            x1t_ = [sb(ph, f"x1t{i}", [128, D]) for i in range(2)]; b_x1t_ = [Buf(), Buf()]
            acc = sb(ph, "acc", [128, D]); b_acc = Buf()
            junk = sb(ph, "junk", [128, D], BF16); b_junk = Buf()
            junk2 = [sb(ph, f"junkd{i}", [128, D], BF16) for i in range(2)]; b_junk2 = [Buf(), Buf()]
            NB = 8
            gb = [sb(ph, f"gb{i}", [128, 2 * D], BF16) for i in range(NB)]; b_gb = [Buf() for _ in range(NB)]
            dg = [sb(ph, f"dg{i}", [128, 128], BF16) for i in range(8)]; b_dg = [Buf() for _ in range(8)]
            pacc = [ps(ph, f"pacc{n}", [128, 512]) for n in range(4)]; b_pacc = [PBuf() for _ in range(4)]
            st2 = sb(ph, "st2", [128, 2]); b_st2 = Buf()
            psc = [ps(ph, f"psc{i}", [128, 512]) for i in range(2)]; b_psc = [PBuf(), PBuf()]
            gi = 0
            qDv = qD.rearrange("(j p) t -> p j t", p=128)
            def adv(g_, n):
                if g_ is None:
                    return
                for _ in range(n):
                    try:
                        next(g_)
                    except StopIteration:
                        return

            def topk_gen(tl, pb):
                tsl = slice(tl * 128, (tl + 1) * 128)
                S.dma("sp", qt[:], qDv[:, :, tsl], reads=[b_qD], writes=[b_qt])
                S.dma("sp", hx2_[pb][:], hx2D[tsl, :], reads=[b_hx2D], writes=[b_hx2_[pb]])
                S.op("pool", lambda e: e.tensor_copy(out=hx2b_[pb][:], in_=hx2_[pb][:]), reads=[b_hx2_[pb]], writes=[b_hx2b_[pb]])
                S.dma("sp", x1t_[pb][:], x1D[tsl, :], reads=[b_x1D], writes=[b_x1t_[pb]])
                for g4 in range(4):
                    pi = g4 % 2
                    for u in range(4):
                        hh = g4 * 4 + u
                        S.op("pe", lambda e: e.matmul(psc[pi][:, u * 128:(u + 1) * 128], lhsT=qt[:, hh, :], rhs=kT[:, hh % 2, :], start=True, stop=True), reads=[b_qt, b_kT], writes=[b_psc[pi]])
                    S.op("dve", lambda e: e.tensor_copy(out=sc[:, g4 * 4:(g4 + 1) * 4, :], in_=v3(psc[pi][:, :], 4)), reads=[b_psc[pi]], writes=[b_sc])
                for hh in range(16):
                    S.op("dve", lambda e: e.max(out=v16[:, hh, 0:8], in_=sc[:, hh, :]), reads=[b_sc], writes=[b_v16])
                    yield
                    S.op("dve", lambda e: e.max_index(out=iu[:, 0:8], in_max=v16[:, hh, 0:8], in_values=sc[:, hh, :]), reads=[b_sc, b_v16], writes=[b_iu])
                    S.op("dve", lambda e: e.match_replace(out=sc2[:, 0:128], in_to_replace=v16[:, hh, 0:8], in_values=sc[:, hh, :], imm_value=-1e30), reads=[b_sc, b_v16], writes=[b_sc2])
                    yield
                    S.op("dve", lambda e: e.max(out=v16[:, hh, 8:16], in_=sc2[:, 0:128]), reads=[b_sc2], writes=[b_v16])
                    S.op("dve", lambda e: e.max_index(out=iu[:, 8:16], in_max=v16[:, hh, 8:16], in_values=sc2[:, 0:128]), reads=[b_sc2, b_v16], writes=[b_iu])
                    yield
                    S.op("dve", lambda e: e.tensor_copy(out=i16[:, hh, :], in_=iu[:, :]), reads=[b_iu], writes=[b_v16])
                for h in range(8):
                    S.op("dve", lambda e: e.tensor_tensor(out=cand[:, h, :].rearrange("p (a b) -> p a b", a=16), in0=v16[:, 2 * h, :].unsqueeze(2).to_broadcast([128, 16, 16]), in1=v16[:, 2 * h + 1, :].unsqueeze(1).to_broadcast([128, 16, 16]), op=ALU.add), reads=[b_v16], writes=[b_cand])
                    yield
                    S.op("dve", lambda e: e.scalar_tensor_tensor(out=ecand[:, h, :].rearrange("p (a b) -> p a b", a=16), in0=i16[:, 2 * h, :].unsqueeze(2).to_broadcast([128, 16, 16]), scalar=128.0, in1=i16[:, 2 * h + 1, :].unsqueeze(1).to_broadcast([128, 16, 16]), op0=ALU.mult, op1=ALU.add), reads=[b_v16], writes=[b_ecand])
                    S.op("dve", lambda e: e.max(out=best[:, h, 0:8], in_=cand[:, h, :]), reads=[b_cand], writes=[b_best])
                    yield
                    S.op("dve", lambda e: e.match_replace(out=sc2[:, :], in_to_replace=best[:, h, 0:8], in_values=cand[:, h, :], imm_value=-1e30), reads=[b_cand, b_best], writes=[b_sc2])
                    S.op("dve", lambda e: e.max(out=best[:, h, 8:16], in_=sc2[:, :]), reads=[b_sc2], writes=[b_best])
                    yield
                S.op("dve", lambda e: e.memset(eid_[pb][:], 0.0), writes=[b_eid_[pb]])
                for h in range(8):
                    for n in range(16):
                        S.op("dve", lambda e: e.scalar_tensor_tensor(out=sc2[:, :], in0=cand[:, h, :], scalar=best[:, h, n:n + 1], in1=ecand[:, h, :], op0=ALU.is_equal, op1=ALU.mult, accum_out=eid_[pb][:, h * 16 + n:h * 16 + n + 1]), reads=[b_cand, b_ecand, b_best], writes=[b_sc2, b_eid_[pb]])
                        yield
                S.op("dve", lambda e: e.tensor_scalar_min(out=eid_[pb][:], in0=eid_[pb][:], scalar1=16383.0), reads=[b_eid_[pb]], writes=[b_eid_[pb]])
                S.op("dve", lambda e: e.tensor_copy(out=eidi_[pb][:], in_=eid_[pb][:]), reads=[b_eid_[pb]], writes=[b_eid_[pb]])
                yield
                S.op("dve", lambda e: e.tensor_tensor(out=gate_[pb][:], in0=best[:], in1=best[:, :, 0:1].to_broadcast([128, 8, 16]), op=ALU.subtract), reads=[b_best], writes=[b_gate_[pb]])
                S.op("act", lambda e: e.activation(out=gate_[pb][:], in_=gate_[pb][:], func=AF.Exp), reads=[b_gate_[pb]], writes=[b_gate_[pb]])
                yield
                S.op("dve", lambda e: e.tensor_reduce(out=gs_[pb][:], in_=gate_[pb][:], axis=AX.X, op=ALU.add), reads=[b_gate_[pb]], writes=[b_gate_[pb]])
                S.op("dve", lambda e: e.reciprocal(out=gs_[pb][:], in_=gs_[pb][:]), reads=[b_gate_[pb]], writes=[b_gate_[pb]])
                yield
                S.op("dve", lambda e: e.tensor_tensor(out=gate_[pb][:], in0=gate_[pb][:], in1=gs_[pb][:].unsqueeze(2).to_broadcast([128, 8, 16]), op=ALU.mult), reads=[b_gate_[pb]], writes=[b_gate_[pb]])

                yield

            def gather_fin(tl, pb, nxt):
                nonlocal_gi = gi_box
                tsl = slice(tl * 128, (tl + 1) * 128)
                S.op("pool", lambda e: e.memset(dots[:], 0.0), writes=b_dsl)
                gflat = gate_[pb][:].rearrange("p a b -> p (a b)")
                for s_ in range(128):
                    bi = gi_box[0] % NB
                    gi_box[0] += 1
                    dj = s_ % 8
                    S.idma(out=gb[bi][:], out_offset=None, in_=uvD, in_offset=bass.IndirectOffsetOnAxis(ap=eidi_[pb][:, s_:s_ + 1], axis=0), reads=[b_eid_[pb], b_tab], writes=[b_gb[bi]])
                    S.op("dve", lambda e: e.scalar_tensor_tensor(out=junk2[s_ % 2][:], in0=gb[bi][:, 0:D], scalar=1.0, in1=hx2b_[pb][:], op0=ALU.mult, op1=ALU.mult, accum_out=dots[:, s_:s_ + 1]), reads=[b_gb[bi], b_hx2b_[pb]], writes=[b_junk2[s_ % 2], b_dsl[s_]])
                    adv(nxt, 2)
                    S.op("act", lambda e: e.activation(out=coef[:, s_:s_ + 1], in_=dots[:, s_:s_ + 1], func=AF.Gelu), reads=[b_dsl[s_]], writes=[b_csl[s_]])
                    S.op("act", lambda e: e.activation(out=coef[:, s_:s_ + 1], in_=coef[:, s_:s_ + 1], func=AF.Copy, scale=gflat[:, s_:s_ + 1]), reads=[b_csl[s_], b_gate_[pb]], writes=[b_csl[s_]])
                    S.op("act", lambda e: e.activation(out=dg[dj][:], in_=identf[:], func=AF.Copy, scale=coef[:, s_:s_ + 1]), reads=[b_const, b_csl[s_]], writes=[b_dg[dj]])
                    for n in range(4):
                        S.op("pe", lambda e: e.matmul(pacc[n][:, :], lhsT=dg[dj][:], rhs=gb[bi][:, D + n * 512:D + (n + 1) * 512], start=(s_ == 0), stop=(s_ == 127)), reads=[b_dg[dj], b_gb[bi]], writes=[b_pacc[n]])
                for n in range(4):
                    S.op("dve", lambda e: e.tensor_tensor(out=acc[:, n * 512:(n + 1) * 512], in0=pacc[n][:, :], in1=gfb[:, n * 512:(n + 1) * 512], op=ALU.mult), reads=[b_pacc[n], b_cb], writes=[b_acc])
                S.op("pool", lambda e: e.tensor_tensor(out=acc[:], in0=acc[:], in1=x1t_[pb][:], op=ALU.add), reads=[b_acc, b_x1t_[pb]], writes=[b_acc])
                S.op("dve", lambda e: e.memset(st2[:], 0.0), writes=[b_st2])
                S.op("act", lambda e: e.activation(out=junk[:], in_=acc[:], func=AF.Square, accum_out=st2[:, 0:1]), reads=[b_acc], writes=[b_junk, b_st2])
                S.op("act", lambda e: e.activation(out=st2[:, 1:2], in_=st2[:, 0:1], func=AF.Sqrt, scale=1.0 / D, bias=EPS), reads=[b_st2], writes=[b_st2])
                S.op("dve", lambda e: e.reciprocal(out=st2[:, 1:2], in_=st2[:, 1:2]), reads=[b_st2], writes=[b_st2])
                S.op("dve", lambda e: e.scalar_tensor_tensor(out=acc[:], in0=acc[:], scalar=st2[:, 1:2], in1=nfb[:], op0=ALU.mult, op1=ALU.mult), reads=[b_acc, b_st2, b_cb], writes=[b_acc])
                S.dma("sp", out_d[tsl, :], acc[:], reads=[b_acc], writes=[b_out])

            gi_box = [0]
            g0 = topk_gen(0, 0)
            adv(g0, 100000)
            for tl in range(OWN // 128):
                nxt = topk_gen(tl + 1, (tl + 1) % 2) if tl + 1 < OWN // 128 else None
                gather_fin(tl, tl % 2, nxt)
                adv(nxt, 100000)
            S.barrier()
        S.mark("peer")
        S.finish()
    return nc


_CACHE = {}


def _prep(inputs):
    f = lambda a: np.ascontiguousarray(np.asarray(a, dtype=np.float32))
    x = f(inputs["x"]); ctx = f(inputs["ctx"]); c = f(inputs["c"]); c_ctx = f(inputs["c_ctx"])
    w_in = f(inputs["w_in"])[0]
    w_ada_all = np.ascontiguousarray(f(inputs["w_ada"])[0].reshape(16, 128, 24, 512).transpose(2, 1, 0, 3))
    b_ada_all = f(inputs["b_ada"]).reshape(1, -1)
    nrm = np.stack([f(inputs["norm_mix"])[0], f(inputs["norm_ffn"])[0], f(inputs["norm_final"])])
    rw_conv = f(inputs["rw_conv"])[0].reshape(9, -1)

    def arr_w(cols):
        w = w_in[:, cols]
        n = w.shape[1] // 128
        return np.ascontiguousarray(w.reshape(16, 128, n, 128).transpose(2, 1, 0, 3))

    s_ = np.arange(128)[:, None] % 64; t_ = np.arange(128)[None, :] % 64
    rb = np.arange(128)[:, None] // 64; cb = np.arange(128)[None, :] // 64
    cm = np.zeros((9, 128, 128), np.float32)
    cm[0] = np.where(cb == 0, s_ < t_, s_ <= t_)
    cm[1] = np.where(cb == 0, s_ > t_, s_ >= t_)
    cm[2] = -1.0 * ((rb == cb) & (s_ < t_))
    cm[3] = -1.0 * ((rb == cb) & (s_ > t_))
    cm[4] = -1.0 * ((rb == cb) & (t_ < s_))
    cm[5] = -1.0 * ((rb == cb) & (t_ > s_))
    cm[6] = (rb == cb)
    cm[7] = (s_ <= t_) & (rb == 0) & (cb == 0)
    cm[8] = (s_ >= t_) & (rb == 0) & (cb == 0)
    g_ = lambda n: f(inputs[n])[0]
    rw_w0, rw_a0 = g_("rw_w0"), g_("rw_a0")
    rw_kk, rw_ka, rw_rk = g_("rw_k_k"), g_("rw_k_a"), g_("rw_r_k").reshape(-1)
    rw_lnw, rw_lnb = g_("rw_ln_w"), g_("rw_ln_b")
    wlb_f = g_("rw_w_lora_b").reshape(128, 1024); alb_f = g_("rw_a_lora_b").reshape(128, 1024); glb_f = g_("rw_g_lora_b")
    maps = []
    for core in range(8):
        b, g = core // 4, core % 4
        own = 256 * g + np.arange(256)
        cols = []
        for hp in range(2):
            for base in (0, 1024, 2048):
                cols.append(base + own[hp * 128:(hp + 1) * 128])
        lora = 3072 + np.arange(416)
        cols.append(lora[0:128]); cols.append(lora[128:256]); cols.append(lora[256:384])
        rwcols = np.concatenate(cols + [lora[384:416]])
        hg = []
        for hh in range(2):
            for s in range(5):
                hg.append(3488 + s * 1024 + own[hh * 128:(hh + 1) * 128])
        wfull = np.zeros((2048, 20 * 128), np.float32)
        wfull[:, 0:9 * 128] = w_in[:, np.concatenate(cols)]
        wfull[:, 9 * 128:9 * 128 + 32] = w_in[:, lora[384:416]]
        wfull[:, 10 * 128:] = w_in[:, np.concatenate(hg)]
        wA = np.ascontiguousarray(wfull.reshape(16, 128, 20, 128).transpose(2, 1, 0, 3))
        convw = np.zeros((10, 128, 9), np.float32)
        cc = np.concatenate(cols)
        convw[0:9] = rw_conv[:, cc].T.reshape(9, 128, 9)
        convw[9, 0:32] = rw_conv[:, lora[384:416]].T
        m = {
            "seq": np.concatenate([ctx[b], x[b]], axis=0),
            "xown": x[b, 1024 * g:1024 * (g + 1)],
            "cmod": np.ascontiguousarray(np.stack([c[b], c_ctx], axis=-1).reshape(16, 128, 2).transpose(1, 0, 2)),
            "w_ada": w_ada_all[6 * g:6 * g + 6], "b_ada": np.ascontiguousarray(b_ada_all[:, 3072 * g:3072 * (g + 1)]), "nrm": nrm, "wA": wA,
            "wB": None, "convw": convw, "cmask": cm,
        }
        rwp = np.zeros((2, 128, 9), np.float32)
        for hp in range(2):
            ch = own[hp * 128:(hp + 1) * 128]
            rwp[hp] = np.stack([rw_w0[0, ch], rw_w0[1, ch], rw_a0[0, ch], rw_a0[1, ch], rw_kk[ch], rw_ka[ch], rw_rk[ch], rw_lnw[ch], rw_lnb[ch]], axis=1)
        m["rwp"] = rwp
        hl = f(inputs["hg_lb"])
        m["hlb"] = np.ascontiguousarray(np.stack([np.stack([hl[0, 0, own[hh * 128:(hh + 1) * 128]], hl[0, 1, own[hh * 128:(hh + 1) * 128]], hl[1, 0, own[hh * 128:(hh + 1) * 128]], hl[1, 1, own[hh * 128:(hh + 1) * 128]]], axis=1) for hh in range(2)]))
        m["hgn"] = np.ascontiguousarray(f(inputs["hg_norm"])[0].reshape(128, 1))
        m["wlb"] = np.ascontiguousarray(np.stack([wlb_f[:, own[hp * 128:(hp + 1) * 128]] for hp in range(2)]))
        m["alb"] = np.ascontiguousarray(np.stack([alb_f[:, own[hp * 128:(hp + 1) * 128]] for hp in range(2)]))
        m["glb"] = np.ascontiguousarray(np.stack([glb_f[:, own[hp * 128:(hp + 1) * 128]] for hp in range(2)]))
        maps.append(m)
    wB = arr_w(8608 + np.arange(4096))
    wupr = f(inputs["w_up_rw"])[0]; wuph = f(inputs["w_up_hg"])[0]
    wcat = np.concatenate([wupr, wuph], axis=0)
    wup = np.ascontiguousarray(wcat.reshape(16, 128, 16, 128).transpose(2, 1, 0, 3))
    wo = np.ascontiguousarray(f(inputs["w_out"])[0].reshape(16, 128, 4, 512).transpose(2, 1, 0, 3))
    wq = np.ascontiguousarray(f(inputs["peer_wq"])[0].reshape(16, 128, 16, 128).transpose(2, 1, 0, 3))
    k1T = np.ascontiguousarray(f(inputs["peer_k1"])[0].T); k2T = np.ascontiguousarray(f(inputs["peer_k2"])[0].T)
    pu = f(inputs["peer_u"])[0]; pv = f(inputs["peer_v"])[0]
    for core, m in enumerate(maps):
        m["wB"] = wB
        sq = np.zeros((128, 4), np.float32); sq[:, core % 4] = 1.0
        m.update(selq=sq, wup=wup, wo=wo, wq=wq, k1T=k1T, k2T=k2T, peer_u=pu, peer_v=pv)
    return maps


def kernel(**inputs):
    maps = _prep(inputs)
    nc = build_nc(STAGE)
    res = run_bass_kernel_spmd(nc, maps, core_ids=list(range(8)))
    _CACHE["res"] = res
    out = np.zeros((2, 4096, D), np.float32)
    for core in range(8):
        b, g = core // 4, core % 4
        out[b, 1024 * g:1024 * (g + 1)] = res.results[core]["out"]
    return out
```

```python
import os
import numpy as np
from contextlib import ExitStack
import concourse.bass as bass
import concourse.mybir as mybir
from concourse.bass_utils import run_bass_kernel_spmd

F32 = mybir.dt.float32
BF16 = mybir.dt.bfloat16
I32 = mybir.dt.int32
U32 = mybir.dt.uint32
AF = mybir.ActivationFunctionType
ALU = mybir.AluOpType
AX = mybir.AxisListType

D = 2048
T = 4352
NCH = 68
OWN = 1024
EPS = 1e-6
STAGE = 99
MARKS = []


class Buf:
    __slots__ = ("w", "r", "excl")

    def __init__(self, excl=False):
        self.w = None
        self.r = {}
        self.excl = excl


def PBuf():
    return Buf(True)


class Sch:
    def __init__(self, nc, es):
        self.nc = nc
        self.eng = {"pe": nc.tensor, "act": nc.scalar, "dve": nc.vector, "pool": nc.gpsimd, "sp": nc.sync}
        self.sem = {}
        self.cnt = {}
        self.NDS = 16
        names = ["pe", "act", "dve", "pool", "cc"] + [f"d_{q}_{i}" for q in ("sp", "act", "pool") for i in range(self.NDS)]
        for p in names:
            self.sem[p] = es.enter_context(nc.semaphore("s_" + p))
            self.cnt[p] = 0
        self.rr = {"sp": 0, "act": 0, "pool": 0}
        self.pe_pos = (0, 0)
        self.seen = {e: {} for e in self.eng}
        self.ninst = 0

    def _deps(self, reads, writes, e=None):
        deps = {}
        for b in reads:
            if b.w is not None and deps.get(b.w[0], 0) < b.w[1]:
                deps[b.w[0]] = b.w[1]
            if b.excl:
                for p, c in b.r.items():
                    if p != e and deps.get(p, 0) < c:
                        deps[p] = c
        for b in writes:
            if b.w is not None and deps.get(b.w[0], 0) < b.w[1]:
                deps[b.w[0]] = b.w[1]
            for p, c in b.r.items():
                if deps.get(p, 0) < c:
                    deps[p] = c
        return deps

    def _wait(self, e, deps):
        eng = self.eng[e]
        seen = self.seen[e]
        for p, c in deps.items():
            if p == "pe" and e == "pe":
                continue
            if seen.get(p, 0) >= c:
                continue
            eng.wait_ge(self.sem[p], c)
            seen[p] = c

    def _mark(self, prod, reads, writes):
        c = self.cnt[prod]
        for b in reads:
            if b.r.get(prod, 0) < c:
                b.r[prod] = c
        for b in writes:
            b.w = (prod, c)
            b.r = {}

    def op(self, e, fn, reads=(), writes=(), pos=(0, 0)):
        if e == "pe":
            if pos != self.pe_pos and self.cnt["pe"] > 0 and self.seen["pe"].get("pe_self", 0) < self.cnt["pe"]:
                self.eng["pe"].wait_ge(self.sem["pe"], self.cnt["pe"])
                self.seen["pe"]["pe_self"] = self.cnt["pe"]
            self.pe_pos = pos
        self._wait(e, self._deps(reads, writes, e))
        inst = fn(self.eng[e])
        self.cnt[e] += 1
        inst.then_inc(self.sem[e], 1)
        self._mark(e, reads, writes)
        self.ninst += 1
        return inst

    def _dsem(self, q):
        i = self.rr[q]
        self.rr[q] = (i + 1) % self.NDS
        prod = f"d_{q}_{i}"
        if self.cnt[prod] > 0:
            self._wait(q, {prod: self.cnt[prod]})
        return prod

    def dma(self, q, out, in_, reads=(), writes=(), **kw):
        prod = self._dsem(q)
        self._wait(q, self._deps(reads, writes))
        inst = self.eng[q].dma_start(out=out, in_=in_, **kw)
        self.cnt[prod] += 16
        inst.then_inc(self.sem[prod], 16)
        self._mark(prod, reads, writes)
        self.ninst += 1
        return inst

    def idma(self, reads=(), writes=(), **kw):
        prod = self._dsem("pool")
        self._wait("pool", self._deps(reads, writes))
        inst = self.eng["pool"].indirect_dma_start(**kw)
        self.cnt[prod] += 16
        inst.then_inc(self.sem[prod], 16)
        self._mark(prod, reads, writes)
        self.ninst += 1
        return inst

    def mark(self, name):
        MARKS.append((name, dict(self.cnt)))

    def barrier(self):
        deps = {p: c for p, c in self.cnt.items() if c > 0}
        for e in self.eng:
            self._wait(e, deps)

    def finish(self):
        self.barrier()


def build_nc(stage=99):
    nc = bass.Bass("TRN2", target_bir_lowering=False)

    def din(name, shape, dt=F32):
        return nc.dram_tensor(name, list(shape), dt, kind="ExternalInput").ap()

    seq = din("seq", [T, D])
    xown = din("xown", [OWN, D])
    cmod = din("cmod", [128, 16, 2])
    w_ada = din("w_ada", [6, 128, 16, 512])
    b_ada = din("b_ada", [1, 3072])
    mod_in = nc.dram_tensor("mod_in", [2, 3072], F32).ap()
    mod_out = nc.dram_tensor("mod_out", [8, 3072], F32).ap()
    nrm = din("nrm", [3, D])
    wA = din("wA", [20, 128, 16, 128])
    wB = din("wB", [32, 128, 16, 128])
    convw = din("convw", [10, 128, 9])
    cmask = din("cmask", [9, 128, 128])
    rwp = din("rwp", [2, 128, 9])
    wlb = din("wlb", [2, 128, 128])
    alb = din("alb", [2, 128, 128])
    glb = din("glb", [2, 160, 128])
    hlb = din("hlb", [2, 128, 4])
    hgn = din("hgn", [128, 1])
    selq = din("selq", [128, 4])
    wup = din("wup", [16, 128, 16, 128])
    wo = din("wo", [4, 128, 16, 512])
    wq = din("wq", [16, 128, 16, 128])
    k1T = din("k1T", [128, 128])
    k2T = din("k2T", [128, 128])
    peer_u = din("peer_u", [16384, D])
    peer_v = din("peer_v", [16384, D])
    SIMTAIL = bool(os.environ.get("K_SIMTAIL"))
    if SIMTAIL:
        ag_ref = din("ag_ref", [2048, 4096])
    out_d = nc.dram_tensor("out", [OWN, D], F32, kind="ExternalOutput").ap()
    dbg = nc.dram_tensor("dbg", [128, 8192], F32, kind="ExternalOutput").ap() if stage < 99 else None
    modD = nc.dram_tensor("modD", [2, 6 * D], F32).ap()
    zT = nc.dram_tensor("zT", [20 * 128, T], F32).ap()
    gT = nc.dram_tensor("gT", [32 * 128, OWN], F32).ap()
    ag_in_rw = [nc.dram_tensor(f"ag_in_rw{p}", [256, 512], F32).ap() for p in range(8)]
    ag_out_rw = [nc.dram_tensor(f"ag_out_rw{p}", [1024, 512], F32).ap() for p in range(8)]
    ag_in_h = [[nc.dram_tensor(f"ag_in_h{hh}_{p}", [128, 512], F32).ap() for p in range(8)] for hh in range(2)]
    ag_out_h = [[nc.dram_tensor(f"ag_out_h{hh}_{p}", [512, 512], F32).ap() for p in range(8)] for hh in range(2)]
    x1D = nc.dram_tensor("x1D", [OWN, D], F32).ap()
    hx2D = nc.dram_tensor("hx2D", [OWN, D], F32).ap()
    qD = nc.dram_tensor("qD", [D, OWN], F32).ap()
    uvD = nc.dram_tensor("uvD", [16384, 2 * D], BF16).ap()

    top = ExitStack()
    with top:
        S = Sch(nc, top)

        uid = [0]

        def sb(es, name, shape, dt=F32):
            uid[0] += 1
            return es.enter_context(nc.sbuf_tensor(f"{name}_{uid[0]}", list(shape), dt))

        def ps(es, name, shape, dt=F32):
            uid[0] += 1
            return es.enter_context(nc.psum_tensor(f"{name}_{uid[0]}", list(shape), dt))

        qs = ["sp", "act"]
        qi = [0]

        def nq():
            qi[0] += 1
            return qs[qi[0] % 2]

        identf = sb(top, "identf", [128, 128]); b_const = Buf()
        identb = sb(top, "identb", [128, 128], BF16)
        S.op("pool", lambda e: e.memset(identf[:], 0.0), writes=[b_const])
        S.op("pool", lambda e: e.affine_select(out=identf[:], in_=identf[:], pattern=[[-1, 128]], compare_op=ALU.not_equal, fill=1.0, base=0, channel_multiplier=1), reads=[b_const], writes=[b_const])
        S.op("dve", lambda e: e.tensor_copy(out=identb[:], in_=identf[:]), reads=[b_const], writes=[b_const])

        with ExitStack() as ph:
            cm = sb(ph, "cm", [128, 16, 2]); b_cm = Buf()
            cmb = sb(ph, "cmb", [128, 16, 2], BF16)
            ones2 = sb(ph, "ones2", [1, 2])
            wst = [sb(ph, f"wada{i}", [128, 16, 512]) for i in range(2)]; b_wst = [Buf(), Buf()]
            wsb = [sb(ph, f"wadab{i}", [128, 16, 512], BF16) for i in range(2)]; b_wsb = [Buf(), Buf()]
            brow = [sb(ph, f"brow{i}", [1, 512]) for i in range(2)]; b_brow = [Buf(), Buf()]
            mrow = [sb(ph, f"mrow{i}", [2, 512]) for i in range(2)]; b_mrow = [Buf(), Buf()]
            pm = [ps(ph, f"pm{i}", [2, 512]) for i in range(2)]; b_pm = [PBuf(), PBuf()]
            b_modD = Buf()
            S.dma("sp", cm[:], cmod, writes=[b_cm])
            S.op("act", lambda e: e.activation(out=cm[:], in_=cm[:], func=AF.Silu), reads=[b_cm], writes=[b_cm])
            S.op("dve", lambda e: e.memset(ones2[:], 1.0), writes=[b_cm])
            S.op("dve", lambda e: e.tensor_copy(out=cmb[:], in_=cm[:]), reads=[b_cm], writes=[b_cm])
            b_modin = Buf()
            for ch in range(6):
                i = ch % 2
                S.dma("sp" if ch % 2 == 0 else "act", wst[i][:], w_ada[ch], writes=[b_wst[i]])
                S.dma("sp", brow[i][:], b_ada[0:1, ch * 512:(ch + 1) * 512], writes=[b_brow[i]])
                S.op("dve", lambda e: e.tensor_copy(out=wsb[i][:, 0:8, :], in_=wst[i][:, 0:8, :]), reads=[b_wst[i]], writes=[b_wsb[i]])
                S.op("pool", lambda e: e.tensor_copy(out=wsb[i][:, 8:16, :], in_=wst[i][:, 8:16, :]), reads=[b_wst[i]], writes=[b_wsb[i]])
                for k in range(16):
                    S.op("pe", lambda e: e.matmul(pm[i][:, :], lhsT=cmb[:, k, :], rhs=wsb[i][:, k, :], start=(k == 0), stop=False), reads=[b_cm, b_wsb[i]], writes=[b_pm[i]])
                S.op("pe", lambda e: e.matmul(pm[i][:, :], lhsT=ones2[0:1, :], rhs=brow[i][0:1, :], start=False, stop=True), reads=[b_cm, b_brow[i]], writes=[b_pm[i]])
                S.op("act", lambda e: e.activation(out=mrow[i][:], in_=pm[i][:], func=AF.Copy), reads=[b_pm[i]], writes=[b_mrow[i]])
                S.dma("sp", mod_in[:, ch * 512:(ch + 1) * 512], mrow[i][:], reads=[b_mrow[i]], writes=[b_modin])
            b_modout = Buf()
            S._wait("pool", S._deps([b_modin], [b_modout], "pool"))
            nc.gpsimd.collective_compute("AllGather", ALU.bypass, replica_groups=[[0, 1, 2, 3], [4, 5, 6, 7]], ins=[mod_in], outs=[mod_out]).then_inc(S.sem["cc"], 1)
            S.cnt["cc"] += 1
            b_modout.w = ("cc", S.cnt["cc"])
            for r in range(4):
                S.dma("sp", modD[:, 3072 * r:3072 * (r + 1)], mod_out[2 * r:2 * r + 2, :], reads=[b_modout], writes=[b_modD])
            S.barrier()

        S.mark("p0_adaLN")

        def bc_load(q, dst, src_row, bufs_w, reads=()):
            S.dma(q, dst, src_row.to_broadcast([128, src_row.shape[1]]), reads=list(reads), writes=bufs_w)

        def make_AB(ph, row, nrow, sc_off, sh_off, tag):
            A = sb(ph, "A" + tag, [128, D]); Bt = sb(ph, "B" + tag, [128, D]); bA = Buf(); bB = Buf()
            bc_load("sp", A[:], modD[row:row + 1, sc_off:sc_off + D], [bA], reads=[b_modD])
            bc_load("act", Bt[:], nrm[nrow:nrow + 1, :], [bB])
            S.op("dve", lambda e: e.scalar_tensor_tensor(out=A[:], in0=A[:], scalar=1.0, in1=Bt[:], op0=ALU.add, op1=ALU.mult), reads=[bA, bB], writes=[bA])
            bc_load("sp", Bt[:], modD[row:row + 1, sh_off:sh_off + D], [bB], reads=[b_modD, bA])
            return A, Bt, bA, bB

        def norm_tiles(ph, src, ntiles, ABsel, dstT, tag, src_buf=None, store=None):
            xt = [sb(ph, f"xt{tag}{i}", [128, D]) for i in range(2)]; b_xt = [Buf(), Buf()]
            hb = [sb(ph, f"hb{tag}{i}", [128, D], BF16) for i in range(2)]; b_hb = [Buf(), Buf()]
            st = [sb(ph, f"st{tag}{i}", [128, 2]) for i in range(2)]; b_st = [Buf(), Buf()]
            pT = [ps(ph, f"pT{tag}{i}", [128, 512], BF16) for i in range(2)]; b_pT = [PBuf(), PBuf()]
            b_dst = Buf()

            def tile_gen(tl):
                i = tl % 2
                A, Bt, bA, bB = ABsel(tl)
                S.dma("sp", xt[i][:], src[tl * 128:(tl + 1) * 128, :], reads=[src_buf] if src_buf is not None else [], writes=[b_xt[i]])
                S.op("dve", lambda e: e.memset(st[i][:], 0.0), writes=[b_st[i]])
                S.op("act", lambda e: e.activation(out=hb[i][:], in_=xt[i][:], func=AF.Square, accum_out=st[i][:, 0:1]), reads=[b_xt[i]], writes=[b_hb[i], b_st[i]])
                yield
                S.op("act", lambda e: e.activation(out=st[i][:, 1:2], in_=st[i][:, 0:1], func=AF.Sqrt, scale=1.0 / D, bias=EPS), reads=[b_st[i]], writes=[b_st[i]])
                yield
                S.op("dve", lambda e: e.reciprocal(out=st[i][:, 1:2], in_=st[i][:, 1:2]), reads=[b_st[i]], writes=[b_st[i]])
                S.op("dve", lambda e: e.scalar_tensor_tensor(out=xt[i][:], in0=xt[i][:], scalar=st[i][:, 1:2], in1=A[:], op0=ALU.mult, op1=ALU.mult), reads=[b_xt[i], b_st[i], bA], writes=[b_xt[i]])
                yield
                if store is None:
                    S.op("pool", lambda e: e.tensor_tensor(out=hb[i][:], in0=xt[i][:], in1=Bt[:], op=ALU.add), reads=[b_xt[i], bB], writes=[b_hb[i]])
                else:
                    S.op("pool", lambda e: e.tensor_tensor(out=xt[i][:], in0=xt[i][:], in1=Bt[:], op=ALU.add), reads=[b_xt[i], bB], writes=[b_xt[i]])
                    S.dma("act", store[0][tl * 128:(tl + 1) * 128, :], xt[i][:], reads=[b_xt[i]], writes=[store[1]])
                    S.op("pool", lambda e: e.tensor_copy(out=hb[i][:], in_=xt[i][:]), reads=[b_xt[i]], writes=[b_hb[i]])
                yield
                for kq in range(4):
                    j = kq % 2
                    for kk in range(4):
                        k = kq * 4 + kk
                        S.op("pe", lambda e: e.transpose(out=pT[j][:, kk * 128:(kk + 1) * 128], in_=hb[i][:, k * 128:(k + 1) * 128], identity=identb[:]), reads=[b_hb[i], b_const], writes=[b_pT[j]])
                    S.op("act" if kq % 2 == 0 else "dve", lambda e: (e.activation(out=dstT[:, kq * 4:(kq + 1) * 4, tl * 128:(tl + 1) * 128], in_=pT[j][:, :].rearrange("p (a b) -> p a b", a=4), func=AF.Copy) if kq % 2 == 0 else e.tensor_copy(out=dstT[:, kq * 4:(kq + 1) * 4, tl * 128:(tl + 1) * 128], in_=pT[j][:, :].rearrange("p (a b) -> p a b", a=4))), reads=[b_pT[j]], writes=[b_dst])
                    yield

            for t0_ in range(0, ntiles, 2):
                gens = [tile_gen(t_) for t_ in range(t0_, min(t0_ + 2, ntiles))]
                while gens:
                    for g_ in list(gens):
                        try:
                            next(g_)
                        except StopIteration:
                            gens.remove(g_)
            return b_dst

        def project(ph, wsrc, nchunks, hT, b_hT, ntok, dstD, b_dstD, tag):
            wf = [sb(ph, f"wf{tag}{i}", [128, 16, 128]) for i in range(3)]; b_wf = [Buf() for _ in range(3)]
            wb = [sb(ph, f"wb{tag}{i}", [128, 16, 128], BF16) for i in range(3)]; b_wb = [Buf() for _ in range(3)]
            ze = [sb(ph, f"ze{tag}{i}", [128, 512]) for i in range(3)]; b_ze = [Buf() for _ in range(3)]
            pz = [ps(ph, f"pz{tag}{i}", [128, 512]) for i in range(3)]; b_pz = [PBuf() for _ in range(3)]
            it = 0
            for j in range(nchunks):
                i = j % 3
                S.dma("sp", wf[i][:], wsrc[j], writes=[b_wf[i]])
                S.op("pool" if j % 2 else "dve", lambda e: e.tensor_copy(out=wb[i][:], in_=wf[i][:]), reads=[b_wf[i]], writes=[b_wb[i]])
                for t0 in range(0, ntok, 512):
                    n = min(512, ntok - t0)
                    pi = it % 3
                    it += 1
                    for k in range(16):
                        S.op("pe", lambda e: e.matmul(pz[pi][:, 0:n], lhsT=wb[i][:, k, :], rhs=hT[:, k, t0:t0 + n], start=(k == 0), stop=(k == 15)), reads=[b_wb[i], b_hT], writes=[b_pz[pi]])
                    S.op("act", lambda e: e.activation(out=ze[pi][:, 0:n], in_=pz[pi][:, 0:n], func=AF.Copy), reads=[b_pz[pi]], writes=[b_ze[pi]])
                    S.dma("act", dstD[j * 128:(j + 1) * 128, t0:t0 + n], ze[pi][:, 0:n], reads=[b_ze[pi]], writes=[b_dstD])

        b_zT = Buf(); b_gT = Buf()
        with ExitStack() as ph:
            hTo = sb(ph, "hTo", [128, 16, OWN], BF16)
            with ExitStack() as ph2:
                A, Bt, bA, bB = make_AB(ph2, 0, 0, 1 * D, 0 * D, "o")
                b_hTo = norm_tiles(ph2, xown, 8, lambda tl: (A, Bt, bA, bB), hTo, "o")
                S.barrier()
            S.mark("p1b_norm_own")
            project(ph, wB, 32, hTo, b_hTo, OWN, gT, b_gT, "g")
            S.barrier()
            S.mark("p2b_gate_proj")
        with ExitStack() as ph:
          if not SIMTAIL:
            hT = sb(ph, "hT", [128, 16, T], BF16)
            with ExitStack() as ph2:
                Ax, Bx, bAx, bBx = make_AB(ph2, 0, 0, 1 * D, 0 * D, "x")
                Ac, Bc, bAc, bBc = make_AB(ph2, 1, 0, 1 * D, 0 * D, "c")
                b_hT = norm_tiles(ph2, seq, 34, lambda tl: (Ac, Bc, bAc, bBc) if tl < 2 else (Ax, Bx, bAx, bBx), hT, "a")
                S.barrier()
            S.mark("p1_norm_all")
            project(ph, wA, 20, hT, b_hT, T, zT, b_zT, "z")
            S.barrier()
            S.mark("p2_head_proj")
        with ExitStack() as ph:
            PADW = 65
            zr = [sb(ph, f"zr{i}", [128, T]) for i in range(2)]; b_zr = [Buf(), Buf()]
            zc = [sb(ph, f"zc{i}", [128, T]) for i in range(2)]; b_zc = [Buf(), Buf()]
            cw = [sb(ph, f"cw{i}", [128, 9]) for i in range(2)]; b_cw = [Buf(), Buf()]
            zp = [[sb(ph, f"zp{i}{v}", [128, 4096 + 2 * PADW], BF16) for v in range(3)] for i in range(2)]; b_zp = [[Buf() for _ in range(3)] for _ in range(2)]
            dgc = [[sb(ph, f"dgc{i}{t_}", [128, 128], BF16) for t_ in range(9)] for i in range(2)]; b_dgc = [Buf(), Buf()]
            pcv = [ps(ph, f"pcv{i}", [128, 512]) for i in range(2)]; b_pcv = [PBuf(), PBuf()]
            for i in range(2):
                for v in range(3):
                    S.op("pool", lambda e: e.memset(zp[i][v][:, 0:PADW], 0.0), writes=[b_zp[i][v]])
                    S.op("pool", lambda e: e.memset(zp[i][v][:, PADW + 4096:], 0.0), writes=[b_zp[i][v]])
            it = 0
            for j in range(0 if SIMTAIL else 10):
                i = j % 2
                S.dma("sp", zr[i][:], zT[j * 128:(j + 1) * 128, :], reads=[b_zT], writes=[b_zr[i]])
                S.dma("sp", cw[i][:], convw[j], writes=[b_cw[i]])
                xmid = [zp[i][v][:, PADW:PADW + 4096] for v in range(3)]
                S.op("act", lambda e: e.activation(out=xmid[0], in_=zr[i][:, 256:T], func=AF.Copy), reads=[b_zr[i]], writes=[b_zp[i][0]])
                S.op("dve", lambda e: e.tensor_copy(out=xmid[1], in_=xmid[0]), reads=[b_zp[i][0]], writes=[b_zp[i][1]])
                S.op("dve", lambda e: e.tensor_copy(out=xmid[2], in_=xmid[0]), reads=[b_zp[i][0]], writes=[b_zp[i][2]])
                S.op("dve", lambda e: e.memset(xmid[1].rearrange("p (r c) -> p r c", c=64)[:, :, 63:64], 0.0), reads=[b_zp[i][1]], writes=[b_zp[i][1]])
                S.op("dve", lambda e: e.memset(xmid[2].rearrange("p (r c) -> p r c", c=64)[:, :, 0:1], 0.0), reads=[b_zp[i][2]], writes=[b_zp[i][2]])
                for tap in range(9):
                    S.op("act", lambda e: e.activation(out=dgc[i][tap][:], in_=identf[:], func=AF.Copy, scale=cw[i][:, tap:tap + 1]), reads=[b_const, b_cw[i]], writes=[b_dgc[i]])
                for tb in range(8):
                    pi = it % 2
                    it += 1
                    n_ = 0
                    for dy in (-1, 0, 1):
                        for dx in (-1, 0, 1):
                            tap = (dy + 1) * 3 + (dx + 1)
                            v = {0: 0, -1: 1, 1: 2}[dx]
                            s0 = PADW + tb * 512 + 64 * dy + dx
                            S.op("pe", lambda e: e.matmul(pcv[pi][:, :], lhsT=dgc[i][tap][:], rhs=zp[i][v][:, s0:s0 + 512], start=(n_ == 0), stop=(n_ == 8)), reads=[b_dgc[i], b_zp[i][v]], writes=[b_pcv[pi]])
                            n_ += 1
                    if pi == 0:
                        S.op("act", lambda e: e.activation(out=zc[i][:, 256 + tb * 512:256 + (tb + 1) * 512], in_=pcv[pi][:, :], func=AF.Copy), reads=[b_pcv[pi]], writes=[b_zc[i]])
                    else:
                        S.op("dve", lambda e: e.tensor_copy(out=zc[i][:, 256 + tb * 512:256 + (tb + 1) * 512], in_=pcv[pi][:, :]), reads=[b_pcv[pi]], writes=[b_zc[i]])
                S.op("dve", lambda e: e.tensor_scalar(out=zc[i][:, 0:256], in0=zr[i][:, 0:256], scalar1=cw[i][:, 4:5], scalar2=None, op0=ALU.mult), reads=[b_zr[i], b_cw[i]], writes=[b_zc[i]])
                S.op("dve", lambda e: e.scalar_tensor_tensor(out=zc[i][:, 1:256], in0=zr[i][:, 0:255], scalar=cw[i][:, 3:4], in1=zc[i][:, 1:256], op0=ALU.mult, op1=ALU.add), reads=[b_zr[i], b_cw[i], b_zc[i]], writes=[b_zc[i]])
                S.op("dve", lambda e: e.scalar_tensor_tensor(out=zc[i][:, 0:255], in0=zr[i][:, 1:256], scalar=cw[i][:, 5:6], in1=zc[i][:, 0:255], op0=ALU.mult, op1=ALU.add), reads=[b_zr[i], b_cw[i], b_zc[i]], writes=[b_zc[i]])
                S.dma("act", zT[j * 128:(j + 1) * 128, :], zc[i][:], reads=[b_zc[i], b_zr[i]], writes=[b_zT])
            S.barrier()

        S.mark("p2c_conv")
        if stage <= 2:
            with ExitStack() as ph:
                dt_ = sb(ph, "dbgt", [128, 8192]); bd = Buf()
                S.dma("sp", dt_[:, 0:T], zT[0:128, :], reads=[b_zT], writes=[bd])
                S.dma("sp", dt_[:, T:T + 1024], gT[0:128, :], reads=[b_gT], writes=[bd])
                S.dma("sp", dt_[:, 5376:5376 + 2048], zT[10 * 128:11 * 128, 0:2048], reads=[b_zT], writes=[bd])
                S.dma("sp", dt_[0:2, 7424:7424 + 512], modD[:, 0:512], reads=[b_modD], writes=[bd])
                S.dma("sp", dbg, dt_[:], reads=[bd])
                S.finish()
            return nc
        C0 = float(np.exp(-0.5))
        b_agin = Buf()
        msk = sb(top, "msk", [128, 9, 128]); b_msk = Buf()
        S.dma("sp", msk[:], cmask.rearrange("m p q -> p m q"), writes=[b_msk])
        bdones = msk[:, 6, :]

        def run(gens):
            gens = list(gens)
            while gens:
                for g_ in list(gens):
                    try:
                        next(g_)
                    except StopIteration:
                        gens.remove(g_)

        order = [list(range(NCH)), [3, 2, 1, 0] + list(range(NCH - 1, 3, -1))]

        b_tab = Buf()
        cst = {"i": 0}

        def cbufs(es):
            return ([sb(es, f"cf{i}", [128, D]) for i in range(2)], [sb(es, f"cb{i}", [128, D], BF16) for i in range(2)], [Buf(), Buf()], [Buf(), Buf()])

        def conv_tiles(cb_, n):
            cf, cb, b_cf, b_cb = cb_
            for _ in range(n):
                t = cst["i"]
                if t >= 256:
                    return
                cst["i"] += 1
                src, c0 = (peer_u, 0) if t < 128 else (peer_v, D)
                i = t % 2
                rs = slice((t % 128) * 128, (t % 128) * 128 + 128)
                S.dma("sp", cf[i][:], src[rs, :], writes=[b_cf[i]])
                S.op("pool", lambda e: e.tensor_copy(out=cb[i][:], in_=cf[i][:]), reads=[b_cf[i]], writes=[b_cb[i]])
                S.dma("pool", uvD[rs, c0:c0 + D], cb[i][:], reads=[b_cb[i]], writes=[b_tab])

        def v3(ap, a):
            return ap.rearrange("p (a b) -> p a b", a=a)

        def rwkv_hp(hp):
            with ExitStack() as php:
                BK = [sb(php, f"BK{d}", [128, NCH, 128], BF16) for d in range(2)]
                AR = [sb(php, f"AR{d}", [128, NCH, 128], BF16) for d in range(2)]
                gam = [sb(php, f"gam{d}", [128, NCH]) for d in range(2)]
                vTb = sb(php, "vTb", [128, T], BF16)
                bonT = sb(php, "bonT", [128, 4096], BF16); ggT = sb(php, "ggT", [128, 4096], BF16)
                Oacc = sb(php, "Oacc", [64, 64, 128])
                prm = sb(php, "prm", [128, 12]); wl = sb(php, "wl", [128, 128]); al = sb(php, "al", [128, 128])
                gl0 = sb(php, "gl0", [128, 128]); gl1 = sb(php, "gl1", [32, 128])
                b_str = Buf(); b_O = Buf(); b_prm = Buf(); b_bon = Buf()
                S.dma("sp", prm[:, 0:9], rwp[hp], writes=[b_prm])
                S.dma("act", wl[:], wlb[hp], writes=[b_prm])
                S.dma("sp", al[:], alb[hp], writes=[b_prm])
                S.dma("act", gl0[:], glb[hp, 0:128, :], writes=[b_prm])
                S.dma("sp", gl1[:], glb[hp, 128:160, :], writes=[b_prm])
                S.op("dve", lambda e: e.tensor_scalar(out=prm[:, 9:10], in0=prm[:, 5:6], scalar1=-1.0, scalar2=1.0, op0=ALU.mult, op1=ALU.add), reads=[b_prm], writes=[b_prm])
                S.op("pool", lambda e: e.memset(Oacc[:], 0.0), writes=[b_O])
                with ExitStack() as ph:
                    TB = 256
                    tl = {}

                    cur_par = [0]

                    def tt(name, p=128, dt=F32):
                        key = (name, cur_par[0])
                        if key not in tl:
                            tl[key] = (sb(ph, "s_" + name, [p, TB], dt), Buf())
                        return tl[key]
                    pAs = [ps(ph, f"pA{i}", [128, 512]) for i in range(2)]; pBs = [ps(ph, f"pB{i}", [128, 512]) for i in range(2)]
                    pCs = [ps(ph, f"pC{i}", [128, 512]) for i in range(2)]; pDs = [ps(ph, f"pD{i}", [128, 512]) for i in range(2)]
                    b_pAs = [PBuf(), PBuf()]; b_pBs = [PBuf(), PBuf()]; b_pCs = [PBuf(), PBuf()]; b_pDs = [PBuf(), PBuf()]
                    def blkgen(blk):
                        t0 = blk * TB
                        c0 = blk * 4
                        cur_par[0] = blk % 2
                        pA, pB, pC, pD = pAs[blk % 2], pBs[blk % 2], pCs[blk % 2], pDs[blk % 2]
                        b_pA, b_pB, b_pC, b_pD = b_pAs[blk % 2], b_pBs[blk % 2], b_pCs[blk % 2], b_pDs[blk % 2]
                        b_pC2 = b_pC
                        rows = [3 * hp, 3 * hp + 1, 3 * hp + 2, 6, 7, 8, 9]
                        nm = ["r", "k", "v", "L0", "L1", "L2", "L3"]
                        for rr, n_ in zip(rows, nm):
                            t_, b_ = tt(n_)
                            S.dma("sp", t_[:], zT[rr * 128:(rr + 1) * 128, t0:t0 + TB], reads=[b_zT], writes=[b_])
                        (r_, b_r), (k_, b_k), (v_, b_v) = tt("r"), tt("k"), tt("v")
                        (L0, b_L0), (L1, b_L1), (L2, b_L2), (L3, b_L3) = tt("L0"), tt("L1"), tt("L2"), tt("L3")
                        yield
                        cur_par[0] = blk % 2
                        th, b_th = tt("th")
                        S.op("act", lambda e: e.activation(out=th[:], in_=L0[:], func=AF.Tanh), reads=[b_L0], writes=[b_th])
                        for d in range(2):
                            S.op("pe", lambda e: e.matmul(pA[:, d * 256:(d + 1) * 256], lhsT=wl[64 * d:64 * d + 64, :], rhs=th[64 * d:64 * d + 64, :], start=True, stop=True), reads=[b_prm, b_th], writes=[b_pA], pos=(64 * d, 0))
                            S.op("pe", lambda e: e.matmul(pB[:, d * 256:(d + 1) * 256], lhsT=al[64 * d:64 * d + 64, :], rhs=L1[64 * d:64 * d + 64, :], start=True, stop=True), reads=[b_prm, b_L1], writes=[b_pB], pos=(64 * d, 0))
                        s2, b_s2 = tt("s2"); s3, b_s3 = tt("s3")
                        S.op("act", lambda e: e.activation(out=s2[:], in_=L2[:], func=AF.Sigmoid), reads=[b_L2], writes=[b_s2])
                        S.op("act", lambda e: e.activation(out=s3[0:32, :], in_=L3[0:32, :], func=AF.Sigmoid), reads=[b_L3], writes=[b_s3])
                        S.op("pe", lambda e: e.matmul(pC[:, 0:256], lhsT=gl0[:, :], rhs=s2[:, :], start=True, stop=False), reads=[b_prm, b_s2], writes=[b_pC])
                        S.op("pe", lambda e: e.matmul(pC[:, 0:256], lhsT=gl1[0:32, :], rhs=s3[0:32, :], start=False, stop=True), reads=[b_prm, b_s3], writes=[b_pC])
                        yield
                        cur_par[0] = blk % 2
                        sg = []; aa = []
                        for d in range(2):
                            sgd, b_sgd = tt(f"sg{d}"); ad, b_ad = tt(f"a{d}")
                            S.op("act", lambda e: e.activation(out=sgd[:], in_=pA[:, d * 256:(d + 1) * 256], func=AF.Sigmoid, bias=prm[:, d:d + 1]), reads=[b_pA, b_prm], writes=[b_sgd])
                            S.op("act", lambda e: e.activation(out=ad[:], in_=pB[:, d * 256:(d + 1) * 256], func=AF.Sigmoid, bias=prm[:, 2 + d:3 + d]), reads=[b_pB, b_prm], writes=[b_ad])
                            sg.append((sgd, b_sgd)); aa.append((ad, b_ad))
                        yield
                        cur_par[0] = blk % 2
                        kkr, b_kkr = tt("kkr"); sq, b_sq = tt("sq"); nr, b_nr = tt("nr"); kk, b_kk = tt("kk")
                        S.op("dve", lambda e: e.tensor_scalar(out=kkr[:], in0=k_[:], scalar1=prm[:, 4:5], scalar2=None, op0=ALU.mult), reads=[b_k, b_prm], writes=[b_kkr])
                        S.op("pool", lambda e: e.tensor_tensor(out=sq[:], in0=kkr[:], in1=kkr[:], op=ALU.mult), reads=[b_kkr], writes=[b_sq])
                        S.op("pe", lambda e: e.matmul(pC[:, 256:512], lhsT=bdones, rhs=sq[:, :], start=True, stop=True), reads=[b_msk, b_sq], writes=[b_pC2])
                        S.op("act", lambda e: e.activation(out=nr[:], in_=pC[:, 256:512], func=AF.Sqrt), reads=[b_pC2], writes=[b_nr])
                        S.op("dve", lambda e: e.tensor_scalar_max(out=nr[:], in0=nr[:], scalar1=1e-12), reads=[b_nr], writes=[b_nr])
                        S.op("dve", lambda e: e.reciprocal(out=nr[:], in_=nr[:]), reads=[b_nr], writes=[b_nr])
                        S.op("pool", lambda e: e.tensor_tensor(out=kk[:], in0=kkr[:], in1=nr[:], op=ALU.mult), reads=[b_kkr, b_nr], writes=[b_kk])
                        yield
                        cur_par[0] = blk % 2
                        kd = []; kka = []
                        for d in range(2):
                            ad, b_ad = aa[d]
                            tm, b_tm = tt("tm"); kdd, b_kdd = tt(f"kd{d}"); kkad, b_kkad = tt(f"kka{d}")
                            S.op("dve", lambda e: e.tensor_scalar(out=tm[:], in0=ad[:], scalar1=prm[:, 5:6], scalar2=prm[:, 9:10], op0=ALU.mult, op1=ALU.add), reads=[b_ad, b_prm], writes=[b_tm])
                            S.op("pool", lambda e: e.tensor_tensor(out=kdd[:], in0=tm[:], in1=k_[:], op=ALU.mult), reads=[b_tm, b_k], writes=[b_kdd])
                            S.op("pool", lambda e: e.tensor_tensor(out=kkad[:], in0=kk[:], in1=ad[:], op=ALU.mult), reads=[b_kk, b_ad], writes=[b_kkad])
                            kd.append((kdd, b_kdd)); kka.append((kkad, b_kkad))
                        yield
                        cur_par[0] = blk % 2
                        for d in range(2):
                            sgd, b_sgd = sg[d]
                            cs, b_cs = tt(f"cs{d}"); ce, b_ce = tt(f"ce{d}"); ci, b_ci = tt(f"ci{d}")
                            for c in range(4):
                                S.op("dve", lambda e: e.tensor_tensor_scan(out=cs[:, c * 64:(c + 1) * 64], data0=sgd[:, c * 64:(c + 1) * 64], data1=sgd[:, c * 64:(c + 1) * 64], initial=0.0, op0=ALU.add, op1=ALU.bypass), reads=[b_sgd], writes=[b_cs])
                            if d == 0:
                                S.op("pool", lambda e: e.tensor_tensor(out=ce[:], in0=cs[:], in1=sgd[:], op=ALU.subtract), reads=[b_cs, b_sgd], writes=[b_ce])
                                ciu, b_ciu = cs, b_cs
                            else:
                                S.op("dve", lambda e: e.tensor_tensor(out=v3(ce[:, :], 4), in0=v3(cs[:, :], 4)[:, :, 63:64].to_broadcast([128, 4, 64]), in1=v3(cs[:, :], 4), op=ALU.subtract), reads=[b_cs], writes=[b_ce])
                                S.op("pool", lambda e: e.tensor_tensor(out=ci[:], in0=ce[:], in1=sgd[:], op=ALU.add), reads=[b_ce, b_sgd], writes=[b_ci])
                                ciu, b_ciu = ci, b_ci
                            Ein, b_Ein = tt("Ein"); Eex, b_Eex = tt("Eex"); Einv, b_Einv = tt("Einv")
                            S.op("act", lambda e: e.activation(out=Ein[:], in_=ciu[:], func=AF.Exp, scale=-C0), reads=[b_ciu], writes=[b_Ein])
                            S.op("act", lambda e: e.activation(out=Eex[:], in_=ce[:], func=AF.Exp, scale=-C0), reads=[b_ce], writes=[b_Eex])
                            S.op("act", lambda e: e.activation(out=Einv[:], in_=ciu[:], func=AF.Exp, scale=C0), reads=[b_ciu], writes=[b_Einv])
                            S.op("act", lambda e: e.activation(out=gam[d][:, c0:c0 + 4], in_=v3(cs[:, :], 4)[:, :, 63], func=AF.Exp, scale=-C0), reads=[b_cs], writes=[b_str])
                            kdd, b_kdd = kd[d]; kkad, b_kkad = kka[d]
                            S.op("dve", lambda e: e.tensor_tensor(out=AR[d][:, c0:c0 + 4, 0:64], in0=v3(kk[:, :], 4), in1=v3(Eex[:, :], 4), op=ALU.mult), reads=[b_kk, b_Eex], writes=[b_str])
                            S.op("pool", lambda e: e.tensor_tensor(out=AR[d][:, c0:c0 + 4, 64:128], in0=v3(r_[:, :], 4), in1=v3(Ein[:, :], 4), op=ALU.mult), reads=[b_r, b_Ein], writes=[b_str])
                            S.op("dve", lambda e: e.scalar_tensor_tensor(out=BK[d][:, c0:c0 + 4, 0:64], in0=v3(kkad[:, :], 4), scalar=-1.0, in1=v3(Einv[:, :], 4), op0=ALU.mult, op1=ALU.mult), reads=[b_kkad, b_Einv], writes=[b_str])
                            S.op("pool", lambda e: e.tensor_tensor(out=BK[d][:, c0:c0 + 4, 64:128], in0=v3(kdd[:, :], 4), in1=v3(Einv[:, :], 4), op=ALU.mult), reads=[b_kdd, b_Einv], writes=[b_str])
                        yield
                        cur_par[0] = blk % 2
                        S.op("act", lambda e: e.activation(out=vTb[:, t0:t0 + TB], in_=v_[:], func=AF.Copy), reads=[b_v], writes=[b_str])
                        if blk >= 1:
                            tx0 = t0 - 256
                            rk, b_rk = tt("rk"); kds, b_kds = tt("kds")
                            S.op("dve", lambda e: e.tensor_scalar(out=rk[:], in0=r_[:], scalar1=prm[:, 6:7], scalar2=None, op0=ALU.mult), reads=[b_r, b_prm], writes=[b_rk])
                            S.op("pool", lambda e: e.tensor_tensor(out=kds[:], in0=kd[0][0][:], in1=kd[1][0][:], op=ALU.add), reads=[kd[0][1], kd[1][1]], writes=[b_kds])
                            S.op("pool", lambda e: e.tensor_tensor(out=kds[:], in0=kds[:], in1=rk[:], op=ALU.mult), reads=[b_kds, b_rk], writes=[b_kds])
                            S.op("pe", lambda e: e.matmul(pD[:, 0:256], lhsT=bdones, rhs=kds[:, :], start=True, stop=True), reads=[b_msk, b_kds], writes=[b_pD])
                            S.op("dve", lambda e: e.tensor_tensor(out=bonT[:, tx0:tx0 + TB], in0=pD[:, 0:256], in1=v_[:], op=ALU.mult), reads=[b_pD, b_v], writes=[b_bon])
                            S.op("act", lambda e: e.activation(out=ggT[:, tx0:tx0 + TB], in_=pC[:, 0:256], func=AF.Copy), reads=[b_pC], writes=[b_bon])
                    for b0 in range(0, T // TB, 2):
                        run([blkgen(b_) for b_ in range(b0, min(b0 + 2, T // TB))])
                    S.barrier()
                S.mark(f"rw{hp}_streams")
                if os.environ.get("K_PHASE") == "streams":
                    return
                with ExitStack() as ph:
                    bank = [[ps(ph, f"bk{d}{i}", [128, 512]) for i in range(3)] for d in range(2)]
                    bankb = [ps(ph, f"bkb{d}", [128, 1024], BF16) for d in range(2)]
                    PM = [v3(bank[d][0][:, 0:256], 2) for d in range(2)]
                    PX = [bank[d][0][:, 256:384] for d in range(2)]
                    PY = [bank[d][0][:, 384:512] for d in range(2)]
                    PN = [[bank[d][1][:, 128 * i:128 * (i + 1)] for i in range(3)] for d in range(2)]
                    PW = [bank[d][2][:, 256:320] for d in range(2)]
                    PS_ = [bank[d][2][:, 320:384] for d in range(2)]
                    PU = [v3(bank[d][2][0:64, 0:128], 2) for d in range(2)]
                    PO = [v3(bank[d][2][0:64, 128:256], 2) for d in range(2)]
                    PVt = [v3(bankb[d][:, 0:128], 2) for d in range(2)]
                    PBt = [v3(bankb[d][:, 128:256], 2) for d in range(2)]
                    nb = lambda: Buf()
                    b_bank = [[PBuf() for _ in range(4)] for _ in range(2)]
                    b_PM = [b_bank[d][0] for d in range(2)]; b_PX = b_PM; b_PY = b_PM
                    b_PN = [[b_bank[d][1]] * 3 for d in range(2)]
                    b_PW = [b_bank[d][2] for d in range(2)]; b_PS = b_PW; b_PU = b_PW; b_PO = b_PW
                    b_PVt = [b_bank[d][3] for d in range(2)]; b_PBt = b_PVt
                    MS = [[sb(ph, f"MS{d}{p}", [128, 2, 128], BF16) for p in range(2)] for d in range(2)]; b_MS = [[nb(), nb()] for _ in range(2)]
                    Xs = [[sb(ph, f"Xs{d}{p}", [128, 128], BF16) for p in range(2)] for d in range(2)]; b_Xs = [[nb(), nb()] for _ in range(2)]
                    Ys = [[sb(ph, f"Ys{d}{p}", [128, 128], BF16) for p in range(2)] for d in range(2)]; b_Ys = [[nb(), nb()] for _ in range(2)]
                    Rs = [[sb(ph, f"Rs{d}{p}", [128, 128], BF16) for p in range(2)] for d in range(2)]; b_Rs = [[nb(), nb()] for _ in range(2)]
                    Yp = [sb(ph, f"Yp{d}", [128, 128]) for d in range(2)]; b_Yp = [nb(), nb()]
                    TT = [[sb(ph, f"TT{d}{p}", [128, 128], BF16) for p in range(2)] for d in range(2)]; b_TT = [[nb(), nb()] for _ in range(2)]
                    UV = [[[sb(ph, f"UV{d}{p}{j}", [128, 64], BF16) for j in range(2)] for p in range(2)] for d in range(2)]
                    b_UV = [[[nb(), nb()] for _ in range(2)] for _ in range(2)]
                    BKt = [[sb(ph, f"BKt{d}{p}", [128, 2, 64], BF16) for p in range(2)] for d in range(2)]; b_BKt = [[nb(), nb()] for _ in range(2)]
                    W0 = [sb(ph, f"W0{d}", [128, 64], BF16) for d in range(2)]; b_W0 = [nb(), nb()]
                    ST = [[sb(ph, f"ST{d}{p}", [128, 64], BF16) for p in range(2)] for d in range(2)]; b_ST = [[nb(), nb()] for _ in range(2)]
                    cvb = cbufs(ph)
                    for d in range(2):
                        S.op("dve", lambda e: e.memset(bank[d][0][:, 256:512], 0.0), writes=[b_PX[d]])
                        S.op("pool", lambda e: e.memset(ST[d][0][:], 0.0), writes=[b_ST[d][0]])
                        for p in range(2):
                            for j in range(2):
                                S.op("pool", lambda e: e.memset(UV[d][p][j][:], 0.0), writes=[b_UV[d][p][j]])

                    def prep(d, i):
                        c = order[d][i]; par = i % 2
                        ARc = AR[d][:, c, :]; BKc = BK[d][:, c, :]
                        for j in range(2):
                            p0 = 64 * j
                            S.op("pe", lambda e: e.matmul(PM[d][:, j, :], lhsT=BKc[p0:p0 + 64, :], rhs=ARc[p0:p0 + 64, :], start=True, stop=True), reads=[b_str], writes=[b_PM[d]], pos=(p0, 0))
                            S.op("pe", lambda e: e.matmul(PX[d][p0:p0 + 64, p0:p0 + 64], lhsT=BKc[p0:p0 + 64, 0:64], rhs=ARc[p0:p0 + 64, 0:64], start=True, stop=True), reads=[b_str], writes=[b_PX[d]], pos=(p0, p0))
                            S.op("pe", lambda e: e.matmul(PY[d][p0:p0 + 64, p0:p0 + 64], lhsT=ARc[p0:p0 + 64, 0:64], rhs=BKc[p0:p0 + 64, 0:64], start=True, stop=True), reads=[b_str], writes=[b_PY[d]], pos=(p0, p0))
                            S.op("pe", lambda e: e.transpose(out=PVt[d][64:128, j, :], in_=vTb[p0:p0 + 64, c * 64:(c + 1) * 64], identity=identb[p0:p0 + 64, p0:p0 + 64]), reads=[b_str, b_const], writes=[b_PVt[d]], pos=(p0, 64))
                            S.op("pe", lambda e: e.transpose(out=PBt[d][:, j, :], in_=BKc[p0:p0 + 64, :], identity=identb[p0:p0 + 64, p0:p0 + 64]), reads=[b_str, b_const], writes=[b_PBt[d]], pos=(p0, 0))
                        yield
                        S.op("dve", lambda e: e.tensor_tensor(out=MS[d][par][:], in0=PM[d], in1=msk[:, d, :].unsqueeze(1).to_broadcast([128, 2, 128]), op=ALU.mult), reads=[b_PM[d], b_msk], writes=[b_MS[d][par]])
                        S.op("dve", lambda e: e.tensor_tensor(out=Xs[d][0][:], in0=PX[d], in1=msk[:, 2 + d, :], op=ALU.mult), reads=[b_PX[d], b_msk], writes=[b_Xs[d][0]])
                        S.op("dve", lambda e: e.tensor_tensor(out=Ys[d][0][:], in0=PY[d], in1=msk[:, 4 + d, :], op=ALU.mult), reads=[b_PY[d], b_msk], writes=[b_Ys[d][0]])
                        S.op("pool", lambda e: e.tensor_tensor(out=Rs[d][0][:], in0=identb[:], in1=Xs[d][0][:], op=ALU.subtract), reads=[b_Xs[d][0], b_const], writes=[b_Rs[d][0]])
                        for j in range(2):
                            S.op("act", lambda e: e.activation(out=UV[d][par][j][64:128, :], in_=PVt[d][64:128, j, :], func=AF.Copy), reads=[b_PVt[d]], writes=[b_UV[d][par][j]])
                        S.op("act", lambda e: e.activation(out=BKt[d][par][:], in_=PBt[d], func=AF.Copy), reads=[b_PBt[d]], writes=[b_BKt[d][par]])
                        yield
                        xi = 0; ri = 0
                        for lvl in range(1, 7):
                            if lvl <= 4:
                                S.op("pe", lambda e: e.matmul(PN[d][0], lhsT=Ys[d][xi][:], rhs=Xs[d][xi][:], start=True, stop=True), reads=[b_Ys[d][xi], b_Xs[d][xi]], writes=[b_PN[d][0]])
                            if lvl <= 5:
                                S.op("pe", lambda e: e.matmul(PN[d][1], lhsT=Xs[d][xi][:], rhs=Ys[d][xi][:], start=True, stop=True), reads=[b_Ys[d][xi], b_Xs[d][xi]], writes=[b_PN[d][1]])
                            if lvl >= 2:
                                S.op("pe", lambda e: e.matmul(PN[d][2], lhsT=identb[:], rhs=Rs[d][ri][:], start=True, stop=False), reads=[b_const, b_Rs[d][ri]], writes=[b_PN[d][2]])
                                S.op("pe", lambda e: e.matmul(PN[d][2], lhsT=Ys[d][xi][:], rhs=Rs[d][ri][:], start=False, stop=True), reads=[b_Ys[d][xi], b_Rs[d][ri]], writes=[b_PN[d][2]])
                            yield
                            if lvl <= 4:
                                S.op("act", lambda e: e.activation(out=Xs[d][1 - xi][:], in_=PN[d][0], func=AF.Copy), reads=[b_PN[d][0]], writes=[b_Xs[d][1 - xi]])
                            if lvl <= 5:
                                S.op("act", lambda e: e.activation(out=Ys[d][1 - xi][:], in_=PN[d][1], func=AF.Copy), reads=[b_PN[d][1]], writes=[b_Ys[d][1 - xi]])
                            if lvl >= 2:
                                if lvl == 6:
                                    S.op("act", lambda e: e.activation(out=TT[d][par][:], in_=PN[d][2], func=AF.Copy), reads=[b_PN[d][2]], writes=[b_TT[d][par]])
                                else:
                                    S.op("act", lambda e: e.activation(out=Rs[d][1 - ri][:], in_=PN[d][2], func=AF.Copy), reads=[b_PN[d][2]], writes=[b_Rs[d][1 - ri]])
                                ri = 1 - ri
                            xi = 1 - xi
                            yield

                    def step(d, i):
                        c = order[d][i]; par = i % 2; cur = i % 2; nxt = 1 - cur
                        isx = c >= 4; xc = c - 4
                        ARc = AR[d][:, c, :]
                        for j in range(2):
                            p0 = 64 * j
                            S.op("pe", lambda e: e.matmul(PW[d][p0:p0 + 64, :], lhsT=ARc[p0:p0 + 64, 0:64], rhs=ST[d][cur][p0:p0 + 64, :], start=True, stop=False), reads=[b_str, b_ST[d][cur]], writes=[b_PW[d]], pos=(p0, p0))
                            S.op("pe", lambda e: e.matmul(PW[d][p0:p0 + 64, :], lhsT=MS[d][par][64:128, j, 0:64], rhs=UV[d][par][j][64:128, :], start=False, stop=True), reads=[b_MS[d][par], b_UV[d][par][j]], writes=[b_PW[d]], pos=(64, p0))
                        S.op("dve", lambda e: e.tensor_copy(out=W0[d][:], in_=PW[d]), reads=[b_PW[d]], writes=[b_W0[d]])
                        yield
                        for j in range(2):
                            p0 = 64 * j
                            S.op("pe", lambda e: e.matmul(PU[d][:, j, :], lhsT=TT[d][par][p0:p0 + 64, p0:p0 + 64], rhs=W0[d][p0:p0 + 64, :], start=True, stop=True), reads=[b_TT[d][par], b_W0[d]], writes=[b_PU[d]], pos=(p0, 0))
                        for j in range(2):
                            S.op("dve", lambda e: e.tensor_copy(out=UV[d][par][j][0:64, :], in_=PU[d][:, j, :]), reads=[b_PU[d]], writes=[b_UV[d][par][j]])
                        yield
                        if isx:
                            for j in range(2):
                                p0 = 64 * j
                                S.op("pe", lambda e: e.matmul(PO[d][:, j, :], lhsT=ARc[p0:p0 + 64, 64:128], rhs=ST[d][cur][p0:p0 + 64, :], start=True, stop=False), reads=[b_str, b_ST[d][cur]], writes=[b_PO[d]], pos=(p0, 0))
                                S.op("pe", lambda e: e.matmul(PO[d][:, j, :], lhsT=MS[d][par][:, j, 64:128], rhs=UV[d][par][j][:, :], start=False, stop=True), reads=[b_MS[d][par], b_UV[d][par][j]], writes=[b_PO[d]])
                            S.op("dve", lambda e: e.tensor_tensor(out=v3(Oacc[:, xc, :], 2), in0=PO[d], in1=v3(Oacc[:, xc, :], 2), op=ALU.add), reads=[b_PO[d], b_O], writes=[b_O])
                        for j in range(2):
                            p0 = 64 * j
                            S.op("pe", lambda e: e.matmul(PS_[d][p0:p0 + 64, :], lhsT=identb[p0:p0 + 64, p0:p0 + 64], rhs=ST[d][cur][p0:p0 + 64, :], start=True, stop=False), reads=[b_const, b_ST[d][cur]], writes=[b_PS[d]], pos=(p0, p0))
                            S.op("pe", lambda e: e.matmul(PS_[d][p0:p0 + 64, :], lhsT=BKt[d][par][:, j, :], rhs=UV[d][par][j][:, :], start=False, stop=True), reads=[b_BKt[d][par], b_UV[d][par][j]], writes=[b_PS[d]], pos=(0, p0))
                        S.op("dve", lambda e: e.tensor_scalar(out=ST[d][nxt][:], in0=PS_[d], scalar1=gam[d][:, c:c + 1], scalar2=None, op0=ALU.mult), reads=[b_PS[d], b_str], writes=[b_ST[d][nxt]])
                        yield

                    if os.environ.get("K_PHASE") == "prep":
                        ns = int(os.environ.get("K_NS", 99))
                        for g_ in (prep(0, 0), prep(1, 0)):
                            for _ in range(ns):
                                try:
                                    next(g_)
                                except StopIteration:
                                    break
                        S.barrier()
                        return
                    if os.environ.get("K_PHASE") != "noprep":
                        run([prep(0, 0), prep(1, 0)])
                    for i in range(int(os.environ.get("K_STEPS", NCH))):
                        gs = [step(0, i), step(1, i)]
                        if i + 1 < NCH:
                            gs += [prep(0, i + 1), prep(1, i + 1)]
                        run(gs)
                        conv_tiles(cvb, 2)
                    S.barrier()
                S.mark(f"rw{hp}_scan")
                with ExitStack() as ph:
                    sm = sb(ph, "sm", [64, 128]); sm2 = sb(ph, "sm2", [64, 128]); b_sm = Buf()
                    sqb = sb(ph, "sqb", [64, 64, 128]); b_sqb = Buf()
                    onT = sb(ph, "onT", [128, 4096]); b_onT = Buf()
                    pR = [ps(ph, f"pR{i}", [128, 512]) for i in range(2)]; b_pR = [PBuf(), PBuf()]
                    O3 = Oacc[:].rearrange("p a (j v) -> p (a j) v", j=2)
                    S.op("dve", lambda e: e.tensor_reduce(out=sm[:], in_=O3, axis=AX.X, op=ALU.add), reads=[b_O], writes=[b_sm])
                    S.op("pool", lambda e: e.tensor_tensor(out=sqb[:], in0=Oacc[:], in1=Oacc[:], op=ALU.mult), reads=[b_O], writes=[b_sqb])
                    S.op("dve", lambda e: e.tensor_reduce(out=sm2[:], in_=sqb[:].rearrange("p a (j v) -> p (a j) v", j=2), axis=AX.X, op=ALU.add), reads=[b_sqb], writes=[b_sm])
                    S.op("dve", lambda e: e.tensor_scalar(out=sm[:], in0=sm[:], scalar1=1.0 / 64, scalar2=None, op0=ALU.mult), reads=[b_sm], writes=[b_sm])
                    S.op("dve", lambda e: e.scalar_tensor_tensor(out=sm2[:], in0=sm2[:], scalar=1.0 / 64, in1=sm2[:], op0=ALU.mult, op1=ALU.bypass), reads=[b_sm], writes=[b_sm])
                    mu2 = sb(ph, "mu2", [64, 128])
                    S.op("dve", lambda e: e.tensor_tensor(out=mu2[:], in0=sm[:], in1=sm[:], op=ALU.mult), reads=[b_sm], writes=[b_sm])
                    S.op("dve", lambda e: e.tensor_tensor(out=sm2[:], in0=sm2[:], in1=mu2[:], op=ALU.subtract), reads=[b_sm], writes=[b_sm])
                    S.op("act", lambda e: e.activation(out=sm2[:], in_=sm2[:], func=AF.Sqrt, bias=64e-5), reads=[b_sm], writes=[b_sm])
                    S.op("dve", lambda e: e.reciprocal(out=sm2[:], in_=sm2[:]), reads=[b_sm], writes=[b_sm])
                    S.op("dve", lambda e: e.tensor_tensor(out=O3, in0=O3, in1=sm[:].unsqueeze(2).to_broadcast([64, 128, 64]), op=ALU.subtract), reads=[b_O, b_sm], writes=[b_O])
                    S.op("dve", lambda e: e.tensor_tensor(out=O3, in0=O3, in1=sm2[:].unsqueeze(2).to_broadcast([64, 128, 64]), op=ALU.mult), reads=[b_O, b_sm], writes=[b_O])
                    for g8 in range(8):
                        pi = g8 % 2
                        for q in range(8):
                            xc = g8 * 8 + q
                            S.op("pe", lambda e: e.transpose(out=pR[pi][:, q * 64:(q + 1) * 64], in_=Oacc[:, xc, :], identity=identf[0:64, 0:64]), reads=[b_O, b_const], writes=[b_pR[pi]])
                        S.op("act", lambda e: e.activation(out=onT[:, g8 * 512:(g8 + 1) * 512], in_=pR[pi][:, :], func=AF.Copy), reads=[b_pR[pi]], writes=[b_onT])
                    S.op("dve", lambda e: e.tensor_scalar(out=onT[:], in0=onT[:], scalar1=prm[:, 7:8], scalar2=prm[:, 8:9], op0=ALU.mult, op1=ALU.add), reads=[b_onT, b_prm], writes=[b_onT])
                    S.op("pool", lambda e: e.tensor_tensor(out=onT[:], in0=onT[:], in1=bonT[:], op=ALU.add), reads=[b_onT, b_bon], writes=[b_onT])
                    S.op("dve", lambda e: e.tensor_tensor(out=onT[:], in0=onT[:], in1=ggT[:], op=ALU.mult), reads=[b_onT, b_bon], writes=[b_onT])
                    for p in range(8):
                        S.dma(nq(), ag_in_rw[p][hp * 128:(hp + 1) * 128, :], onT[:, 512 * p:512 * (p + 1)], reads=[b_onT], writes=[b_agin])
                    S.barrier()

        b_agin_h = [Buf(), Buf()]
        b_agout_rw = Buf(); b_agout_h = [Buf(), Buf()]

        def gather(ins_, outs_, b_in, b_out):
            S._wait("pool", S._deps([b_in], [b_out], "pool"))
            for p in range(8):
                nc.gpsimd.collective_compute("AllGather", ALU.bypass, replica_groups=[[0, 1, 2, 3], [4, 5, 6, 7]], ins=[ins_[p]], outs=[outs_[p]]).then_inc(S.sem["cc"], 1)
                S.cnt["cc"] += 1
            b_out.w = ("cc", S.cnt["cc"])
            b_out.r = {}

        for hp in range(0 if SIMTAIL else int(os.environ.get("K_HP", 2))):
            rwkv_hp(hp)
            S.mark(f"rw{hp}_readout")
        if not SIMTAIL:
            gather(ag_in_rw, ag_out_rw, b_agin, b_agout_rw)
        def hgrn_head(hh):
            with ExitStack() as php:
                QD = [sb(php, f"QD{d}", [128, T], BF16) for d in range(2)]
                KD = [sb(php, f"KD{d}", [128, T], BF16) for d in range(2)]
                QG = [sb(php, f"QG{d}", [128, T], BF16) for d in range(2)]
                KL = [sb(php, f"KL{d}", [128, T], BF16) for d in range(2)]
                iTb = sb(php, "iTb", [128, T], BF16)
                gmh = [sb(php, f"gmh{d}", [128, NCH]) for d in range(2)]
                sog = sb(php, "sog", [128, 4096])
                Oh = sb(php, "Oh", [64, 64, 128])
                hpr = sb(php, "hpr", [128, 12])
                b_str = Buf(); b_O = Buf(); b_hp = Buf(); b_sog = Buf()
                S.dma("sp", hpr[:, 0:4], hlb[hh], writes=[b_hp])
                S.dma("act", hpr[:, 4:5], hgn, writes=[b_hp])
                S.op("dve", lambda e: e.tensor_tensor(out=hpr[:, 5:7], in0=hpr[:, 0:2], in1=hpr[:, 2:4], op=ALU.subtract), reads=[b_hp], writes=[b_hp])
                S.op("act", lambda e: e.activation(out=hpr[:, 5:7], in_=hpr[:, 5:7], func=AF.Sigmoid), reads=[b_hp], writes=[b_hp])
                S.op("dve", lambda e: e.tensor_scalar(out=hpr[:, 7:9], in0=hpr[:, 5:7], scalar1=-1.0, scalar2=1.0, op0=ALU.mult, op1=ALU.add), reads=[b_hp], writes=[b_hp])
                S.op("pool", lambda e: e.memset(Oh[:], 0.0), writes=[b_O])
                with ExitStack() as ph:
                    TB = 256
                    tl = {}

                    cur_par = [0]

                    def tt(name, p=128, dt=F32):
                        key = (name, cur_par[0])
                        if key not in tl:
                            tl[key] = (sb(ph, "h_" + name, [p, TB], dt), Buf())
                        return tl[key]
                    def blkgen(blk):
                        t0 = blk * TB
                        c0 = blk * 4
                        cur_par[0] = blk % 2
                        for s_i, n_ in enumerate(["q", "ff", "fb", "i", "og"]):
                            t_, b_ = tt(n_)
                            rr = 10 + 5 * hh + s_i
                            S.dma("sp", t_[:], zT[rr * 128:(rr + 1) * 128, t0:t0 + TB], reads=[b_zT], writes=[b_])
                        (q_, b_q), (i_, b_i), (og_, b_og) = tt("q"), tt("i"), tt("og")
                        yield
                        cur_par[0] = blk % 2
                        qs, b_qs = tt("qs")
                        S.op("act", lambda e: e.activation(out=qs[:], in_=q_[:], func=AF.Silu), reads=[b_q], writes=[b_qs])
                        if blk >= 1:
                            S.op("act", lambda e: e.activation(out=sog[:, t0 - 256:t0 - 256 + TB], in_=og_[:], func=AF.Silu), reads=[b_og], writes=[b_sog])
                        S.op("pool", lambda e: e.tensor_copy(out=iTb[:, t0:t0 + TB], in_=i_[:]), reads=[b_i], writes=[b_str])
                        for d in range(2):
                            yield
                            cur_par[0] = blk % 2
                            f_, b_f = tt("ff" if d == 0 else "fb")
                            sgf, b_sgf = tt("sgf"); fg, b_fg = tt("fg"); lf, b_lf = tt("lf"); kk_, b_kk = tt("kk_")
                            cs, b_cs = tt("cs"); ce, b_ce = tt("ce"); ci2, b_ci2 = tt("ci2")
                            S.op("act", lambda e: e.activation(out=sgf[:], in_=f_[:], func=AF.Sigmoid), reads=[b_f], writes=[b_sgf])
                            S.op("dve", lambda e: e.tensor_scalar(out=fg[:], in0=sgf[:], scalar1=hpr[:, 7 + d:8 + d], scalar2=hpr[:, 5 + d:6 + d], op0=ALU.mult, op1=ALU.add), reads=[b_sgf, b_hp], writes=[b_fg])
                            S.op("act", lambda e: e.activation(out=lf[:], in_=fg[:], func=AF.Ln), reads=[b_fg], writes=[b_lf])
                            S.op("pool", lambda e: e.tensor_scalar(out=kk_[:], in0=fg[:], scalar1=-1.0, scalar2=1.0, op0=ALU.mult, op1=ALU.add), reads=[b_fg], writes=[b_kk])
                            for c in range(4):
                                S.op("dve", lambda e: e.tensor_tensor_scan(out=cs[:, c * 64:(c + 1) * 64], data0=lf[:, c * 64:(c + 1) * 64], data1=lf[:, c * 64:(c + 1) * 64], initial=0.0, op0=ALU.add, op1=ALU.bypass), reads=[b_lf], writes=[b_cs])
                            if d == 0:
                                ci, b_ci = cs, b_cs
                                iref, ilast = 32, 63
                            else:
                                S.op("dve", lambda e: e.tensor_tensor(out=v3(ce[:, :], 4), in0=v3(cs[:, :], 4)[:, :, 63:64].to_broadcast([128, 4, 64]), in1=v3(cs[:, :], 4), op=ALU.subtract), reads=[b_cs], writes=[b_ce])
                                S.op("pool", lambda e: e.tensor_tensor(out=ci2[:], in0=ce[:], in1=lf[:], op=ALU.add), reads=[b_ce, b_lf], writes=[b_ci2])
                                ci, b_ci = ci2, b_ci2
                                iref, ilast = 31, 0
                            dr, b_dr = tt("dr"); dl, b_dl = tt("dl")
                            E1, b_E1 = tt("E1"); E2, b_E2 = tt("E2"); E3, b_E3 = tt("E3"); E4, b_E4 = tt("E4")
                            S.op("dve", lambda e: e.tensor_tensor(out=v3(dr[:, :], 4), in0=v3(ci[:, :], 4), in1=v3(ci[:, :], 4)[:, :, iref:iref + 1].to_broadcast([128, 4, 64]), op=ALU.subtract), reads=[b_ci], writes=[b_dr])
                            S.op("dve", lambda e: e.tensor_tensor(out=v3(dl[:, :], 4), in0=v3(ci[:, :], 4), in1=v3(ci[:, :], 4)[:, :, ilast:ilast + 1].to_broadcast([128, 4, 64]), op=ALU.subtract), reads=[b_ci], writes=[b_dl])
                            S.op("act", lambda e: e.activation(out=E1[:], in_=dr[:], func=AF.Exp), reads=[b_dr], writes=[b_E1])
                            S.op("act", lambda e: e.activation(out=E2[:], in_=dr[:], func=AF.Exp, scale=-1.0), reads=[b_dr], writes=[b_E2])
                            S.op("act", lambda e: e.activation(out=E3[:], in_=ci[:], func=AF.Exp), reads=[b_ci], writes=[b_E3])
                            S.op("act", lambda e: e.activation(out=E4[:], in_=dl[:], func=AF.Exp, scale=-1.0), reads=[b_dl], writes=[b_E4])
                            S.op("act", lambda e: e.activation(out=gmh[d][:, c0:c0 + 4], in_=v3(ci[:, :], 4)[:, :, ilast], func=AF.Exp), reads=[b_ci], writes=[b_str])
                            S.op("dve", lambda e: e.tensor_tensor(out=QD[d][:, t0:t0 + TB], in0=qs[:], in1=E1[:], op=ALU.mult), reads=[b_qs, b_E1], writes=[b_str])
                            S.op("pool", lambda e: e.tensor_tensor(out=KD[d][:, t0:t0 + TB], in0=kk_[:], in1=E2[:], op=ALU.mult), reads=[b_kk, b_E2], writes=[b_str])
                            S.op("dve", lambda e: e.tensor_tensor(out=QG[d][:, t0:t0 + TB], in0=qs[:], in1=E3[:], op=ALU.mult), reads=[b_qs, b_E3], writes=[b_str])
                            S.op("pool", lambda e: e.tensor_tensor(out=KL[d][:, t0:t0 + TB], in0=kk_[:], in1=E4[:], op=ALU.mult), reads=[b_kk, b_E4], writes=[b_str])
                    for b0 in range(0, T // TB, 2):
                        run([blkgen(b_) for b_ in range(b0, min(b0 + 2, T // TB))])
                    S.barrier()
                with ExitStack() as ph:
                    bA = [ps(ph, f"hbA{d}", [128, 512]) for d in range(2)]
                    bB = [ps(ph, f"hbB{d}", [128, 512]) for d in range(2)]
                    bC = [ps(ph, f"hbC{d}", [128, 1024], BF16) for d in range(2)]
                    b_bA = [PBuf(), PBuf()]; b_bB = [PBuf(), PBuf()]; b_bC = [PBuf(), PBuf()]
                    PSc = [bA[d][0:64, 0:64] for d in range(2)]
                    PO = [bA[d][0:64, 64:192] for d in range(2)]
                    PSn = [bB[d][:, 0:128] for d in range(2)]
                    PVt = [bC[d][0:64, 0:128] for d in range(2)]
                    PKt = [bC[d][0:64, 128:256] for d in range(2)]
                    S32 = [sb(ph, f"S32{d}", [128, 128]) for d in range(2)]; b_S32 = [Buf(), Buf()]
                    Sb = [[sb(ph, f"Sb{d}{p}", [128, 128], BF16) for p in range(2)] for d in range(2)]; b_Sb = [[Buf(), Buf()] for _ in range(2)]
                    Msc = [[sb(ph, f"Msc{d}{p}", [64, 64], BF16) for p in range(2)] for d in range(2)]; b_Msc = [[Buf(), Buf()] for _ in range(2)]
                    Vt = [[sb(ph, f"Vt{d}{p}", [64, 128], BF16) for p in range(2)] for d in range(2)]; b_Vt = [[Buf(), Buf()] for _ in range(2)]
                    KLt = [[sb(ph, f"KLt{d}{p}", [64, 128], BF16) for p in range(2)] for d in range(2)]; b_KLt = [[Buf(), Buf()] for _ in range(2)]
                    cvb = cbufs(ph)
                    for d in range(2):
                        S.op("dve", lambda e: e.memset(S32[d][:], 0.0), writes=[b_S32[d]])
                        S.op("pool", lambda e: e.memset(Sb[d][0][:], 0.0), writes=[b_Sb[d][0]])

                    def prep(d, i):
                        c = order[d][i]; par = i % 2
                        sl = slice(c * 64, (c + 1) * 64)
                        S.op("pe", lambda e: e.matmul(PSc[d], lhsT=KD[d][:, sl], rhs=QD[d][:, sl], start=True, stop=True), reads=[b_str], writes=[b_bA[d]])
                        S.op("pe", lambda e: e.transpose(out=PVt[d], in_=iTb[:, sl], identity=identb[:]), reads=[b_str, b_const], writes=[b_bC[d]])
                        S.op("pe", lambda e: e.transpose(out=PKt[d], in_=KL[d][:, sl], identity=identb[:]), reads=[b_str, b_const], writes=[b_bC[d]])
                        yield
                        S.op("dve", lambda e: e.tensor_tensor(out=Msc[d][par][:], in0=PSc[d], in1=msk[0:64, 7 + d, 0:64], op=ALU.mult), reads=[b_bA[d], b_msk], writes=[b_Msc[d][par]])
                        S.op("act", lambda e: e.activation(out=Vt[d][par][:], in_=PVt[d], func=AF.Copy), reads=[b_bC[d]], writes=[b_Vt[d][par]])
                        S.op("act", lambda e: e.activation(out=KLt[d][par][:], in_=PKt[d], func=AF.Copy), reads=[b_bC[d]], writes=[b_KLt[d][par]])
                        yield

                    def step(d, i):
                        c = order[d][i]; par = i % 2; cur = i % 2; nxt = 1 - cur
                        sl = slice(c * 64, (c + 1) * 64)
                        isx = c >= 4; xc = c - 4
                        if isx:
                            S.op("pe", lambda e: e.matmul(PO[d], lhsT=Msc[d][par][:], rhs=Vt[d][par][:], start=True, stop=False), reads=[b_Msc[d][par], b_Vt[d][par]], writes=[b_bA[d]])
                            S.op("pe", lambda e: e.matmul(PO[d], lhsT=QG[d][:, sl], rhs=Sb[d][cur][:], start=False, stop=True), reads=[b_str, b_Sb[d][cur]], writes=[b_bA[d]])
                            S.op("dve", lambda e: e.tensor_tensor(out=Oh[:, xc, :], in0=PO[d], in1=Oh[:, xc, :], op=ALU.add), reads=[b_bA[d], b_O], writes=[b_O])
                        S.op("pe", lambda e: e.matmul(PSn[d], lhsT=KLt[d][par][:], rhs=Vt[d][par][:], start=True, stop=True), reads=[b_KLt[d][par], b_Vt[d][par]], writes=[b_bB[d]])
                        yield
                        S.op("dve", lambda e: e.scalar_tensor_tensor(out=S32[d][:], in0=S32[d][:], scalar=gmh[d][:, c:c + 1], in1=PSn[d], op0=ALU.mult, op1=ALU.add), reads=[b_S32[d], b_str, b_bB[d]], writes=[b_S32[d]])
                        S.op("act", lambda e: e.activation(out=Sb[d][nxt][:], in_=S32[d][:], func=AF.Copy), reads=[b_S32[d]], writes=[b_Sb[d][nxt]])
                        yield

                    run([prep(0, 0), prep(1, 0)])
                    for i in range(NCH):
                        gs = [step(0, i), step(1, i)]
                        if i + 1 < NCH:
                            gs += [prep(0, i + 1), prep(1, i + 1)]
                        run(gs)
                        conv_tiles(cvb, 1)
                    S.barrier()
                with ExitStack() as ph:
                    sqb = sb(ph, "hsq", [64, 64, 128]); b_sqb = Buf()
                    ssm = sb(ph, "hss", [64, 64]); b_ss = Buf()
                    ohT = sb(ph, "ohT", [128, 4096]); b_ohT = Buf()
                    pR = [ps(ph, f"hpR{i}", [128, 512]) for i in range(2)]; b_pR = [PBuf(), PBuf()]
                    S.op("pool", lambda e: e.tensor_tensor(out=sqb[:], in0=Oh[:], in1=Oh[:], op=ALU.mult), reads=[b_O], writes=[b_sqb])
                    S.op("dve", lambda e: e.tensor_reduce(out=ssm[:], in_=sqb[:], axis=AX.X, op=ALU.add), reads=[b_sqb], writes=[b_ss])
                    S.op("act", lambda e: e.activation(out=ssm[:], in_=ssm[:], func=AF.Sqrt, scale=1.0 / 128, bias=EPS), reads=[b_ss], writes=[b_ss])
                    S.op("dve", lambda e: e.reciprocal(out=ssm[:], in_=ssm[:]), reads=[b_ss], writes=[b_ss])
                    S.op("dve", lambda e: e.tensor_tensor(out=Oh[:], in0=Oh[:], in1=ssm[:].unsqueeze(2).to_broadcast([64, 64, 128]), op=ALU.mult), reads=[b_O, b_ss], writes=[b_O])
                    for g8 in range(8):
                        pi = g8 % 2
                        for q in range(8):
                            xc = g8 * 8 + q
                            S.op("pe", lambda e: e.transpose(out=pR[pi][:, q * 64:(q + 1) * 64], in_=Oh[:, xc, :], identity=identf[0:64, 0:64]), reads=[b_O, b_const], writes=[b_pR[pi]])
                        S.op("act", lambda e: e.activation(out=ohT[:, g8 * 512:(g8 + 1) * 512], in_=pR[pi][:, :], func=AF.Copy), reads=[b_pR[pi]], writes=[b_ohT])
                    S.op("dve", lambda e: e.scalar_tensor_tensor(out=ohT[:], in0=ohT[:], scalar=hpr[:, 4:5], in1=sog[:], op0=ALU.mult, op1=ALU.mult), reads=[b_ohT, b_hp, b_sog], writes=[b_ohT])
                    for p in range(8):
                        S.dma(nq(), ag_in_h[hh][p][:, :], ohT[:, 512 * p:512 * (p + 1)], reads=[b_ohT], writes=[b_agin_h[hh]])
                    S.barrier()

        for hh in range(0 if SIMTAIL else 2):
            hgrn_head(hh)
            gather(ag_in_h[hh], ag_out_h[hh], b_agin_h[hh], b_agout_h[hh])
            S.mark(f"hg{hh}_all")
        if SIMTAIL:
            for p in range(8):
                for r in range(4):
                    S.dma("sp", ag_out_rw[p][256 * r:256 * r + 256, :], ag_ref[512 * r:512 * r + 256, 512 * p:512 * (p + 1)], writes=[b_agout_rw])
                    for hh in range(2):
                        S.dma("sp", ag_out_h[hh][p][128 * r:128 * r + 128, :], ag_ref[512 * r + 256 + 128 * hh:512 * r + 384 + 128 * hh, 512 * p:512 * (p + 1)], writes=[b_agout_h[hh]])

        b_x1D = Buf(); b_hx2D = Buf(); b_qD = Buf(); b_out = Buf()
        with ExitStack() as pht:
            mT = sb(pht, "mT", [128, 16, OWN], BF16); b_mT = Buf()
            with ExitStack() as ph:
                yTb = sb(ph, "yTb", [128, 16, OWN], BF16); b_yT = Buf()
                sel = sb(ph, "sel", [128, 4]); b_sel = Buf()
                S.dma("sp", sel[:], selq, writes=[b_sel])
                ld = [sb(ph, f"yl{i}", [128, 4096]) for i in range(2)]; b_ld = [Buf(), Buf()]
                ya = [sb(ph, f"ya{i}", [128, OWN]) for i in range(2)]; b_ya = [Buf(), Buf()]
                for kc in range(16):
                    i = kc % 2
                    kk_ = kc % 8
                    for p in range(8):
                        if kc < 8:
                            src_, bsrc = ag_out_rw[p][256 * (kk_ // 2) + 128 * (kk_ % 2):256 * (kk_ // 2) + 128 * (kk_ % 2) + 128, :], b_agout_rw
                        else:
                            src_, bsrc = ag_out_h[kk_ % 2][p][128 * (kk_ // 2):128 * (kk_ // 2) + 128, :], b_agout_h[kk_ % 2]
                        S.dma(nq(), ld[i][:, 512 * p:512 * (p + 1)], src_, reads=[bsrc], writes=[b_ld[i]])
                    S.op("dve", lambda e: e.tensor_scalar(out=ya[i][:], in0=ld[i][:, 0:OWN], scalar1=sel[:, 0:1], scalar2=None, op0=ALU.mult), reads=[b_ld[i], b_sel], writes=[b_ya[i]])
                    for q in range(1, 4):
                        o_ = yTb[:, kc, :] if q == 3 else ya[i][:]
                        S.op("dve", lambda e: e.scalar_tensor_tensor(out=o_, in0=ld[i][:, q * OWN:(q + 1) * OWN], scalar=sel[:, q:q + 1], in1=ya[i][:], op0=ALU.mult, op1=ALU.add), reads=[b_ld[i], b_sel, b_ya[i]], writes=[b_yT] if q == 3 else [b_ya[i]])
                wuf = [sb(ph, f"wuf{i}", [128, 16, 128]) for i in range(2)]; b_wuf = [Buf(), Buf()]
                wub = [sb(ph, f"wub{i}", [128, 16, 128], BF16) for i in range(2)]; b_wub = [Buf(), Buf()]
                gl = [[sb(ph, f"gl{i}{br}", [128, 512]) for br in range(2)] for i in range(2)]; b_gl = [[Buf(), Buf()] for _ in range(2)]
                pU = [[ps(ph, f"pU{i}{br}", [128, 512]) for br in range(2)] for i in range(2)]; b_pU = [[PBuf(), PBuf()] for _ in range(2)]
                it = 0
                for j in range(16):
                    i = j % 2
                    S.dma(nq(), wuf[i][:], wup[j], writes=[b_wuf[i]])
                    S.op("pool", lambda e: e.tensor_copy(out=wub[i][:], in_=wuf[i][:]), reads=[b_wuf[i]], writes=[b_wub[i]])
                    for tb in range(2):
                        t0 = tb * 512
                        pi = it % 2
                        it += 1
                        for br in range(2):
                            for kc in range(8):
                                S.op("pe", lambda e: e.matmul(pU[pi][br][:, :], lhsT=wub[i][:, 8 * br + kc, :], rhs=yTb[:, 8 * br + kc, t0:t0 + 512], start=(kc == 0), stop=(kc == 7)), reads=[b_wub[i], b_yT], writes=[b_pU[pi][br]])
                            S.dma(nq(), gl[pi][br][:], gT[(16 * br + j) * 128:(16 * br + j + 1) * 128, t0:t0 + 512], reads=[b_gT], writes=[b_gl[pi][br]])
                            S.op("act", lambda e: e.activation(out=gl[pi][br][:], in_=gl[pi][br][:], func=AF.Sigmoid), reads=[b_gl[pi][br]], writes=[b_gl[pi][br]])
                            S.op("dve", lambda e: e.tensor_tensor(out=gl[pi][br][:], in0=pU[pi][br][:, :], in1=gl[pi][br][:], op=ALU.mult), reads=[b_pU[pi][br], b_gl[pi][br]], writes=[b_gl[pi][br]])
                        S.op("pool", lambda e: e.tensor_tensor(out=mT[:, j, t0:t0 + 512], in0=gl[pi][0][:], in1=gl[pi][1][:], op=ALU.add), reads=[b_gl[pi][0], b_gl[pi][1]], writes=[b_mT])
                S.barrier()
            S.mark("tailA1_up_merge")
            hx2T = sb(pht, "hx2T", [128, 16, OWN], BF16)
            with ExitStack() as ph:
                wof = sb(ph, "wof", [128, 16, 512]); b_wof = Buf()
                wob = [sb(ph, f"wob{n}", [128, 16, 512], BF16) for n in range(4)]; b_wob = Buf()
                for n in range(4):
                    S.dma(nq(), wof[:], wo[n], writes=[b_wof])
                    S.op("pool" if n % 2 else "dve", lambda e: e.tensor_copy(out=wob[n][:], in_=wof[:]), reads=[b_wof], writes=[b_wob])
                gmb = sb(ph, "gmb", [128, D]); b_gmb = Buf()
                bc_load("sp", gmb[:], modD[0:1, 2 * D:3 * D], [b_gmb], reads=[b_modD])
                xt = [sb(ph, f"xt2{i}", [128, D]) for i in range(2)]; b_xt = [Buf(), Buf()]
                o1 = [sb(ph, f"o1{i}", [128, D]) for i in range(2)]; b_o1 = [Buf(), Buf()]
                pO = [ps(ph, f"pO{n}", [128, 512]) for n in range(4)]; b_pO = [PBuf() for _ in range(4)]
                for tl in range(8):
                    i = tl % 2
                    S.dma(nq(), xt[i][:], xown[tl * 128:(tl + 1) * 128, :], writes=[b_xt[i]])
                    for n in range(4):
                        for k in range(16):
                            S.op("pe", lambda e: e.matmul(pO[n][:, :], lhsT=mT[:, k, tl * 128:(tl + 1) * 128], rhs=wob[n][:, k, :], start=(k == 0), stop=(k == 15)), reads=[b_mT, b_wob], writes=[b_pO[n]])
                        S.op("dve", lambda e: e.tensor_tensor(out=o1[i][:, n * 512:(n + 1) * 512], in0=pO[n][:, :], in1=gmb[:, n * 512:(n + 1) * 512], op=ALU.mult), reads=[b_pO[n], b_gmb], writes=[b_o1[i]])
                    S.op("pool", lambda e: e.tensor_tensor(out=o1[i][:], in0=o1[i][:], in1=xt[i][:], op=ALU.add), reads=[b_o1[i], b_xt[i]], writes=[b_o1[i]])
                    S.dma(nq(), x1D[tl * 128:(tl + 1) * 128, :], o1[i][:], reads=[b_o1[i]], writes=[b_x1D])
                S.barrier()
            with ExitStack() as ph:
                A2, B2, bA2, bB2 = make_AB(ph, 0, 1, 4 * D, 3 * D, "f")
                b_hx2T = norm_tiles(ph, x1D, 8, lambda tl: (A2, B2, bA2, bB2), hx2T, "f", src_buf=b_x1D, store=(hx2D, b_hx2D))
                S.barrier()
            with ExitStack() as ph:
                project(ph, wq, 16, hx2T, b_hx2T, OWN, qD, b_qD, "q")
                S.barrier()
        S.mark("tailA2B_out_norm_q")
        with ExitStack() as ph:
            cvb = cbufs(ph)
            conv_tiles(cvb, 512)
            S.barrier()
        with ExitStack() as ph:
            kT = sb(ph, "kT", [128, 2, 128]); b_kT = Buf()
            S.dma("sp", kT[:, 0, :], k1T, writes=[b_kT])
            S.dma("act", kT[:, 1, :], k2T, writes=[b_kT])
            gfb = sb(ph, "gfb", [128, D]); nfb = sb(ph, "nfb", [128, D]); b_cb = Buf()
            bc_load("sp", gfb[:], modD[0:1, 5 * D:6 * D], [b_cb], reads=[b_modD])
            bc_load("act", nfb[:], nrm[2:3, :], [b_cb])
            qt = sb(ph, "qt", [128, 16, 128]); b_qt = Buf()
            sc = sb(ph, "sc", [128, 16, 128]); b_sc = Buf()
            sc2 = sb(ph, "sc2", [128, 256]); b_sc2 = Buf()
            v16 = sb(ph, "v16", [128, 16, 16]); i16 = sb(ph, "i16", [128, 16, 16]); iu = sb(ph, "iu", [128, 16], U32); b_v16 = Buf(); b_iu = Buf()
            cand = sb(ph, "cand", [128, 8, 256]); ecand = sb(ph, "ecand", [128, 8, 256]); b_cand = Buf(); b_ecand = Buf()
            best = sb(ph, "best", [128, 8, 16]); b_best = Buf()
            eid_ = [sb(ph, f"eid{i}", [128, 128]) for i in range(2)]; eidi_ = [sb(ph, f"eidi{i}", [128, 128], I32) for i in range(2)]; b_eid_ = [Buf(), Buf()]
            gate_ = [sb(ph, f"gate{i}", [128, 8, 16]) for i in range(2)]; gs_ = [sb(ph, f"gs{i}", [128, 8]) for i in range(2)]; b_gate_ = [Buf(), Buf()]
            dots = sb(ph, "dots", [128, 128]); coef = sb(ph, "coef", [128, 128]); b_dots = Buf(); b_coef = Buf()
            b_dsl = [Buf() for _ in range(128)]; b_csl = [Buf() for _ in range(128)]
            hx2_ = [sb(ph, f"hx2{i}", [128, D]) for i in range(2)]; b_hx2_ = [Buf(), Buf()]
            hx2b_ = [sb(ph, f"hx2b{i}", [128, D], BF16) for i in range(2)]; b_hx2b_ = [Buf(), Buf()]
            x1t_ = [sb(ph, f"x1t{i}", [128, D]) for i in range(2)]; b_x1t_ = [Buf(), Buf()]
            acc = sb(ph, "acc", [128, D]); b_acc = Buf()
            junk = sb(ph, "junk", [128, D], BF16); b_junk = Buf()
            junk2 = [sb(ph, f"junkd{i}", [128, D], BF16) for i in range(2)]; b_junk2 = [Buf(), Buf()]
            NB = 10
            gb = [sb(ph, f"gb{i}", [128, 2 * D], BF16) for i in range(NB)]; b_gb = [Buf() for _ in range(NB)]
            dg = [sb(ph, f"dg{i}", [128, 128], BF16) for i in range(8)]; b_dg = [Buf() for _ in range(8)]
            pacc = [ps(ph, f"pacc{n}", [128, 512]) for n in range(4)]; b_pacc = [PBuf() for _ in range(4)]
            st2 = sb(ph, "st2", [128, 2]); b_st2 = Buf()
            psc = [ps(ph, f"psc{i}", [128, 512]) for i in range(2)]; b_psc = [PBuf(), PBuf()]
            gi = 0
            qDv = qD.rearrange("(j p) t -> p j t", p=128)
            def adv(g_, n):
                if g_ is None:
                    return
                for _ in range(n):
                    try:
                        next(g_)
                    except StopIteration:
                        return

            def topk_gen(tl, pb):
                tsl = slice(tl * 128, (tl + 1) * 128)
                S.dma("sp", qt[:], qDv[:, :, tsl], reads=[b_qD], writes=[b_qt])
                S.dma("sp", hx2_[pb][:], hx2D[tsl, :], reads=[b_hx2D], writes=[b_hx2_[pb]])
                S.op("pool", lambda e: e.tensor_copy(out=hx2b_[pb][:], in_=hx2_[pb][:]), reads=[b_hx2_[pb]], writes=[b_hx2b_[pb]])
                S.dma("sp", x1t_[pb][:], x1D[tsl, :], reads=[b_x1D], writes=[b_x1t_[pb]])
                for g4 in range(4):
                    pi = g4 % 2
                    for u in range(4):
                        hh = g4 * 4 + u
                        S.op("pe", lambda e: e.matmul(psc[pi][:, u * 128:(u + 1) * 128], lhsT=qt[:, hh, :], rhs=kT[:, hh % 2, :], start=True, stop=True), reads=[b_qt, b_kT], writes=[b_psc[pi]])
                    S.op("dve", lambda e: e.tensor_copy(out=sc[:, g4 * 4:(g4 + 1) * 4, :], in_=v3(psc[pi][:, :], 4)), reads=[b_psc[pi]], writes=[b_sc])
                for hh in range(16):
                    S.op("dve", lambda e: e.max(out=v16[:, hh, 0:8], in_=sc[:, hh, :]), reads=[b_sc], writes=[b_v16])
                    yield
                    S.op("dve", lambda e: e.max_index(out=iu[:, 0:8], in_max=v16[:, hh, 0:8], in_values=sc[:, hh, :]), reads=[b_sc, b_v16], writes=[b_iu])
                    S.op("dve", lambda e: e.match_replace(out=sc2[:, 0:128], in_to_replace=v16[:, hh, 0:8], in_values=sc[:, hh, :], imm_value=-1e30), reads=[b_sc, b_v16], writes=[b_sc2])
                    yield
                    S.op("dve", lambda e: e.max(out=v16[:, hh, 8:16], in_=sc2[:, 0:128]), reads=[b_sc2], writes=[b_v16])
                    S.op("dve", lambda e: e.max_index(out=iu[:, 8:16], in_max=v16[:, hh, 8:16], in_values=sc2[:, 0:128]), reads=[b_sc2, b_v16], writes=[b_iu])
                    yield
                    S.op("dve", lambda e: e.tensor_copy(out=i16[:, hh, :], in_=iu[:, :]), reads=[b_iu], writes=[b_v16])
                for h in range(8):
                    S.op("dve", lambda e: e.tensor_tensor(out=cand[:, h, :].rearrange("p (a b) -> p a b", a=16), in0=v16[:, 2 * h, :].unsqueeze(2).to_broadcast([128, 16, 16]), in1=v16[:, 2 * h + 1, :].unsqueeze(1).to_broadcast([128, 16, 16]), op=ALU.add), reads=[b_v16], writes=[b_cand])
                    yield
                    S.op("dve", lambda e: e.scalar_tensor_tensor(out=ecand[:, h, :].rearrange("p (a b) -> p a b", a=16), in0=i16[:, 2 * h, :].unsqueeze(2).to_broadcast([128, 16, 16]), scalar=128.0, in1=i16[:, 2 * h + 1, :].unsqueeze(1).to_broadcast([128, 16, 16]), op0=ALU.mult, op1=ALU.add), reads=[b_v16], writes=[b_ecand])
                    S.op("dve", lambda e: e.max(out=best[:, h, 0:8], in_=cand[:, h, :]), reads=[b_cand], writes=[b_best])
                    yield
                    S.op("dve", lambda e: e.match_replace(out=sc2[:, :], in_to_replace=best[:, h, 0:8], in_values=cand[:, h, :], imm_value=-1e30), reads=[b_cand, b_best], writes=[b_sc2])
                    S.op("dve", lambda e: e.max(out=best[:, h, 8:16], in_=sc2[:, :]), reads=[b_sc2], writes=[b_best])
                    yield
                S.op("dve", lambda e: e.memset(eid_[pb][:], 0.0), writes=[b_eid_[pb]])
                for h in range(8):
                    for n in range(16):
                        S.op("dve", lambda e: e.scalar_tensor_tensor(out=sc2[:, :], in0=cand[:, h, :], scalar=best[:, h, n:n + 1], in1=ecand[:, h, :], op0=ALU.is_equal, op1=ALU.mult, accum_out=eid_[pb][:, h * 16 + n:h * 16 + n + 1]), reads=[b_cand, b_ecand, b_best], writes=[b_sc2, b_eid_[pb]])
                        yield
                S.op("dve", lambda e: e.tensor_scalar_min(out=eid_[pb][:], in0=eid_[pb][:], scalar1=16383.0), reads=[b_eid_[pb]], writes=[b_eid_[pb]])
                S.op("dve", lambda e: e.tensor_copy(out=eidi_[pb][:], in_=eid_[pb][:]), reads=[b_eid_[pb]], writes=[b_eid_[pb]])
                yield
                S.op("dve", lambda e: e.tensor_tensor(out=gate_[pb][:], in0=best[:], in1=best[:, :, 0:1].to_broadcast([128, 8, 16]), op=ALU.subtract), reads=[b_best], writes=[b_gate_[pb]])
                S.op("act", lambda e: e.activation(out=gate_[pb][:], in_=gate_[pb][:], func=AF.Exp), reads=[b_gate_[pb]], writes=[b_gate_[pb]])
                yield
                S.op("dve", lambda e: e.tensor_reduce(out=gs_[pb][:], in_=gate_[pb][:], axis=AX.X, op=ALU.add), reads=[b_gate_[pb]], writes=[b_gate_[pb]])
                S.op("dve", lambda e: e.reciprocal(out=gs_[pb][:], in_=gs_[pb][:]), reads=[b_gate_[pb]], writes=[b_gate_[pb]])
                yield
                S.op("dve", lambda e: e.tensor_tensor(out=gate_[pb][:], in0=gate_[pb][:], in1=gs_[pb][:].unsqueeze(2).to_broadcast([128, 8, 16]), op=ALU.mult), reads=[b_gate_[pb]], writes=[b_gate_[pb]])

                yield

            def gather_fin(tl, pb, nxt):
                nonlocal_gi = gi_box
                tsl = slice(tl * 128, (tl + 1) * 128)
                S.op("pool", lambda e: e.memset(dots[:], 0.0), writes=b_dsl)
                gflat = gate_[pb][:].rearrange("p a b -> p (a b)")
                for s_ in range(128):
                    bi = gi_box[0] % NB
                    gi_box[0] += 1
                    dj = s_ % 8
                    S.idma(out=gb[bi][:], out_offset=None, in_=uvD, in_offset=bass.IndirectOffsetOnAxis(ap=eidi_[pb][:, s_:s_ + 1], axis=0), reads=[b_eid_[pb], b_tab], writes=[b_gb[bi]])
                    S.op("dve", lambda e: e.scalar_tensor_tensor(out=junk2[s_ % 2][:], in0=gb[bi][:, 0:D], scalar=1.0, in1=hx2b_[pb][:], op0=ALU.mult, op1=ALU.mult, accum_out=dots[:, s_:s_ + 1]), reads=[b_gb[bi], b_hx2b_[pb]], writes=[b_junk2[s_ % 2], b_dsl[s_]])
                    adv(nxt, 2)
                    S.op("act", lambda e: e.activation(out=coef[:, s_:s_ + 1], in_=dots[:, s_:s_ + 1], func=AF.Gelu), reads=[b_dsl[s_]], writes=[b_csl[s_]])
                    S.op("act", lambda e: e.activation(out=coef[:, s_:s_ + 1], in_=coef[:, s_:s_ + 1], func=AF.Copy, scale=gflat[:, s_:s_ + 1]), reads=[b_csl[s_], b_gate_[pb]], writes=[b_csl[s_]])
                    S.op("act", lambda e: e.activation(out=dg[dj][:], in_=identf[:], func=AF.Copy, scale=coef[:, s_:s_ + 1]), reads=[b_const, b_csl[s_]], writes=[b_dg[dj]])
                    for n in range(4):
                        S.op("pe", lambda e: e.matmul(pacc[n][:, :], lhsT=dg[dj][:], rhs=gb[bi][:, D + n * 512:D + (n + 1) * 512], start=(s_ == 0), stop=(s_ == 127)), reads=[b_dg[dj], b_gb[bi]], writes=[b_pacc[n]])
                for n in range(4):
                    S.op("dve", lambda e: e.tensor_tensor(out=acc[:, n * 512:(n + 1) * 512], in0=pacc[n][:, :], in1=gfb[:, n * 512:(n + 1) * 512], op=ALU.mult), reads=[b_pacc[n], b_cb], writes=[b_acc])
                S.op("pool", lambda e: e.tensor_tensor(out=acc[:], in0=acc[:], in1=x1t_[pb][:], op=ALU.add), reads=[b_acc, b_x1t_[pb]], writes=[b_acc])
                S.op("dve", lambda e: e.memset(st2[:], 0.0), writes=[b_st2])
                S.op("act", lambda e: e.activation(out=junk[:], in_=acc[:], func=AF.Square, accum_out=st2[:, 0:1]), reads=[b_acc], writes=[b_junk, b_st2])
                S.op("act", lambda e: e.activation(out=st2[:, 1:2], in_=st2[:, 0:1], func=AF.Sqrt, scale=1.0 / D, bias=EPS), reads=[b_st2], writes=[b_st2])
                S.op("dve", lambda e: e.reciprocal(out=st2[:, 1:2], in_=st2[:, 1:2]), reads=[b_st2], writes=[b_st2])
                S.op("dve", lambda e: e.scalar_tensor_tensor(out=acc[:], in0=acc[:], scalar=st2[:, 1:2], in1=nfb[:], op0=ALU.mult, op1=ALU.mult), reads=[b_acc, b_st2, b_cb], writes=[b_acc])
                S.dma("sp", out_d[tsl, :], acc[:], reads=[b_acc], writes=[b_out])

            gi_box = [0]
            g0 = topk_gen(0, 0)
            adv(g0, 100000)
            for tl in range(OWN // 128):
                nxt = topk_gen(tl + 1, (tl + 1) % 2) if tl + 1 < OWN // 128 else None
                gather_fin(tl, tl % 2, nxt)
                adv(nxt, 100000)
            S.barrier()
        S.mark("peer")
        S.finish()
    return nc


_CACHE = {}


def _prep(inputs):
    f = lambda a: np.ascontiguousarray(np.asarray(a, dtype=np.float32))
    x = f(inputs["x"]); ctx = f(inputs["ctx"]); c = f(inputs["c"]); c_ctx = f(inputs["c_ctx"])
    w_in = f(inputs["w_in"])[0]
    w_ada_all = np.ascontiguousarray(f(inputs["w_ada"])[0].reshape(16, 128, 24, 512).transpose(2, 1, 0, 3))
    b_ada_all = f(inputs["b_ada"]).reshape(1, -1)
    nrm = np.stack([f(inputs["norm_mix"])[0], f(inputs["norm_ffn"])[0], f(inputs["norm_final"])])
    rw_conv = f(inputs["rw_conv"])[0].reshape(9, -1)

    def arr_w(cols):
        w = w_in[:, cols]
        n = w.shape[1] // 128
        return np.ascontiguousarray(w.reshape(16, 128, n, 128).transpose(2, 1, 0, 3))

    s_ = np.arange(128)[:, None] % 64; t_ = np.arange(128)[None, :] % 64
    rb = np.arange(128)[:, None] // 64; cb = np.arange(128)[None, :] // 64
    cm = np.zeros((9, 128, 128), np.float32)
    cm[0] = np.where(cb == 0, s_ < t_, s_ <= t_)
    cm[1] = np.where(cb == 0, s_ > t_, s_ >= t_)
    cm[2] = -1.0 * ((rb == cb) & (s_ < t_))
    cm[3] = -1.0 * ((rb == cb) & (s_ > t_))
    cm[4] = -1.0 * ((rb == cb) & (t_ < s_))
    cm[5] = -1.0 * ((rb == cb) & (t_ > s_))
    cm[6] = (rb == cb)
    cm[7] = (s_ <= t_) & (rb == 0) & (cb == 0)
    cm[8] = (s_ >= t_) & (rb == 0) & (cb == 0)
    g_ = lambda n: f(inputs[n])[0]
    rw_w0, rw_a0 = g_("rw_w0"), g_("rw_a0")
    rw_kk, rw_ka, rw_rk = g_("rw_k_k"), g_("rw_k_a"), g_("rw_r_k").reshape(-1)
    rw_lnw, rw_lnb = g_("rw_ln_w"), g_("rw_ln_b")
    wlb_f = g_("rw_w_lora_b").reshape(128, 1024); alb_f = g_("rw_a_lora_b").reshape(128, 1024); glb_f = g_("rw_g_lora_b")
    maps = []
    for core in range(8):
        b, g = core // 4, core % 4
        own = 256 * g + np.arange(256)
        cols = []
        for hp in range(2):
            for base in (0, 1024, 2048):
                cols.append(base + own[hp * 128:(hp + 1) * 128])
        lora = 3072 + np.arange(416)
        cols.append(lora[0:128]); cols.append(lora[128:256]); cols.append(lora[256:384])
        rwcols = np.concatenate(cols + [lora[384:416]])
        hg = []
        for hh in range(2):
            for s in range(5):
                hg.append(3488 + s * 1024 + own[hh * 128:(hh + 1) * 128])
        wfull = np.zeros((2048, 20 * 128), np.float32)
        wfull[:, 0:9 * 128] = w_in[:, np.concatenate(cols)]
        wfull[:, 9 * 128:9 * 128 + 32] = w_in[:, lora[384:416]]
        wfull[:, 10 * 128:] = w_in[:, np.concatenate(hg)]
        wA = np.ascontiguousarray(wfull.reshape(16, 128, 20, 128).transpose(2, 1, 0, 3))
        convw = np.zeros((10, 128, 9), np.float32)
        cc = np.concatenate(cols)
        convw[0:9] = rw_conv[:, cc].T.reshape(9, 128, 9)
        convw[9, 0:32] = rw_conv[:, lora[384:416]].T
        m = {
            "seq": np.concatenate([ctx[b], x[b]], axis=0),
            "xown": x[b, 1024 * g:1024 * (g + 1)],
            "cmod": np.ascontiguousarray(np.stack([c[b], c_ctx], axis=-1).reshape(16, 128, 2).transpose(1, 0, 2)),
            "w_ada": w_ada_all[6 * g:6 * g + 6], "b_ada": np.ascontiguousarray(b_ada_all[:, 3072 * g:3072 * (g + 1)]), "nrm": nrm, "wA": wA,
            "wB": None, "convw": convw, "cmask": cm,
        }
        rwp = np.zeros((2, 128, 9), np.float32)
        for hp in range(2):
            ch = own[hp * 128:(hp + 1) * 128]
            rwp[hp] = np.stack([rw_w0[0, ch], rw_w0[1, ch], rw_a0[0, ch], rw_a0[1, ch], rw_kk[ch], rw_ka[ch], rw_rk[ch], rw_lnw[ch], rw_lnb[ch]], axis=1)
        m["rwp"] = rwp
        hl = f(inputs["hg_lb"])
        m["hlb"] = np.ascontiguousarray(np.stack([np.stack([hl[0, 0, own[hh * 128:(hh + 1) * 128]], hl[0, 1, own[hh * 128:(hh + 1) * 128]], hl[1, 0, own[hh * 128:(hh + 1) * 128]], hl[1, 1, own[hh * 128:(hh + 1) * 128]]], axis=1) for hh in range(2)]))
        m["hgn"] = np.ascontiguousarray(f(inputs["hg_norm"])[0].reshape(128, 1))
        m["wlb"] = np.ascontiguousarray(np.stack([wlb_f[:, own[hp * 128:(hp + 1) * 128]] for hp in range(2)]))
        m["alb"] = np.ascontiguousarray(np.stack([alb_f[:, own[hp * 128:(hp + 1) * 128]] for hp in range(2)]))
        m["glb"] = np.ascontiguousarray(np.stack([glb_f[:, own[hp * 128:(hp + 1) * 128]] for hp in range(2)]))
        maps.append(m)
    wB = arr_w(8608 + np.arange(4096))
    wupr = f(inputs["w_up_rw"])[0]; wuph = f(inputs["w_up_hg"])[0]
    wcat = np.concatenate([wupr, wuph], axis=0)
    wup = np.ascontiguousarray(wcat.reshape(16, 128, 16, 128).transpose(2, 1, 0, 3))
    wo = np.ascontiguousarray(f(inputs["w_out"])[0].reshape(16, 128, 4, 512).transpose(2, 1, 0, 3))
    wq = np.ascontiguousarray(f(inputs["peer_wq"])[0].reshape(16, 128, 16, 128).transpose(2, 1, 0, 3))
    k1T = np.ascontiguousarray(f(inputs["peer_k1"])[0].T); k2T = np.ascontiguousarray(f(inputs["peer_k2"])[0].T)
    pu = f(inputs["peer_u"])[0]; pv = f(inputs["peer_v"])[0]
    for core, m in enumerate(maps):
        m["wB"] = wB
        sq = np.zeros((128, 4), np.float32); sq[:, core % 4] = 1.0
        m.update(selq=sq, wup=wup, wo=wo, wq=wq, k1T=k1T, k2T=k2T, peer_u=pu, peer_v=pv)
    return maps


def kernel(**inputs):
    maps = _prep(inputs)
    nc = build_nc(STAGE)
    res = run_bass_kernel_spmd(nc, maps, core_ids=list(range(8)))
    _CACHE["res"] = res
    out = np.zeros((2, 4096, D), np.float32)
    for core in range(8):
        b, g = core // 4, core % 4
        out[b, 1024 * g:1024 * (g + 1)] = res.results[core]["out"]
    return out
```
